# Optimizing a Trainium2 kernel written in Bass

```python
import jax, jax.numpy as jnp
from jax import lax
import numpy as np

D_MODEL = 1024
BATCH = 8
SEQ = 2048
DEPTH = 2

ATTN_WIDTH = D_MODEL // 2
ATTN_HEAD_DIM = 64
ATTN_HEADS = ATTN_WIDTH // ATTN_HEAD_DIM
MOBA_BLOCK = 256
MOBA_TOPK = 3
Q_CHUNK = 128
SSD_INNER = D_MODEL // 2
SSD_HEAD_DIM = 64
SSD_HEADS = SSD_INNER // SSD_HEAD_DIM
SSD_GROUPS = 2
SSD_STATE = 128
SSD_CONV = 4
SSD_CHUNK = 256
SSD_CONV_CH = SSD_INNER + 2 * SSD_GROUPS * SSD_STATE
IN_COLS = 3 * ATTN_WIDTH + SSD_INNER + SSD_CONV_CH + SSD_HEADS
MIX_WIDTH = ATTN_WIDTH + SSD_INNER
CONF_KERNEL = 31
FFN_HIDDEN = -(-8 * D_MODEL // (3 * 256)) * 256
N_EVEN = (DEPTH + 1) // 2
N_ODD = DEPTH // 2
DN_ALPHA = (2 * DEPTH) ** 0.25
DN_BETA = (8 * DEPTH) ** -0.25
LN_EPS = 1e-5
RMS_EPS = 1e-5

kernel_name = 'moba_mamba2_conformer_deepnorm_hybrid'


def layer_norm(x, g, b):
    xf = x.astype(jnp.float32)
    mu = xf.mean(-1, keepdims=True)
    var = jnp.square(xf - mu).mean(-1, keepdims=True)
    y = (xf - mu) * lax.rsqrt(var + LN_EPS) * g.astype(jnp.float32) + b.astype(jnp.float32)
    return y.astype(x.dtype)


def pad_seq(x, mult, axis=1):
    p = (-x.shape[axis]) % mult
    if p == 0:
        return x
    pads = [(0, 0)] * x.ndim
    pads[axis] = (0, p)
    return jnp.pad(x, pads)


def causal_depthwise_conv(x, w, b):
    k_w, ch = w.shape
    y = lax.conv_general_dilated(x, w[:, None, :].astype(x.dtype), window_strides=(1,),
                                 padding=[(k_w - 1, 0)],
                                 dimension_numbers=('NWC', 'WIO', 'NWC'),
                                 feature_group_count=ch)
    return y + b


def alibi_slopes(n):
    return 2.0 ** (-8.0 * jnp.arange(1, n + 1, dtype=jnp.float32) / n)


def moba_attention(q, k, v):
    bsz, seq, heads, dh = q.shape
    n_blk = -(-seq // MOBA_BLOCK)
    n_sel = min(MOBA_TOPK, max(n_blk - 1, 1))
    n_qc = seq // Q_CHUNK
    k_blk = pad_seq(k, MOBA_BLOCK).transpose(0, 2, 1, 3).reshape(bsz, heads, n_blk, MOBA_BLOCK, dh)
    v_blk = pad_seq(v, MOBA_BLOCK).transpose(0, 2, 1, 3).reshape(bsz, heads, n_blk, MOBA_BLOCK, dh)
    k_mean = k_blk.mean(axis=3)
    q_ch = q.transpose(0, 2, 1, 3).reshape(bsz, heads, n_qc, Q_CHUNK, dh)
    slopes = alibi_slopes(heads)
    scale = dh ** -0.5
    h_idx = jnp.arange(heads)[:, None, None]
    blk_ids = jnp.arange(n_blk)
    in_blk = jnp.arange(MOBA_BLOCK)

    def one_chunk(n):
        b = n // n_qc
        c = n % n_qc
        qi = q_ch[b, :, c]
        kb, vb, km = k_blk[b], v_blk[b], k_mean[b]
        q_start = c * Q_CHUNK
        own = q_start // MOBA_BLOCK
        q_pos = q_start + jnp.arange(Q_CHUNK)
        gate = jnp.einsum('hqd,hnd->hqn', qi, km).astype(jnp.float32)
        gate = jnp.where(blk_ids < own, gate, -jnp.inf)
        _, sel = lax.top_k(gate, n_sel)
        valid = sel < own
        k_sel = kb[h_idx, sel]
        v_sel = vb[h_idx, sel]
        s_sel = jnp.einsum('hqd,hqkjd->hqkj', qi, k_sel).astype(jnp.float32) * scale
        dist_sel = (q_pos[None, :, None, None] - (sel[..., None] * MOBA_BLOCK + in_blk)).astype(jnp.float32)
        s_sel = jnp.where(valid[..., None], s_sel - slopes[:, None, None, None] * dist_sel, -jnp.inf)
        k_own = kb[:, own]
        v_own = vb[:, own]
        s_own = jnp.einsum('hqd,hjd->hqj', qi, k_own).astype(jnp.float32) * scale
        dist_own = q_pos[:, None] - (own * MOBA_BLOCK + in_blk)[None, :]
        s_own = jnp.where(dist_own >= 0,
                          s_own - slopes[:, None, None] * dist_own.astype(jnp.float32), -jnp.inf)
        scores = jnp.concatenate([s_sel.reshape(heads, Q_CHUNK, n_sel * MOBA_BLOCK), s_own], axis=-1)
        p = jax.nn.softmax(scores, axis=-1).astype(v.dtype)
        p_sel = p[..., :n_sel * MOBA_BLOCK].reshape(heads, Q_CHUNK, n_sel, MOBA_BLOCK)
        p_own = p[..., n_sel * MOBA_BLOCK:]
        return (jnp.einsum('hqkj,hqkjd->hqd', p_sel, v_sel)
                + jnp.einsum('hqj,hjd->hqd', p_own, v_own))

    out = lax.map(one_chunk, jnp.arange(bsz * n_qc))
    return out.reshape(bsz, n_qc, heads, Q_CHUNK, dh).transpose(0, 1, 3, 2, 4).reshape(bsz, seq, heads * dh)


def ssd_chunked_scan(xs, dt, a, bm, cm):
    bsz, seq, heads, hp = xs.shape
    groups, n_st = bm.shape[2], bm.shape[3]
    rep = heads // groups
    L = SSD_CHUNK
    xs_p, dt_p, bm_p, cm_p = [pad_seq(t, L) for t in (xs, dt, bm, cm)]
    n_c = xs_p.shape[1] // L
    x_dt = (xs_p * dt_p[..., None].astype(xs.dtype)).reshape(bsz, n_c, L, groups, rep, hp)
    bm_c = bm_p.reshape(bsz, n_c, L, groups, n_st)
    cm_c = cm_p.reshape(bsz, n_c, L, groups, n_st)
    a_dt = (dt_p * a).reshape(bsz, n_c, L, groups, rep).transpose(0, 3, 4, 1, 2)
    a_cs = jnp.cumsum(a_dt, axis=-1)
    causal = jnp.tril(jnp.ones((L, L), dtype=bool))
    seg = jnp.where(causal, a_cs[..., :, None] - a_cs[..., None, :], -jnp.inf)
    decay_in = jnp.exp(seg).astype(xs.dtype)
    cb = jnp.einsum('bclgn,bcsgn->bgcls', cm_c, bm_c)
    y_diag = jnp.einsum('bgrcls,bcsgrp->bclgrp', cb[:, :, None] * decay_in, x_dt)
    decay_to_end = jnp.exp(a_cs[..., -1:] - a_cs).astype(xs.dtype).transpose(0, 3, 4, 1, 2)
    chunk_states = jnp.einsum('bcsgn,bcsgrp->bcgrpn', bm_c, x_dt * decay_to_end[..., None])
    chunk_decay = jnp.exp(a_cs[..., -1])

    def step(h, inp):
        st, dec = inp
        return h * dec[..., None, None] + st, h

    h0 = jnp.zeros((bsz, groups, rep, hp, n_st), jnp.float32)
    _, h_in = lax.scan(step, h0, (jnp.moveaxis(chunk_states.astype(jnp.float32), 1, 0),
                                  jnp.moveaxis(chunk_decay, -1, 0)))
    decay_from_start = jnp.exp(a_cs).astype(xs.dtype).transpose(0, 3, 4, 1, 2)
    y_off = jnp.einsum('bclgn,cbgrpn->bclgrp', cm_c, h_in.astype(xs.dtype)) * decay_from_start[..., None]
    return (y_diag + y_off).reshape(bsz, n_c * L, heads, hp)[:, :seq]


def gated_rmsnorm(y, z, w):
    g = (y * jax.nn.silu(z)).astype(jnp.float32)
    sh = g.shape
    g = g.reshape(sh[0], sh[1], SSD_GROUPS, -1)
    g = g * lax.rsqrt(jnp.mean(jnp.square(g), axis=-1, keepdims=True) + RMS_EPS)
    return (g.reshape(sh) * w.astype(jnp.float32)).astype(y.dtype)


def moba_ssd_mixer(x, w_in, conv_w, conv_b, dt_bias, a_log, d_skip, norm_w, w_out):
    bsz, seq, _ = x.shape
    h = x @ w_in
    cuts = [ATTN_WIDTH, 2 * ATTN_WIDTH, 3 * ATTN_WIDTH, 3 * ATTN_WIDTH + SSD_INNER,
            3 * ATTN_WIDTH + SSD_INNER + SSD_CONV_CH]
    q, k, v, z, xbc, dt = jnp.split(h, cuts, axis=-1)
    hs = (bsz, seq, ATTN_HEADS, ATTN_HEAD_DIM)
    attn = moba_attention(q.reshape(hs), k.reshape(hs), v.reshape(hs))
    xbc = jax.nn.silu(causal_depthwise_conv(xbc, conv_w, conv_b))
    xs, bm, cm = jnp.split(xbc, [SSD_INNER, SSD_INNER + SSD_GROUPS * SSD_STATE], axis=-1)
    xs = xs.reshape(bsz, seq, SSD_HEADS, SSD_HEAD_DIM)
    bm = bm.reshape(bsz, seq, SSD_GROUPS, SSD_STATE)
    cm = cm.reshape(bsz, seq, SSD_GROUPS, SSD_STATE)
    dt = jax.nn.softplus((dt + dt_bias).astype(jnp.float32))
    a = -jnp.exp(a_log.astype(jnp.float32))
    y = ssd_chunked_scan(xs, dt, a, bm, cm) + xs * d_skip[:, None]
    y = gated_rmsnorm(y.reshape(bsz, seq, SSD_INNER), z, norm_w)
    return jnp.concatenate([attn, y], axis=-1) @ w_out


def conformer_conv_module(x, w_pw1, b_pw1, dw_w, dw_b, ln_g, ln_b, w_pw2, b_pw2):
    h = x @ w_pw1 + b_pw1
    val, gate = jnp.split(h, 2, axis=-1)
    h = val * jax.nn.sigmoid(gate)
    h = causal_depthwise_conv(h, dw_w, dw_b)
    h = jax.nn.silu(layer_norm(h, ln_g, ln_b))
    return h @ w_pw2 + b_pw2


def swiglu_ffn(x, w_gate, w_up, w_down):
    return (jax.nn.silu(x @ w_gate) * (x @ w_up)) @ w_down


def setup_inputs(seed: int = 0) -> dict:
    key = jax.random.key(seed)
    ks = jax.random.split(key, 32)
    f32 = jnp.float32

    def nrm(k, shape, scale):
        return jax.random.normal(k, shape, f32) * scale

    dt_init = jnp.exp(jax.random.uniform(ks[5], (N_EVEN, SSD_HEADS), f32)
                      * (np.log(0.1) - np.log(0.001)) + np.log(0.001))
    inp = {
        'x': nrm(ks[0], (BATCH, SEQ, D_MODEL), 1.0),
        'mix_w_in': nrm(ks[1], (N_EVEN, D_MODEL, IN_COLS), D_MODEL ** -0.5),
        'ssd_conv_w': nrm(ks[2], (N_EVEN, SSD_CONV, SSD_CONV_CH), SSD_CONV ** -0.5),
        'ssd_conv_b': nrm(ks[3], (N_EVEN, SSD_CONV_CH), 0.02),
        'ssd_dt_bias': dt_init + jnp.log(-jnp.expm1(-dt_init)),
        'ssd_a_log': jnp.log(jax.random.uniform(ks[6], (N_EVEN, SSD_HEADS), f32, 1.0, 16.0)),
        'ssd_d': 1.0 + nrm(ks[7], (N_EVEN, SSD_HEADS), 0.02),
        'ssd_norm_w': 1.0 + nrm(ks[8], (N_EVEN, SSD_INNER), 0.02),
        'mix_w_out': nrm(ks[9], (N_EVEN, MIX_WIDTH, D_MODEL), MIX_WIDTH ** -0.5 * DN_BETA),
        'conv_w_pw1': nrm(ks[10], (N_ODD, D_MODEL, 2 * D_MODEL), D_MODEL ** -0.5),
        'conv_b_pw1': nrm(ks[11], (N_ODD, 2 * D_MODEL), 0.02),
        'conv_dw_w': nrm(ks[12], (N_ODD, CONF_KERNEL, D_MODEL), CONF_KERNEL ** -0.5),
        'conv_dw_b': nrm(ks[13], (N_ODD, D_MODEL), 0.02),
        'conv_ln_g': 1.0 + nrm(ks[14], (N_ODD, D_MODEL), 0.02),
        'conv_ln_b': nrm(ks[15], (N_ODD, D_MODEL), 0.02),
        'conv_w_pw2': nrm(ks[16], (N_ODD, D_MODEL, D_MODEL), D_MODEL ** -0.5 * DN_BETA),
        'conv_b_pw2': nrm(ks[17], (N_ODD, D_MODEL), 0.02),
        'ffn_w_gate': nrm(ks[18], (DEPTH, D_MODEL, FFN_HIDDEN), D_MODEL ** -0.5),
        'ffn_w_up': nrm(ks[19], (DEPTH, D_MODEL, FFN_HIDDEN), D_MODEL ** -0.5),
        'ffn_w_down': nrm(ks[20], (DEPTH, FFN_HIDDEN, D_MODEL), FFN_HIDDEN ** -0.5 * DN_BETA),
        'ln_mix_g': 1.0 + nrm(ks[21], (DEPTH, D_MODEL), 0.02),
        'ln_mix_b': nrm(ks[22], (DEPTH, D_MODEL), 0.02),
        'ln_ffn_g': 1.0 + nrm(ks[23], (DEPTH, D_MODEL), 0.02),
        'ln_ffn_b': nrm(ks[24], (DEPTH, D_MODEL), 0.02),
    }
    return inp


def reference(x, mix_w_in, ssd_conv_w, ssd_conv_b, ssd_dt_bias, ssd_a_log, ssd_d, ssd_norm_w,
              mix_w_out, conv_w_pw1, conv_b_pw1, conv_dw_w, conv_dw_b, conv_ln_g, conv_ln_b,
              conv_w_pw2, conv_b_pw2, ffn_w_gate, ffn_w_up, ffn_w_down,
              ln_mix_g, ln_mix_b, ln_ffn_g, ln_ffn_b):
    for layer in range(DEPTH):
        j = layer // 2
        if layer % 2 == 0:
            mixed = moba_ssd_mixer(x, mix_w_in[j], ssd_conv_w[j], ssd_conv_b[j], ssd_dt_bias[j],
                                   ssd_a_log[j], ssd_d[j], ssd_norm_w[j], mix_w_out[j])
        else:
            mixed = conformer_conv_module(x, conv_w_pw1[j], conv_b_pw1[j], conv_dw_w[j], conv_dw_b[j],
                                          conv_ln_g[j], conv_ln_b[j], conv_w_pw2[j], conv_b_pw2[j])
        x = layer_norm(DN_ALPHA * x + mixed, ln_mix_g[layer], ln_mix_b[layer])
        x = layer_norm(DN_ALPHA * x + swiglu_ffn(x, ffn_w_gate[layer], ffn_w_up[layer], ffn_w_down[layer]),
                       ln_ffn_g[layer], ln_ffn_b[layer])
    return x
```

```python
from contextlib import ExitStack
import numpy as np
import concourse.bass as bass
import concourse.mybir as mybir
from concourse.bass_utils import run_bass_kernel_spmd

F32 = mybir.dt.float32
BF16 = mybir.dt.bfloat16
AF = mybir.ActivationFunctionType
ALU = mybir.AluOpType
AX = mybir.AxisListType

ENGS = ("pe", "act", "dve", "pool", "sp")

D = 1024
SEQ = 2048
NKT = 8
FFN = 2816
NFT = 22
IN_COLS = 3080
ALPHA = float(4 ** 0.25)
LN_EPS = 1e-5
RMS_EPS = 1e-5


import heapq


class _Op:
    __slots__ = ("eng", "fn", "reads", "writes", "dma", "deps", "sig", "sigval", "waits", "idx", "cost")


class Sched:
    def __init__(self):
        self.ops = []
        self.lastw = {}
        self.readers = {}
        self.barrier_at = []

    def add(self, eng, fn, reads=(), writes=(), dma=None, cost=300.0):
        o = _Op()
        o.eng = eng; o.fn = fn; o.reads = tuple(reads); o.writes = tuple(writes); o.dma = dma
        o.idx = len(self.ops); o.sig = False; o.sigval = None; o.waits = []; o.cost = cost
        deps = set()
        for k in o.reads:
            w = self.lastw.get(k)
            if w is not None:
                deps.add(w)
        for k in o.writes:
            w = self.lastw.get(k)
            if w is not None:
                deps.add(w)
            for r in self.readers.get(k, ()):
                deps.add(r)
        deps.discard(o.idx)
        o.deps = deps
        for k in o.reads:
            self.readers.setdefault(k, []).append(o.idx)
        for k in o.writes:
            self.lastw[k] = o.idx
            self.readers[k] = []
        self.ops.append(o)
        return o

    def barrier(self):
        self.barrier_at.append(len(self.ops))

    def reorder(self):
        ops = self.ops
        n = len(ops)
        bounds = sorted(set([0] + [b for b in self.barrier_at if 0 < b < n] + [n]))
        lastdma = {}
        sdeps = [set(o.deps) for o in ops]
        for o in ops:
            if o.dma is not None:
                p = lastdma.get(o.dma)
                if p is not None:
                    sdeps[o.idx].add(p)
                lastdma[o.dma] = o.idx
        for (f0, f1) in getattr(self, "fix_act_ranges", []):
            lastact = None
            for o in ops[f0:f1]:
                if o.eng == "act":
                    if lastact is not None:
                        sdeps[o.idx].add(lastact)
                    lastact = o.idx
        succ = [[] for _ in range(n)]
        for o in ops:
            for d in sdeps[o.idx]:
                succ[d].append(o.idx)
        finish = [0.0] * n
        new_order = []
        LAT = 250.0
        rsel = None
        for r in range(len(bounds) - 1):
            lo, hi = bounds[r], bounds[r + 1]
            if rsel is not None and r not in rsel:
                new_order.extend(range(lo, hi))
                for i in range(lo, hi):
                    finish[i] = 0.0
                continue
            t_eng = {}
            indeg = {}
            heap = []

            def est(i):
                o = ops[i]
                q = o.eng if o.dma is None else ("q", o.eng)
                t = t_eng.get(q, 0.0)
                for d in sdeps[i]:
                    if d >= lo:
                        if ops[d].dma is not None and d not in o.deps:
                            f = finish[d] - ops[d].cost + 60.0
                        else:
                            f = finish[d] + (LAT if ops[d].eng != o.eng or ops[d].dma is not None else 60.0)
                        if f > t:
                            t = f
                return t
            for i in range(lo, hi):
                k = sum(1 for d in sdeps[i] if d >= lo)
                indeg[i] = k
                if k == 0:
                    heapq.heappush(heap, (est(i), i))
            while heap:
                key, i = heapq.heappop(heap)
                e = est(i)
                if e > key + 1e-6:
                    heapq.heappush(heap, (e, i))
                    continue
                o = ops[i]
                q = o.eng if o.dma is None else ("q", o.eng)
                if o.dma is not None:
                    t_eng[q] = e + 60.0
                    t_eng[o.eng] = max(t_eng.get(o.eng, 0.0), e) + 60.0 if o.eng != "sp" else t_eng.get(o.eng, 0.0)
                    finish[i] = e + o.cost
                else:
                    t_eng[q] = e + o.cost
                    finish[i] = e + o.cost
                new_order.append(i)
                for sidx in succ[i]:
                    if lo <= sidx < hi:
                        indeg[sidx] -= 1
                        if indeg[sidx] == 0:
                            heapq.heappush(heap, (est(sidx), sidx))
            assert len(new_order) == hi, (len(new_order), hi)
        remap = {old: new for new, old in enumerate(new_order)}
        new_ops = [ops[i] for i in new_order]
        for o in new_ops:
            o.idx = remap[o.idx]
            o.deps = set(remap[d] for d in o.deps)
        self.ops = new_ops

    def finalize(self):
        if getattr(self, "do_reorder", True):
            self.reorder()
        ops = self.ops
        for b in self.barrier_at:
            last = {}
            for o in ops[:b]:
                if o.fn is None:
                    continue
                if o.dma is not None:
                    last[("dma", o.dma)] = o.idx
                else:
                    last[("eng", o.eng)] = o.idx
            first = {}
            for o in ops[b:]:
                if o.eng not in first:
                    first[o.eng] = o.idx
            for e, fi in first.items():
                for le in last.values():
                    ops[fi].deps.add(le)
        for o in ops:
            nd = set()
            for d in o.deps:
                p = ops[d]
                if p.dma is None and o.dma is None and p.eng == "pe" and o.eng == "pe":
                    continue
                nd.add(d)
            o.deps = nd
            for d in nd:
                ops[d].sig = True
        cnt = {}
        for o in ops:
            if o.dma is not None:
                key = ("dma", o.dma)
                cnt[key] = cnt.get(key, 0) + 16
                o.sigval = (key, cnt[key])
            elif o.sig:
                key = ("eng", o.eng)
                cnt[key] = cnt.get(key, 0) + 1
                o.sigval = (key, cnt[key])
        self.semkeys = list(cnt.keys())
        seen = {e: {} for e in ENGS}
        for o in ops:
            need = {}
            for d in o.deps:
                k, v = ops[d].sigval
                if v > need.get(k, 0):
                    need[k] = v
            s = seen[o.eng]
            for k, v in need.items():
                if s.get(k, 0) >= v:
                    continue
                s[k] = v
                o.waits.append((k, v))
        return cnt

    def emit(self, nc, stack):
        cnt = self.finalize()
        sems = {}
        for i, k in enumerate(self.semkeys):
            sems[k] = stack.enter_context(nc.semaphore("sm%d" % i))
        block = stack.enter_context(nc.Block())
        ops = self.ops

        def run(engname):
            def body(eng):
                for o in ops:
                    if o.eng != engname:
                        continue
                    for (k, v) in o.waits:
                        eng.wait_ge(sems[k], v)
                    if o.fn is None:
                        continue
                    ins = o.fn(eng)
                    if o.dma is not None:
                        ins.then_inc(sems[o.sigval[0]], 16)
                    elif o.sig:
                        ins.then_inc(sems[o.sigval[0]], 1)
            return body

        block.tensor(run("pe"))
        block.scalar(run("act"))
        block.vector(run("dve"))
        block.gpsimd(run("pool"))
        block.sync(run("sp"))
        return cnt


class Recorder:
    def __init__(self):
        self.segs = [[]]

    def add(self, eng, fn, reads=(), writes=(), dma=None, cost=300.0):
        self.segs[-1].append((eng, fn, tuple(reads), tuple(writes), dma, cost))

    def cut(self):
        if self.segs[-1]:
            self.segs.append([])


def merge_streams(real, recs):
    segs = [[sg for sg in r.segs if sg] for r in recs]
    tot = [max(1, sum(len(sg) for sg in ss)) for ss in segs]
    done = [0] * len(recs)
    idx = [0] * len(recs)
    while True:
        cand = [i for i in range(len(recs)) if idx[i] < len(segs[i])]
        if not cand:
            break
        i = min(cand, key=lambda i: done[i] / tot[i])
        sg = segs[i][idx[i]]
        for a in sg:
            real.add(*a)
        done[i] += len(sg)
        idx[i] += 1


def _col8(v):
    return np.ascontiguousarray(np.asarray(v, np.float32).reshape(-1, 128).T)


class PCols:
    def __init__(self):
        self.parts = []
        self.off = {}
        self.n = 0

    def put(self, name, arr):
        arr = np.asarray(arr, np.float32)
        assert arr.shape[0] == 128
        self.off[name] = (self.n, arr.shape[1])
        self.parts.append(arr)
        self.n += arr.shape[1]

    def array(self):
        return np.ascontiguousarray(np.concatenate(self.parts, axis=1))


def pack_params(inp):
    pc = PCols()
    for l in range(2):
        pc.put("ln_mix_g%d" % l, _col8(inp["ln_mix_g"][l]))
        pc.put("ln_mix_b%d" % l, _col8(inp["ln_mix_b"][l]))
        pc.put("ln_ffn_g%d" % l, _col8(inp["ln_ffn_g"][l]))
        pc.put("ln_ffn_b%d" % l, _col8(inp["ln_ffn_b"][l]))
    pc.put("b_pw1", _col8(inp["conv_b_pw1"][0]))
    dw = np.asarray(inp["conv_dw_w"][0], np.float32)
    pc.put("dw_w", np.concatenate([_col8(dw[k]) for k in range(31)], axis=1))
    pc.put("dw_b", _col8(inp["conv_dw_b"][0]))
    pc.put("cln_g", _col8(inp["conv_ln_g"][0]))
    pc.put("cln_b", _col8(inp["conv_ln_b"][0]))
    pc.put("b_pw2", _col8(inp["conv_b_pw2"][0]))
    cw = np.asarray(inp["ssd_conv_w"][0], np.float32)
    pc.put("sconv_w", np.concatenate([_col8(cw[k]) for k in range(4)], axis=1))
    pc.put("sconv_b", _col8(inp["ssd_conv_b"][0]))
    pc.put("snorm_w", _col8(inp["ssd_norm_w"][0]))
    pc.put("ssd_d", _col8(np.repeat(np.asarray(inp["ssd_d"][0], np.float32), 64)))
    pc.put("dt_bias", np.tile(np.asarray(inp["ssd_dt_bias"][0], np.float32)[None, :], (128, 1)))
    pc.put("a_log", np.tile(np.asarray(inp["ssd_a_log"][0], np.float32)[None, :], (128, 1)))
    return pc


class K:
    def __init__(self, nc, pc_off, npc, stages, dbg=None):
        self.nc = nc
        self.S = Sched()
        self.pc_off = pc_off
        self.npc = npc
        self.stages = stages
        self.stages_opt = set(stages) | ({'mix0_out'} if 'nomix0out' not in stages else set())
        self.dbg = dbg or {}
        self._psrr = 0
        self._psrr2 = 0

    @staticmethod
    def _fd(ap):
        n = 1
        for d in ap.shape[1:]:
            n *= d
        return n

    def _cost(self, eng, out):
        fd = self._fd(out)
        if eng == "act":
            return 200.0 + 0.85 * fd
        if eng == "dve":
            return 90.0 + 1.0 * fd
        if eng == "pool":
            return 300.0 + 2.6 * fd
        return 300.0

    def MM(self, out, lhsT, rhs, start, stop, r, w):
        self.S.add("pe", lambda e: e.matmul(out, lhsT, rhs, start=start, stop=stop), r, w, cost=max(64.0, self._fd(rhs)) / 2.4 + 10.0)

    def TR(self, out, in_, ident, r, w):
        self.S.add("pe", lambda e: e.transpose(out, in_, ident), r, w, cost=100.0)

    def ACT(self, out, in_, func, r, w, bias=None, scale=None):
        kw = {}
        if bias is not None:
            kw["bias"] = bias
        if scale is not None:
            kw["scale"] = scale
        self.S.add("act", lambda e: e.activation(out, in_, func, **kw), r, w, cost=self._cost("act", out))

    def TT(self, eng, out, in0, in1, op, r, w):
        self.S.add(eng, lambda e: e.tensor_tensor(out, in0, in1, op), r, w, cost=self._cost(eng, out))

    def TS(self, eng, out, in0, s1, s2, op0, op1, r, w):
        if s2 is None:
            self.S.add(eng, lambda e: e.tensor_scalar(out, in0, s1, None, op0), r, w, cost=self._cost(eng, out))
        else:
            self.S.add(eng, lambda e: e.tensor_scalar(out, in0, s1, s2, op0, op1), r, w, cost=self._cost(eng, out))

    def STT(self, eng, out, in0, scalar, in1, op0, op1, r, w):
        self.S.add(eng, lambda e: e.scalar_tensor_tensor(out, in0, scalar, in1, op0, op1), r, w, cost=self._cost(eng, out))

    def CP(self, eng, out, in_, r, w):
        if eng == "act":
            self.S.add("act", lambda e: e.copy(out, in_), r, w, cost=self._cost("act", out))
        else:
            self.S.add(eng, lambda e: e.tensor_copy(out, in_), r, w, cost=self._cost(eng, out))

    def OP(self, eng, meth, args, r, w, **kw):
        self.S.add(eng, lambda e: getattr(e, meth)(*args, **kw), r, w, cost=self._cost(eng, args[0]))

    def cut(self):
        if hasattr(self.S, "cut"):
            self.S.cut()

    def MEMSET(self, eng, ap, val, w):
        self.S.add(eng, lambda e: e.memset(ap, val), (), w, cost=self._cost(eng, ap) * 0.5)

    def DMA(self, eng, out, in_, r, w, key):
        nbytes = self._fd(out) * 128 * (2 if out.dtype == BF16 else 4)
        self.S.add(eng, lambda e: e.dma_start(out=out, in_=in_), r, w, dma=key, cost=2000.0 + nbytes / 180.0)

    def pcol(self, name, j=0, n=1, rows=slice(0, 128)):
        o, cnt = self.pc_off[name]
        return self.pc[rows, o + j:o + j + n]

    def carve(self, off_bytes, nbytes, dtype):
        assert off_bytes % 4 == 0 and nbytes % 4 == 0
        a = self.arena[:, off_bytes // 4:(off_bytes + nbytes) // 4]
        if dtype == BF16:
            a = a.bitcast(BF16)
        return a

    def build(self):
        nc = self.nc
        st = ExitStack()
        self.st = st
        dr = {}

        def din(name, shape):
            dr[name] = nc.dram_tensor(name, list(shape), F32, kind="ExternalInput").ap()

        din("xT", (D, SEQ))
        din("pcols", (128, self.npc))
        din("ctab", (128, 1024))
        din("wtab", (128, 16))
        din("w_in", (D, IN_COLS))
        din("w_out", (D, D))
        din("w_pw1", (D, 2 * D))
        din("w_pw2", (D, D))
        for l in range(2):
            din("w_gate%d" % l, (D, FFN))
            din("w_up%d" % l, (D, FFN))
            din("w_down%d" % l, (FFN, D))
        dr["outT"] = nc.dram_tensor("outT", [D, SEQ], F32, kind="ExternalOutput").ap()
        for name, shape in self.dbg.items():
            dr[name] = nc.dram_tensor(name, list(shape), BF16 if name.endswith("T") else F32, kind="ExternalOutput").ap()
        self.dr = dr

        OFF_X = 0
        OFF_XT = 65536
        OFF_W = 98304
        WSZ = 49152
        OFF_S = OFF_W + WSZ
        SSZ = 61440
        self.OFF_W, self.WSZ, self.OFF_S, self.SSZ = OFF_W, WSZ, OFF_S, SSZ
        ARENA = OFF_S + SSZ
        self.arena = st.enter_context(nc.sbuf_tensor("arena", [128, ARENA // 4], F32))
        self.pc = st.enter_context(nc.sbuf_tensor("pc", [128, self.npc], F32))
        self.ones_bf = st.enter_context(nc.sbuf_tensor("ones_bf", [128, 128], BF16))
        self.eps_col = st.enter_context(nc.sbuf_tensor("eps_col", [128, 2], F32))
        self.ident_bf = st.enter_context(nc.sbuf_tensor("ident_bf", [128, 128], BF16))
        self.ident_f = st.enter_context(nc.sbuf_tensor("ident_f", [128, 128], F32))
        self.psum = st.enter_context(nc.psum_tensor("psum", [128, 8, 512], F32))
        self.X = self.carve(OFF_X, 65536, F32).rearrange("p (k t) -> p k t", k=8)
        self.XT = self.carve(OFF_XT, 32768, BF16).rearrange("p (k t) -> p k t", k=8)

        S = self.S
        self.MEMSET("pool", self.ones_bf[:], 1.0, [("ones",)])
        self.MEMSET("pool", self.eps_col[:], LN_EPS, [("ones",)])
        self.MEMSET("pool", self.ident_f[:], 1.0, [("identf",)])
        S.add("pool", lambda e: e.affine_select(self.ident_f[:], self.ident_f[:], [[-1, 128]], ALU.is_equal, 0.0,
                                                base=0, channel_multiplier=1),
              [("identf",)], [("identf",)])
        self.CP("dve", self.ident_bf[:], self.ident_f[:], [("identf",)], [("ident",)])
        self.DMA("sp", self.pc[:], dr["pcols"], [], [("pc",)], "pc")
        if self.stages and self.stages[0] == "mix0":
            for kt in range(NKT):
                self.DMA("pool", self.XT[:, kt, :].rearrange("p (a b) -> p a b", a=2),
                         dr["xT"][kt * 128:(kt + 1) * 128, :].rearrange("p (a b) -> p a b", a=2), [],
                         [("XT", kt, c) for c in range(4)], "xtload%d" % kt)
        else:
            self.load_X()
            for kt in range(NKT):
                for c in range(4):
                    eng = "dve" if (kt + c) % 2 == 0 else "act"
                    self.CP(eng, self.XT[:, kt, c * 512:(c + 1) * 512], self.X[:, kt, c * 512:(c + 1) * 512],
                            [("X", kt, c)], [("XT", kt, c)])

        for stg in self.stages:
            if stg == "ffn0":
                self.ffn(0)
            elif stg == "ffn1":
                self.ffn(1)
            elif stg == "mix0":
                self.mixer0()
            elif stg == "mix1":
                self.mixer1()

        for kt in range(NKT):
            self.DMA("sp", dr["outT"][kt * 128:(kt + 1) * 128, :], self.X[:, kt, :],
                     [("X", kt, c) for c in range(4)], [("out", kt)], "outst")
        S.add("sp", None, [("out", kt) for kt in range(NKT)] + [("dbg", n) for n in self.dbg], [])
        cnt = S.emit(nc, st)
        st.close()
        return cnt

    def load_X(self):
        for kt in range(NKT):
            self.DMA("sp", self.X[:, kt, :], self.dr["xT"][kt * 128:(kt + 1) * 128, :], [], [("X", kt, c) for c in range(4)], "xload%d" % kt)

    def wslot(self, i, nsub, ncols):
        assert nsub * ncols * 2 <= 8192
        a = self.carve(self.OFF_W + i * 8192, nsub * ncols * 2, BF16)
        return a.rearrange("p (k c) -> p k c", k=nsub)

    def load_w_cols(self, dst, w_dram, c0, ncols, key, wkey, nk=8, k0=0):
        src = w_dram[k0 * 128:(k0 + nk) * 128, c0:c0 + ncols].rearrange("(k p) c -> p k c", p=128)
        self.DMA("pool", dst, src, [], [wkey], key)

    def ln_stats(self, srcs, keys):
        sc = self.ln_scr
        rb, sq, mean, rstd, mr = sc["rb"], sc["sq"], sc["mean"], sc["rstd"], sc["mr"]
        ps_s = self.psum[:, 6, :]
        ps_q = self.psum[:, 7, :]
        n = len(srcs)
        for kt in range(n):
            j = kt % 2
            self.CP("act", rb[:, j, :], srcs[kt], [keys[kt]], [("ln_rb", j)])
            self.ACT(sq[:, j, :], srcs[kt], AF.Square, [keys[kt]], [("ln_sq", j)])
            self.MM(ps_s, self.ones_bf[:], rb[:, j, :], kt == 0, kt == n - 1, [("ones",), ("ln_rb", j)], [("ps", 6)])
            self.MM(ps_q, self.ones_bf[:], sq[:, j, :], kt == 0, kt == n - 1, [("ones",), ("ln_sq", j)], [("ps", 7)])
        nf = 1.0 / (128 * n)
        self.ACT(mean[:], ps_s, AF.Identity, [("ps", 6)], [("ln_mean",)], scale=nf)
        self.TT("dve", mr[:], mean[:], mean[:], ALU.mult, [("ln_mean",)], [("ln_mr",)])
        self.STT("dve", rstd[:], ps_q, nf, mr[:], ALU.mult, ALU.subtract, [("ps", 7), ("ln_mr",)], [("ln_rstd",)])
        self.ACT(rstd[:], rstd[:], AF.Ln, [("ln_rstd",)], [("ln_rstd",)], bias=self.eps_col[:, 0:1])
        self.ACT(rstd[:], rstd[:], AF.Exp, [("ln_rstd",)], [("ln_rstd",)], scale=-0.5)
        self.TT("dve", mr[:], mean[:], rstd[:], ALU.mult, [("ln_mean",), ("ln_rstd",)], [("ln_mr",)])

    def ln_scratch(self, off):
        sc = {}
        o = off
        sc["rb"] = self.carve(o, 2048, BF16).rearrange("p (j t) -> p j t", j=2); o += 2048
        sc["sq"] = self.carve(o, 2048, BF16).rearrange("p (j t) -> p j t", j=2); o += 2048
        for nm in ("mean", "rstd", "mr"):
            sc[nm] = self.carve(o, 2048, F32); o += 2048
        self.ln_scr = sc
        return o

    def ln_chunk(self, c, gname, bname):
        tok = slice(c * 512, (c + 1) * 512)
        sc = self.ln_scr
        rstd, mr = sc["rstd"], sc["mr"]
        self.ln_stats([self.X[:, kt, tok] for kt in range(NKT)], [("X", kt, c) for kt in range(NKT)])
        for kt in range(NKT):
            xs = self.X[:, kt, tok]
            gcol = self.pcol(gname, kt)
            self.STT("dve", xs, xs, gcol, rstd[:], ALU.mult, ALU.mult, [("X", kt, c), ("ln_rstd",), ("pc",)], [("X", kt, c)])
            self.STT("dve", xs, mr[:], gcol, xs, ALU.mult, ALU.subtract, [("X", kt, c), ("ln_mr",), ("pc",)], [("X", kt, c)])
            self.ACT(self.XT[:, kt, tok], xs, AF.Identity, [("X", kt, c), ("pc",)], [("XT", kt, c)],
                     bias=self.pcol(bname, kt), scale=-1.0)
            self.ACT(xs, xs, AF.Identity, [("X", kt, c), ("pc",)], [("X", kt, c)],
                     bias=self.pcol(bname, kt), scale=-1.0)

    def ffn(self, l):
        S = self.S
        dr = self.dr
        S.barrier()
        OFF_S = self.OFF_S
        H = self.carve(OFF_S, 45056, BF16).rearrange("p (f t) -> p f t", f=NFT)
        o = OFF_S + 45056
        sg = self.carve(o, 2 * 2048, F32).rearrange("p (j t) -> p j t", j=2); o += 4096
        wd_all = self.carve(self.OFF_W, 45056, BF16).rearrange("p (f c) -> p f c", f=NFT)
        end_ln = self.ln_scratch(o)
        assert end_ln <= self.OFF_S + self.SSZ, end_ln
        groups = [(0, 4), (4, 4), (8, 4), (12, 4), (16, 4), (20, 2)]
        wg, wu, wdn = dr["w_gate%d" % l], dr["w_up%d" % l], dr["w_down%d" % l]
        for half in range(2):
            t0 = half * 1024
            for gi, (f0, nf) in enumerate(groups):
                sl = (gi % 2) * 2
                gsl = self.wslot(sl, 8, 512)
                usl = self.wslot(sl + 1, 8, 512)
                self.load_w_cols(gsl[:, :, 0:nf * 128], wg, f0 * 128, nf * 128, "wsl%d" % sl, ("wsl", sl))
                self.load_w_cols(usl[:, :, 0:nf * 128], wu, f0 * 128, nf * 128, "wsl%d" % (sl + 1), ("wsl", sl + 1))
                for fi in range(nf):
                    f = f0 + fi
                    for cc in range(2):
                        c = half * 2 + cc
                        tok = slice(c * 512, (c + 1) * 512)
                        bg = self._psrr % 2
                        bu = 2 + self._psrr % 2
                        self._psrr += 1
                        pg = self.psum[:, bg, :]
                        pu = self.psum[:, bu, :]
                        for kt in range(NKT):
                            self.MM(pg, gsl[:, kt, fi * 128:(fi + 1) * 128], self.XT[:, kt, tok], kt == 0, kt == NKT - 1,
                                    [("wsl", sl), ("XT", kt, c)], [("ps", bg)])
                        for kt in range(NKT):
                            self.MM(pu, usl[:, kt, fi * 128:(fi + 1) * 128], self.XT[:, kt, tok], kt == 0, kt == NKT - 1,
                                    [("wsl", sl + 1), ("XT", kt, c)], [("ps", bu)])
                        j = bg
                        self.ACT(sg[:, j, :], pg, AF.Silu, [("ps", bg)], [("sg", j)])
                        self.TT("dve", H[:, f, cc * 512:(cc + 1) * 512], sg[:, j, :], pu, ALU.mult,
                                [("sg", j), ("ps", bu)], [("H", f, cc)])
            for i, (f0, nf) in enumerate(groups):
                src = wdn[f0 * 128:(f0 + nf) * 128, :].rearrange("(f p) c -> p f c", p=128)
                self.DMA("pool", wd_all[:, f0:f0 + nf, :], src, [], [("wsl", i)], "wsl%d" % i)
            for cc in range(2):
                c = half * 2 + cc
                tok = slice(c * 512, (c + 1) * 512)
                for dt in range(NKT):
                    b = 4 + dt % 2
                    po = self.psum[:, b, :]
                    for f in range(NFT):
                        self.MM(po, wd_all[:, f, dt * 128:(dt + 1) * 128], H[:, f, cc * 512:(cc + 1) * 512],
                                f == 0, f == NFT - 1, [("wsl", f // 4), ("H", f, cc)], [("ps", b)])
                    self.STT("dve", self.X[:, dt, tok], self.X[:, dt, tok], ALPHA, po, ALU.mult, ALU.add,
                             [("X", dt, c), ("ps", b)], [("X", dt, c)])
                self.ln_chunk(c, "ln_ffn_g%d" % l, "ln_ffn_b%d" % l)
        S.barrier()

    def mixer0(self):
        S = self.S
        dr = self.dr
        S.barrier()
        o = self.OFF_W
        lim = self.OFF_S + self.SSZ
        self.catT = self.carve(o, 32768, BF16).rearrange("p (k t) -> p k t", k=8); o += 32768
        if "catT" in self.dbg:
            self.MEMSET("pool", self.catT[:], 0.0, [("catT", k, c) for k in range(8) for c in range(4)])
        real = self.S
        f0 = len(real.ops)
        ra = Recorder()
        self.S = ra
        self.attention(0, 65536)
        rs = Recorder()
        self.S = rs
        self.ssd(o, lim)
        self.S = real
        merge_streams(real, [ra, rs])
        if not hasattr(real, "fix_act_ranges"):
            real.fix_act_ranges = []
        real.fix_act_ranges.append((f0, len(real.ops)))
        S.barrier()
        if "catT" in self.dbg:
            self.DMA("sp", dr["catT"].rearrange("(k p) t -> p k t", p=128), self.catT[:], [("catT", k, c) for k in range(8) for c in range(4)],
                     [("dbg", "catT")], "dbgcat")
        self.load_X()
        if "mix0_out" in self.stages_opt:
            self.mix0_out(o, lim)
        S.barrier()

    def attention(self, o, lim):
        S = self.S
        dr = self.dr
        wqp = self.carve(o, 12288, BF16).rearrange("p (b k s c) -> p b k s c", b=2, k=8, s=3); o += 12288
        qT = self.carve(o, 8192, BF16).rearrange("p (b t) -> p b t", b=2); o += 8192
        kT = self.carve(o, 16384, BF16).rearrange("p (b h t) -> p b h t", b=2, h=2); o += 16384
        Vp = self.carve(o, 2 * 4608, BF16).rearrange("p (b t h e) -> p b t h e", b=2, t=16, h=2); o += 9216
        ctab = self.carve(o, 4096, F32).rearrange("p (q h n) -> p q h n", q=16, h=8); o += 4096
        wtab = self.carve(o, 64, F32).rearrange("p (a h) -> p a h", a=2); o += 64
        tri = self.carve(o, 256, BF16); o += 256
        E = self.carve(o, 3072, BF16).rearrange("p (b t) -> p b t", b=3); o += 3072
        Oacc = self.carve(o, 2 * 1040, F32).rearrange("p (b q e) -> p b q e", b=2, q=4); o += 2080
        ksum = self.carve(o, 64, F32).rearrange("p (b n) -> p b n", b=2); o += 64
        o = (o + 63) // 64 * 64
        kmean = self.carve(o, 128, BF16).rearrange("p (b h n) -> p b h n", b=2, h=2); o += 128
        gm = self.carve(o, 64, F32).rearrange("p (h n) -> p h n", h=2); o += 64
        top8 = self.carve(o, 64, F32).rearrange("p (h n) -> p h n", h=2); o += 64
        mc = self.carve(o, 2 * 1024, F32).rearrange("p (b q h n) -> p b q h n", b=2, q=16, h=2); o += 2048
        rcp = self.carve(o, 32, F32).rearrange("p (b q) -> p b q", b=2); o += 32
        atok = self.carve(o, 4096, BF16).rearrange("p (b q f) -> p b q f", b=4, q=4); o += 4096
        trif = self.carve(o, 512, F32); o += 512
        assert o <= lim, (o, lim)
        self.DMA("sp", ctab.rearrange("p q h n -> p (q h n)"), dr["ctab"], [], [("ctab",)], "ctab")
        self.DMA("sp", wtab.rearrange("p a h -> p (a h)"), dr["wtab"], [], [("wtab",)], "wtab")
        self.MEMSET("pool", trif[:], 1.0, [("trif",)])
        self.OP("pool", "affine_select", (trif[:], trif[:], [[1, 128]], ALU.is_ge, 0.0), [("trif",)], [("trif",)],
                base=0, channel_multiplier=-1)
        self.CP("pool", tri[:], trif[:], [("trif",)], [("tri",)])
        self.MEMSET("pool", kT[:], 0.0, [("kT", b, c) for b in range(2) for c in range(4)])
        self.MEMSET("pool", kmean[:], 0.0, [("kmean", 0), ("kmean", 1)])
        psb = self.psum.rearrange("p b t -> p (b t)").bitcast(BF16).rearrange("p (b t) -> p b t", b=8)

        w3 = dr["w_in"][:, 0:1536].rearrange("(k p) (s c) -> p k s c", p=128, s=3)
        for hp in range(4):
            pb = hp % 2
            for si in range(3):
                self.DMA("pool", wqp[:, pb, :, si, :], w3[:, :, si, hp * 128:(hp + 1) * 128], [], [("wqp", pb)], "wqp%d" % pb)
            self.cut()
            for c in range(4):
                tok = slice(c * 512, (c + 1) * 512)
                pq = self.psum[:, 0, :]
                pk = self.psum[:, 1, :]
                for kt in range(NKT):
                    self.MM(pq, wqp[:, pb, kt, 0, :], self.XT[:, kt, tok], kt == 0, kt == NKT - 1,
                            [("wqp", pb), ("XT", kt, c)], [("ps", 0)])
                for kt in range(NKT):
                    self.MM(pk, wqp[:, pb, kt, 1, :], self.XT[:, kt, tok], kt == 0, kt == NKT - 1,
                            [("wqp", pb), ("XT", kt, c)], [("ps", 1)])
                self.ACT(qT[:, pb, tok], pq, AF.Identity, [("ps", 0)], [("qT", pb, c)], scale=0.125)
                self.CP("dve", kT[0:64, pb, 0, tok], pk[0:64, :], [("ps", 1)], [("kT", pb, c)])
                self.CP("dve", kT[64:128, pb, 1, tok], pk[64:128, :], [("ps", 1)], [("kT", pb, c)])
                self.OP("dve", "tensor_reduce", (ksum[:, pb, 2 * c:2 * c + 2], pk.rearrange("p (n j) -> p n j", n=2), AX.X, ALU.add),
                        [("ps", 1)], [("ksum", pb, c)])
                self.cut()
            self.ACT(kmean[0:64, pb, 0, 0:8], ksum[0:64, pb, :], AF.Identity, [("ksum", pb, c) for c in range(4)], [("kmean", pb)], scale=1.0 / 256)
            self.ACT(kmean[64:128, pb, 1, 0:8], ksum[64:128, pb, :], AF.Identity, [("ksum", pb, c) for c in range(4)], [("kmean", pb)], scale=1.0 / 256)
            skip = ""
            for g4 in range(4):
                if "v" in skip:
                    break
                pv = self.psum[:, g4 % 2, :].rearrange("p (t f) -> p t f", t=4)
                for ti in range(4):
                    tt = g4 * 4 + ti
                    for kt in range(NKT):
                        self.MM(pv[:, ti, :], self.XT[:, kt, tt * 128:(tt + 1) * 128], wqp[:, pb, kt, 2, :],
                                kt == 0, kt == NKT - 1, [("wqp", pb), ("XT", kt, tt // 4)], [("ps", g4 % 2)])
                for ti in range(4):
                    tt = g4 * 4 + ti
                    for hh in range(2):
                        h = 2 * hp + hh
                        self.TS("dve" if hh == 0 else "pool" if False else "dve", Vp[:, pb, tt, hh, 0:64], pv[:, ti, hh * 64:(hh + 1) * 64],
                                wtab[:, tt % 2, h:h + 1], None, ALU.mult, None, [("ps", g4 % 2), ("wtab",)], [("Vp", pb)])
                self.cut()
            for hh in range(2):
                if "o" in skip:
                    break
                h = 2 * hp + hh
                for par in range(2):
                    self.CP("dve", Vp[:, pb, par:16:2, hh, 64], wtab[:, par, h:h + 1].to_broadcast([128, 8]), [("wtab",)], [("Vp", pb)])
            self.MEMSET("pool", gm[:], -1e30, [("gm",)])
            pgt_all = self.psum[:, 0, :].rearrange("p (q h n) -> p q h n", q=16, h=2)
            for qt in range(2, 16):
                if "m" in skip:
                    break
                self.MM(pgt_all[:, qt, :, :].rearrange("p h n -> p (h n)"), qT[:, pb, qt * 128:(qt + 1) * 128],
                        kmean[:, pb, :, :].rearrange("p h n -> p (h n)"),
                        True, True, [("qT", pb, qt // 4), ("kmean", pb)], [("ps", 0)])
            for qt in range(2, 16):
                if "c" in skip:
                    break
                own = qt // 2
                self.CP("dve", gm[:, :, 0:own], pgt_all[:, qt, :, 0:own], [("ps", 0)], [("gm",)])
                for hh in range(2):
                    h = 2 * hp + hh
                    self.OP("dve", "max", (top8[:, hh, :], gm[:, hh, :]), [("gm",)], [("top8", hh)])
                    self.STT("dve", mc[:, pb, qt, hh, 0:own], gm[:, hh, 0:own], top8[:, hh, 2:3], ctab[:, qt, h, 0:own],
                             ALU.is_ge, ALU.mult, [("gm",), ("top8", hh), ("ctab",)], [("mc", pb, qt)])
            self.cut()
            for hh in range(2):
                h = 2 * hp + hh
                slope = 2.0 ** (-(h + 1))
                hs_ = slice(hh * 64, (hh + 1) * 64)
                for qc in range(4):
                    ob = (hh * 4 + qc) % 2
                    self.MEMSET("pool", Oacc[:, ob], 0.0, [("Oacc", ob)])
                    units = [(t, 0) for t in range(4 * qc)] + [(4 * qc + a, a) for a in range(4)]
                    nu = len(units)
                    sb = {}

                    def emit_s(ui, units=units, sb=sb):
                        t, a = units[ui]
                        b = 1 + self._psrr % 2
                        eb = self._psrr % 3
                        self._psrr += 1
                        n = (4 - a) * 128
                        q0 = qc * 512 + a * 128
                        ps = self.psum[:, b, 0:n]
                        self.MM(ps, kT[:, pb, hh, t * 128:(t + 1) * 128], qT[:, pb, q0:q0 + n], True, True,
                                [("kT", pb, t // 4), ("qT", pb, qc)], [("ps", b)])
                        self.ACT(E[:, eb, 0:n], ps, AF.Exp, [("ps", b)], [("E", eb)])
                        if t >= 4 * qc:
                            self.TT("pool", E[:, eb, 0:128], E[:, eb, 0:128], tri[:], ALU.mult, [("E", eb), ("tri",)], [("E", eb)])
                        sb[ui] = eb

                    for ui in range(min(2, nu)):
                        emit_s(ui)
                    ui = 0
                    while ui < nu:
                        t, a = units[ui]
                        n_blk = t // 2
                        blk_units = [ui]
                        if ui + 1 < nu and units[ui + 1][0] // 2 == n_blk:
                            blk_units.append(ui + 1)
                        pob = 3 + (self._psrr2 % 2)
                        self._psrr2 += 1
                        po = self.psum[:, pob, 0:260].rearrange("p (q e) -> p q e", q=4)
                        for i in range(4):
                            qt = 4 * qc + i
                            srcu = [u for u in blk_units if units[u][1] <= i and units[u][0] <= qt]
                            if not srcu:
                                continue
                            for si, u in enumerate(srcu):
                                tu, au = units[u]
                                self.MM(po[:, i, :], E[:, sb[u], (i - au) * 128:(i - au + 1) * 128], Vp[:, pb, tu, hh, 0:65],
                                        si == 0, si == len(srcu) - 1, [("E", sb[u]), ("Vp", pb)], [("ps", pob)])
                        for i in range(4):
                            qt = 4 * qc + i
                            srcu = [u for u in blk_units if units[u][1] <= i and units[u][0] <= qt]
                            if not srcu:
                                continue
                            own = qt // 2
                            if n_blk == own:
                                cown = float(np.exp(slope * 128.0)) if qt % 2 == 0 else 1.0
                                self.STT("dve", Oacc[:, ob, i, :], po[:, i, :], cown, Oacc[:, ob, i, :], ALU.mult, ALU.add,
                                         [("ps", pob), ("Oacc", ob)], [("Oacc", ob)])
                            else:
                                self.STT("dve", Oacc[:, ob, i, :], po[:, i, :], mc[:, pb, qt, hh, n_blk:n_blk + 1], Oacc[:, ob, i, :],
                                         ALU.mult, ALU.add, [("ps", pob), ("Oacc", ob), ("mc", pb, qt)], [("Oacc", ob)])
                        for u in blk_units:
                            if u + 2 < nu:
                                emit_s(u + 2)
                        ui += len(blk_units)
                        self.cut()
                    self.OP("dve", "reciprocal", (rcp[:, ob, :], Oacc[:, ob, :, 64]), [("Oacc", ob)], [("rcp", ob)])
                    ab = qc
                    self.TT("dve", atok[:, ab, :, hs_], Oacc[:, ob, :, 0:64], rcp[:, ob, :].unsqueeze(2).to_broadcast([128, 4, 64]),
                            ALU.mult, [("Oacc", ob), ("rcp", ob)], [("atok", ab, hh)])
                    if hh == 1:
                        pt = psb[:, 0, 0:512].rearrange("p (q f) -> p q f", q=4)
                        for i in range(4):
                            self.TR(pt[:, i, :], atok[:, ab, i, :], self.ident_bf[:], [("atok", ab, 0), ("atok", ab, 1), ("ident",)], [("ps", 0)])
                        self.CP("act", self.catT[:, hp, qc * 512:(qc + 1) * 512], pt.rearrange("p q f -> p (q f)"), [("ps", 0)], [("catT", hp, qc)])
                    self.cut()

    def ssd(self, o, lim):
        S = self.S
        dr = self.dr
        L = 256

        def cv(nbytes, dt_):
            nonlocal o
            o = (o + 63) // 64 * 64
            a = self.carve(o, nbytes, dt_)
            o += nbytes
            return a
        wssd = cv(8 * 1544 * 2, BF16).rearrange("p (k c) -> p k c", k=8)
        xpad = cv(2 * 260 * 4, F32).rearrange("p (c t) -> p c t", c=2)
        halo = cv(8 * 4 * 4, F32).rearrange("p (c t) -> p c t", c=8)
        acc = cv(2 * 256 * 4, F32).rearrange("p (j t) -> p j t", j=2)
        zsil = cv(4 * 256 * 4, F32).rearrange("p (c t) -> p c t", c=4)
        xsT = cv(4 * 256 * 4, F32).rearrange("p (c t) -> p c t", c=4)
        xsb = cv(4 * 256 * 2, BF16).rearrange("p (c t) -> p c t", c=4)
        BT = cv(2 * 256 * 2, BF16).rearrange("p (g t) -> p g t", g=2)
        CT = cv(2 * 256 * 2, BF16).rearrange("p (g t) -> p g t", g=2)
        xtok = cv(2 * 512 * 2, BF16).rearrange("p (s f) -> p s f", s=2)
        xdtz = cv(2 * 8 * 128 * 2, BF16).rearrange("p (s h f) -> p s h f", s=2, h=8)
        xdtd = cv(2 * 512 * 2, BF16).rearrange("p (s f) -> p s f", s=2)
        Btok = cv(2 * 2 * 128 * 2, BF16).rearrange("p (s g n) -> p s g n", s=2, g=2)
        dtt = cv(2 * 8 * 4, F32).rearrange("p (s h) -> p s h", s=2)
        adt = cv(2 * 8 * 4, F32).rearrange("p (s h) -> p s h", s=2)
        adt_hi = cv(2 * 8 * 2, BF16).rearrange("p (s h) -> p s h", s=2)
        adt_lo = cv(2 * 8 * 2, BF16).rearrange("p (s h) -> p s h", s=2)
        adt_r = cv(2 * 8 * 4, F32).rearrange("p (s h) -> p s h", s=2)
        acs = cv(2 * 8 * 4, F32).rearrange("p (s h) -> p s h", s=2)
        dte = cv(2 * 8 * 4, F32).rearrange("p (s h) -> p s h", s=2)
        cdec = cv(8 * 4, F32)
        Aneg = cv(8 * 4, F32)
        tri_le = cv(128 * 2, BF16)
        maskT = cv(2 * 256 * 2, BF16).rearrange("p (s l) -> p s l", s=2)
        CBm = cv(2 * 2 * 256 * 2, BF16).rearrange("p (g s l) -> p g s l", g=2, s=2)
        seg2 = cv(2 * 2 * 256 * 4, F32).rearrange("p (j s l) -> p j s l", j=2, s=2)
        MT = cv(2 * 2 * 256 * 2, BF16).rearrange("p (j s l) -> p j s l", j=2, s=2)
        dfs2 = cv(2 * 256 * 4, F32).rearrange("p (j l) -> p j l", j=2)
        CD = cv(2 * 256 * 2, BF16).rearrange("p (j l) -> p j l", j=2)
        Abc = cv(2 * 2 * 2 * 128 * 2, BF16).rearrange("p (b j s m) -> p b j s m", b=2, j=2, s=2)
        maskf = seg2[:, 0]
        trif = dfs2[:, 0, 0:128]
        hin = cv(4 * 128 * 4, F32).rearrange("p (q f) -> p q f", q=4)
        hz = cv(8 * 128 * 2, BF16).rearrange("p (h f) -> p h f", h=8)
        yg = cv(4 * 256 * 4, F32).rearrange("p (c t) -> p c t", c=4)
        ysq = cv(1 * 256 * 2, BF16).rearrange("p (j t) -> p j t", j=1)
        rs = cv(256 * 4, F32)
        one_col = cv(4, F32)
        assert o <= lim, (o, lim)
        psb = self.psum.rearrange("p b t -> p (b t)").bitcast(BF16).rearrange("p (b t) -> p b t", b=8)

        for i, (c0, ncol) in enumerate([(0, 512), (512, 512), (1024, 512), (1536, 8)]):
            self.load_w_cols(wssd[:, :, c0:c0 + ncol], dr["w_in"], 1536 + c0, ncol, "wsl%d" % i, ("wssd", i))
        self.MEMSET("pool", one_col[:], 1.0, [("one_col",)])
        self.MEMSET("pool", trif, 1.0, [("dfs", 0)])
        self.OP("pool", "affine_select", (trif, trif, [[1, 128]], ALU.is_ge, 0.0), [("dfs", 0)], [("dfs", 0)],
                base=0, channel_multiplier=-1)
        self.CP("pool", tri_le[:], trif, [("dfs", 0)], [("tri_le",)])
        self.MEMSET("pool", maskf[:], 1.0, [("seg", 0)])
        for st in range(2):
            self.OP("pool", "affine_select", (maskf[:, st, :], maskf[:, st, :], [[1, 256]], ALU.is_ge, 0.0), [("seg", 0)], [("seg", 0)],
                    base=-128 * st, channel_multiplier=-1)
        self.CP("pool", maskT[:], maskf[:], [("seg", 0)], [("maskT",)])
        self.MEMSET("pool", halo[:], 0.0, [("halo", ct) for ct in range(8)])
        self.MEMSET("pool", xdtz[:], 0.0, [("xdtz",)])
        self.MEMSET("pool", hin[:], 0.0, [("hin",)])
        self.MEMSET("pool", hz[:], 0.0, [("hz",)])
        self.ACT(Aneg[:], self.pcol("a_log", 0, 8), AF.Exp, [("pc",)], [("Aneg",)])
        self.TS("dve", Aneg[:], Aneg[:], -1.0, None, ALU.mult, None, [("Aneg",)], [("Aneg",)])
        osw, _ = self.pc_off["sconv_w"]

        for c in range(8):
            t0 = c * L
            tok = slice(t0, t0 + L)
            cq = c // 2
            for zt in range(4):
                b = zt % 2
                pz = self.psum[:, 5, b * L:(b + 1) * L]
                for kt in range(NKT):
                    self.MM(pz, wssd[:, kt, zt * 128:(zt + 1) * 128], self.XT[:, kt, tok], kt == 0, kt == NKT - 1,
                            [("wssd", 0), ("XT", kt, cq)], [("ps", 5)])
                self.ACT(zsil[:, zt, :], pz, AF.Silu, [("ps", 5)], [("zsil", zt)])
                self.cut()
            for ct in range(8):
                b = ct % 2
                px = self.psum[:, 5, b * L:(b + 1) * L]
                for kt in range(NKT):
                    self.MM(px, wssd[:, kt, 512 + ct * 128:512 + (ct + 1) * 128], self.XT[:, kt, tok], kt == 0, kt == NKT - 1,
                            [("wssd", 1 + ct // 4), ("XT", kt, cq)], [("ps", 5)])
                j = ct % 2
                self.CP("pool", xpad[:, j, 0:3], halo[:, ct, 0:3], [("halo", ct)], [("xpad", j)])
                self.CP("act", xpad[:, j, 3:3 + L], px, [("ps", 5)], [("xpad", j)])
                self.CP("pool", halo[:, ct, 0:3], xpad[:, j, L:L + 3], [("xpad", j)], [("halo", ct)])
                ceng = "dve"
                self.TS(ceng, acc[:, j, :], xpad[:, j, 0:L], self.pc[:, osw + ct:osw + ct + 1], None, ALU.mult, None,
                        [("xpad", j), ("pc",)], [("acc", j)])
                for k in range(1, 4):
                    self.STT(ceng, acc[:, j, :], xpad[:, j, k:k + L], self.pc[:, osw + k * 8 + ct:osw + k * 8 + ct + 1], acc[:, j, :],
                             ALU.mult, ALU.add, [("xpad", j), ("acc", j), ("pc",)], [("acc", j)])
                if ct < 4:
                    self.ACT(xsT[:, ct, :], acc[:, j, :], AF.Silu, [("acc", j), ("pc",)], [("xsT", ct)], bias=self.pcol("sconv_b", ct))
                    self.ACT(xsb[:, ct, :], acc[:, j, :], AF.Silu, [("acc", j), ("pc",)], [("xsb", ct)], bias=self.pcol("sconv_b", ct))
                elif ct < 6:
                    self.ACT(BT[:, ct - 4, :], acc[:, j, :], AF.Silu, [("acc", j), ("pc",)], [("BT", ct - 4)], bias=self.pcol("sconv_b", ct))
                else:
                    self.ACT(CT[:, ct - 6, :], acc[:, j, :], AF.Silu, [("acc", j), ("pc",)], [("CT", ct - 6)], bias=self.pcol("sconv_b", ct))
            self.cut()
            P71 = [("ps", 5)]
            pdt = self.psum[:, 5, 256:272].rearrange("p (s h) -> p s h", s=2)
            for st in range(2):
                for kt in range(NKT):
                    self.MM(pdt[:, st, :], self.XT[:, kt, t0 + st * 128:t0 + (st + 1) * 128], wssd[:, kt, 1536:1544], kt == 0, kt == NKT - 1,
                            [("wssd", 3), ("XT", kt, cq)], P71)
            self.TT("dve", dtt[:], pdt, self.pcol("dt_bias", 0, 8).unsqueeze(1).to_broadcast([128, 2, 8]), ALU.add,
                    P71 + [("pc",)], [("dtt",)])
            self.ACT(dtt[:], dtt[:], AF.Exp, [("dtt",)], [("dtt",)])
            self.ACT(dtt[:], dtt[:], AF.Ln, [("dtt",), ("one_col",)], [("dtt",)], bias=one_col[:, 0:1])
            self.TT("dve", adt[:], dtt[:], Aneg[:].unsqueeze(1).to_broadcast([128, 2, 8]), ALU.mult, [("dtt",), ("Aneg",)], [("adt",)])
            self.CP("dve", adt_hi[:], adt[:], [("adt",)], [("adt_hi",)])
            self.TT("dve", adt_r[:], adt[:], adt_hi[:], ALU.subtract, [("adt",), ("adt_hi",)], [("adt_r",)])
            self.CP("dve", adt_lo[:], adt_r[:], [("adt_r",)], [("adt_lo",)])
            pacs = self.psum[:, 5, 272:288].rearrange("p (s h) -> p s h", s=2)
            ptot = self.psum[:, 5, 288:296]
            hl = [(adt_hi, ("adt_hi",)), (adt_lo, ("adt_lo",))]
            for i, (a_, k_) in enumerate(hl):
                self.MM(pacs[:, 0, :], tri_le[:], a_[:, 0, :], i == 0, i == 1, [("tri_le",), k_], P71)
            for i, (a_, k_) in enumerate(hl):
                self.MM(pacs[:, 1, :], self.ones_bf[:], a_[:, 0, :], i == 0, False, [("ones",), k_], P71)
            for i, (a_, k_) in enumerate(hl):
                self.MM(pacs[:, 1, :], tri_le[:], a_[:, 1, :], False, i == 1, [("tri_le",), k_], P71)
            n_ = 0
            for st in range(2):
                for i, (a_, k_) in enumerate(hl):
                    self.MM(ptot, self.ones_bf[:], a_[:, st, :], n_ == 0, n_ == 3, [("ones",), k_], P71)
                    n_ += 1
            self.CP("dve", acs[:], pacs, P71, [("acs",)])
            self.ACT(cdec[:], ptot, AF.Exp, P71, [("cdec",)])
            self.TT("dve", dte[:], ptot.unsqueeze(1).to_broadcast([128, 2, 8]), acs[:], ALU.subtract, P71 + [("acs",)], [("dte",)])
            self.ACT(dte[:], dte[:], AF.Exp, [("dte",)], [("dte",)])
            for st in range(2):
                self.cut()
                pt = psb[:, 5, 512:1024].rearrange("p (q f) -> p q f", q=4)
                for ct in range(4):
                    self.TR(pt[:, ct, :], xsb[:, ct, st * 128:(st + 1) * 128], self.ident_bf[:], [("xsb", ct), ("ident",)], P71)
                self.CP("act", xtok[:, st, :], pt.rearrange("p q f -> p (q f)"), P71, [("xtok", st)])
                pt2 = psb[:, 5, 512:768].rearrange("p (g n) -> p g n", g=2)
                for g in range(2):
                    self.TR(pt2[:, g, :], BT[:, g, st * 128:(st + 1) * 128], self.ident_bf[:], [("BT", g), ("ident",)], P71)
                self.CP("act", Btok[:, st, :, :].rearrange("p g n -> p (g n)"), pt2.rearrange("p g n -> p (g n)"), P71, [("Btok", st)])
                for h in range(8):
                    self.TS("pool", xdtz[:, st, h, (h % 2) * 64:(h % 2) * 64 + 64], xtok[:, st, h * 64:(h + 1) * 64], dtt[:, st, h:h + 1], None,
                            ALU.mult, None, [("xtok", st), ("dtt",)], [("xdtz",)])
                    self.ACT(xdtd[:, st, h * 64:(h + 1) * 64], xdtz[:, st, h, (h % 2) * 64:(h % 2) * 64 + 64], AF.Copy,
                             [("xdtz",), ("dte",)], [("xdtd", st)], scale=dte[:, st, h:h + 1])
            for g in range(2):
                for st in range(2):
                    pcb = self.psum[:, 5, 256:512]
                    self.MM(pcb, BT[:, g, st * 128:(st + 1) * 128], CT[:, g, :], True, True, [("BT", g), ("CT", g)], P71)
                    self.TT("dve", CBm[:, g, st, :], pcb, maskT[:, st, :], ALU.mult, P71 + [("maskT",)], [("CBm", g)])
            for pr in range(4):
                g = pr // 2
                self.cut()
                py = self.psum[:, 7, 0:L]
                for hh in range(2):
                    h = 2 * pr + hh
                    j = h % 2
                    seg = seg2[:, j]
                    dfs = dfs2[:, j]
                    for i, a_ in enumerate((adt_hi, adt_lo)):
                        for st in range(2):
                            self.CP("pool", Abc[:, j, i, st, :], a_[:, st, h:h + 1].to_broadcast([128, 128]),
                                    [("adt_hi",) if i == 0 else ("adt_lo",)], [("Abc", j)])
                    pbb = (6,)
                    pbc = self.psum[:, 6, 0:L]
                    self.MM(pbc[:, 0:128], Abc[:, j, 0, 0, :], tri_le[:], True, False, [("Abc", j), ("tri_le",)], [("ps",) + pbb])
                    self.MM(pbc[:, 0:128], Abc[:, j, 1, 0, :], tri_le[:], False, True, [("Abc", j), ("tri_le",)], [("ps",) + pbb])
                    self.MM(pbc[:, 128:256], Abc[:, j, 0, 0, :], self.ones_bf[:], True, False, [("Abc", j), ("ones",)], [("ps",) + pbb])
                    self.MM(pbc[:, 128:256], Abc[:, j, 1, 0, :], self.ones_bf[:], False, False, [("Abc", j), ("ones",)], [("ps",) + pbb])
                    self.MM(pbc[:, 128:256], Abc[:, j, 0, 1, :], tri_le[:], False, False, [("Abc", j), ("tri_le",)], [("ps",) + pbb])
                    self.MM(pbc[:, 128:256], Abc[:, j, 1, 1, :], tri_le[:], False, True, [("Abc", j), ("tri_le",)], [("ps",) + pbb])
                    for st in range(2):
                        self.TS("dve", seg[:, st, :], pbc, acs[:, st, h:h + 1], 0.0, ALU.subtract, ALU.min,
                                [("ps",) + pbb, ("acs",)], [("seg", j)])
                    self.ACT(seg, seg, AF.Exp, [("seg", j)], [("seg", j)])
                    self.TT("dve", MT[:, j], seg, CBm[:, g], ALU.mult, [("seg", j), ("CBm", g)], [("MT", j)])
                    self.ACT(dfs, pbc, AF.Exp, [("ps",) + pbb], [("dfs", j)])
                    self.TT("pool", CD[:, j, :], CT[:, g, :], dfs, ALU.mult, [("CT", g), ("dfs", j)], [("CD", j)])
                    for st in range(2):
                        self.MM(py, xdtz[:, st, h, :], MT[:, j, st, :], hh == 0 and st == 0, False, [("xdtz",), ("MT", j)], [("ps", 7)])
                    self.MM(py, hz[:, h, :], CD[:, j, :], False, hh == 1, [("hz",), ("CD", j)], [("ps", 7)])
                    self.cut()
                self.STT("dve", yg[:, pr, :], xsT[:, pr, :], self.pcol("ssd_d", pr), py, ALU.mult, ALU.add,
                         [("xsT", pr), ("ps", 7), ("pc",)], [("yg", pr)])
                self.TT("pool", yg[:, pr, :], yg[:, pr, :], zsil[:, pr, :], ALU.mult, [("yg", pr), ("zsil", pr)], [("yg", pr)])
            for pr in range(4):
                g = pr // 2
                self.cut()
                pst = self.psum[:, 5, 256:384]
                for st in range(2):
                    self.MM(pst, Btok[:, st, g, :], xdtd[:, st, pr * 128:(pr + 1) * 128], st == 0, st == 1,
                            [("Btok", st), ("xdtd", st)], P71)
                for hh in range(2):
                    h = 2 * pr + hh
                    cs = slice(hh * 64, hh * 64 + 64)
                    self.STT("dve", hin[:, pr, cs], hin[:, pr, cs], cdec[:, h:h + 1], pst[:, cs], ALU.mult, ALU.add,
                             [("hin",), ("cdec",)] + P71, [("hin",)])
                    self.CP("pool", hz[:, h, cs], hin[:, pr, cs], [("hin",)], [("hz",)])
            for g in range(2):
                self.cut()
                pss = self.psum[:, 5, 256:512]
                for i in range(2):
                    ct = 2 * g + i
                    self.ACT(ysq[:, 0, :], yg[:, ct, :], AF.Square, [("yg", ct)], [("ysq", 0)])
                    self.MM(pss, self.ones_bf[:], ysq[:, 0, :], i == 0, i == 1, [("ones",), ("ysq", 0)], P71)
                self.ACT(rs[:], pss, AF.Ln, P71, [("rs",)], bias=self.eps_col[:, 0:1], scale=1.0 / 256)
                self.ACT(rs[:], rs[:], AF.Exp, [("rs",)], [("rs",)], scale=-0.5)
                for i in range(2):
                    ct = 2 * g + i
                    self.TT("dve", yg[:, ct, :], yg[:, ct, :], rs[:], ALU.mult, [("yg", ct), ("rs",)], [("yg", ct)])
                    self.ACT(self.catT[:, 4 + ct, tok], yg[:, ct, :], AF.Identity, [("yg", ct), ("pc",)], [("catT", 4 + ct, cq)],
                             scale=self.pcol("snorm_w", ct))

    def mix0_out(self, o, lim):
        S = self.S
        dr = self.dr
        wo = self.carve(o, 16384, BF16).rearrange("p (k c) -> p k c", k=8); o += 16384
        o = self.ln_scratch(o)
        assert o <= lim
        for i in range(2):
            self.load_w_cols(wo[:, :, i * 512:(i + 1) * 512], dr["w_out"], i * 512, 512, "wsl%d" % i, ("wo", i))
        for c in range(4):
            tok = slice(c * 512, (c + 1) * 512)
            for dt in range(NKT):
                b = dt % 2
                po = self.psum[:, b, :]
                for kt in range(NKT):
                    self.MM(po, wo[:, kt, dt * 128:(dt + 1) * 128], self.catT[:, kt, tok], kt == 0, kt == NKT - 1,
                            [("wo", dt // 4), ("catT", kt, c)], [("ps", b)])
                self.STT("dve", self.X[:, dt, tok], self.X[:, dt, tok], ALPHA, po, ALU.mult, ALU.add,
                         [("X", dt, c), ("ps", b)], [("X", dt, c)])
            self.ln_chunk(c, "ln_mix_g0", "ln_mix_b0")

    def mixer1(self):
        S = self.S
        dr = self.dr
        S.barrier()
        pw1 = self.carve(self.OFF_W, 32768, BF16).rearrange("p (k c) -> p k c", k=8)
        pw2 = self.carve(self.OFF_W + 32768, 16384, BF16).rearrange("p (k c) -> p k c", k=8)
        for i in range(4):
            self.load_w_cols(pw1[:, :, i * 512:(i + 1) * 512], dr["w_pw1"], i * 512, 512, "wsl%d" % i, ("wsl", i))
        for i in range(2):
            self.load_w_cols(pw2[:, :, i * 512:(i + 1) * 512], dr["w_pw2"], i * 512, 512, "wsl%d" % (4 + i), ("wsl", 4 + i))
        o = self.OFF_S
        hp = self.carve(o, 8704, BF16)[:, 0:8 * 542].rearrange("p (c t) -> p c t", c=8); o += 8704
        diag2 = self.carve(o, 2 * 7936, BF16).rearrange("p (b k m) -> p b k m", b=2, k=31); o += 2 * 7936
        sig = self.carve(o, 2048, F32); o += 2048
        conv_o = self.carve(o, 16384, F32).rearrange("p (c t) -> p c t", c=8); o += 16384
        hs = self.carve(o, 8192, BF16).rearrange("p (c t) -> p c t", c=8); o += 8192
        o = self.ln_scratch(o)
        assert o <= self.OFF_S + self.SSZ, o - self.OFF_S
        sc = self.ln_scr
        for ct in range(8):
            self.MEMSET("pool", hp[:, ct, 0:30], 0.0, [("hp", ct)])
        ow, _ = self.pc_off["dw_w"]
        for c in range(4):
            tok = slice(c * 512, (c + 1) * 512)
            for dt in range(NKT):
                self.ACT(self.X[:, dt, tok], self.X[:, dt, tok], AF.Identity, [("X", dt, c), ("pc",)], [("X", dt, c)],
                         bias=self.pcol("b_pw2", dt), scale=ALPHA)
            for ct in range(8):
                pv = self.psum[:, 0 + ct % 2, :]
                pg = self.psum[:, 2 + ct % 2, :]
                pcv = self.psum[:, 4 + ct % 2, :]
                for kt in range(NKT):
                    self.MM(pv, pw1[:, kt, ct * 128:(ct + 1) * 128], self.XT[:, kt, tok], kt == 0, kt == NKT - 1,
                            [("wsl", ct // 4), ("XT", kt, c)], [("ps", ct % 2)])
                for kt in range(NKT):
                    self.MM(pg, pw1[:, kt, 1024 + ct * 128:1024 + (ct + 1) * 128], self.XT[:, kt, tok], kt == 0, kt == NKT - 1,
                            [("wsl", 2 + ct // 4), ("XT", kt, c)], [("ps", 2 + ct % 2)])
                self.ACT(sig[:], pg, AF.Sigmoid, [("ps", 2 + ct % 2), ("pc",)], [("sig",)], bias=self.pcol("b_pw1", 8 + ct))
                self.STT("dve", hp[:, ct, 30:542], pv, self.pcol("b_pw1", ct), sig[:], ALU.add, ALU.mult,
                         [("ps", ct % 2), ("sig",), ("pc",)], [("hp", ct)])
                wk = self.pc[:, ow + ct:ow + 248:8]
                diag = diag2[:, ct % 2]
                self.TT("pool", diag, self.ident_bf[:].unsqueeze(1).to_broadcast([128, 31, 128]),
                        wk.unsqueeze(2).to_broadcast([128, 31, 128]), ALU.mult, [("ident",), ("pc",)], [("diag", ct % 2)])
                for k in range(31):
                    self.MM(pcv, diag[:, k, :], hp[:, ct, k:k + 512], k == 0, k == 30,
                            [("diag", ct % 2), ("hp", ct)], [("ps", 4 + ct % 2)])
                self.ACT(conv_o[:, ct, :], pcv, AF.Identity, [("ps", 4 + ct % 2), ("pc",)], [("conv_o", ct)],
                         bias=self.pcol("dw_b", ct))
                self.CP("pool", hp[:, ct, 0:30], hp[:, ct, 512:542], [("hp", ct)], [("hp", ct)])
            self.ln_stats([conv_o[:, ct, :] for ct in range(8)], [("conv_o", ct) for ct in range(8)])
            for ct in range(8):
                xs = conv_o[:, ct, :]
                gcol = self.pcol("cln_g", ct)
                self.STT("dve", xs, xs, gcol, sc["rstd"][:], ALU.mult, ALU.mult, [("conv_o", ct), ("ln_rstd",), ("pc",)], [("conv_o", ct)])
                self.STT("dve", xs, sc["mr"][:], gcol, xs, ALU.mult, ALU.subtract, [("conv_o", ct), ("ln_mr",), ("pc",)], [("conv_o", ct)])
                self.ACT(hs[:, ct, :], xs, AF.Silu, [("conv_o", ct), ("pc",)], [("hs", ct)],
                         bias=self.pcol("cln_b", ct), scale=-1.0)
            for dt in range(NKT):
                b = dt % 2
                po = self.psum[:, b, :]
                for ct in range(8):
                    self.MM(po, pw2[:, ct, dt * 128:(dt + 1) * 128], hs[:, ct, :], ct == 0, ct == 7,
                            [("wsl", 4 + dt // 4), ("hs", ct)], [("ps", b)])
                self.TT("dve", self.X[:, dt, tok], self.X[:, dt, tok], po, ALU.add, [("X", dt, c), ("ps", b)], [("X", dt, c)])
            self.ln_chunk(c, "ln_mix_g1", "ln_mix_b1")
        S.barrier()

_STAGES = ["mix0", "ffn0", "mix1", "ffn1"]


def _prep_inputs(inp):
    pc = pack_params(inp)
    pcarr = pc.array()
    shared = {
        "pcols": pcarr,
        "w_in": np.ascontiguousarray(np.asarray(inp["mix_w_in"][0], np.float32)),
        "w_out": np.ascontiguousarray(np.asarray(inp["mix_w_out"][0], np.float32)),
        "w_pw1": np.ascontiguousarray(np.asarray(inp["conv_w_pw1"][0], np.float32)),
        "w_pw2": np.ascontiguousarray(np.asarray(inp["conv_w_pw2"][0], np.float32)),
    }
    for l in range(2):
        shared["w_gate%d" % l] = np.ascontiguousarray(np.asarray(inp["ffn_w_gate"][l], np.float32))
        shared["w_up%d" % l] = np.ascontiguousarray(np.asarray(inp["ffn_w_up"][l], np.float32))
        shared["w_down%d" % l] = np.ascontiguousarray(np.asarray(inp["ffn_w_down"][l], np.float32))
    slopes = 2.0 ** (-(np.arange(8, dtype=np.float64) + 1.0))
    qt = np.arange(16, dtype=np.float64)[:, None, None]
    nn = np.arange(8, dtype=np.float64)[None, None, :]
    ct = np.exp(np.minimum(-slopes[None, :, None] * 128.0 * (qt - 2 * nn - 1), 80.0))
    shared["ctab"] = np.ascontiguousarray(np.tile(ct.reshape(1, -1), (128, 1)).astype(np.float32))
    jrel = (np.arange(128, dtype=np.float64)[:, None, None] + 128.0 * np.arange(2)[None, :, None] - 128.0)
    shared["wtab"] = np.ascontiguousarray(np.exp(slopes[None, None, :] * jrel).reshape(128, 16).astype(np.float32))
    x = np.asarray(inp["x"], np.float32)
    in_maps = []
    for b in range(8):
        m = dict(shared)
        m["xT"] = np.ascontiguousarray(x[b].T)
        in_maps.append(m)
    return pc, in_maps


def run(inp, stages=_STAGES, dbg=None, trace=False):
    pc, in_maps = _prep_inputs(inp)
    nc = bass.Bass("TRN2", target_bir_lowering=False)
    k = K(nc, pc.off, pc.n, stages, dbg)
    k.build()
    res = run_bass_kernel_spmd(nc, in_maps, core_ids=list(range(8)), trace=trace)
    return res


def kernel(**inputs):
    res = run(inputs)
    out = np.stack([np.ascontiguousarray(r["outT"].T) for r in res.results], axis=0)
    return out.astype(np.float32)
```

```python
from contextlib import ExitStack
import numpy as np
import concourse.bass as bass
import concourse.mybir as mybir
from concourse.bass_utils import run_bass_kernel_spmd

F32 = mybir.dt.float32
BF16 = mybir.dt.bfloat16
AF = mybir.ActivationFunctionType
ALU = mybir.AluOpType
AX = mybir.AxisListType

ENGS = ("pe", "act", "dve", "pool", "sp")

D = 1024
SEQ = 2048
NKT = 8
FFN = 2816
NFT = 22
IN_COLS = 3080
ALPHA = float(4 ** 0.25)
LN_EPS = 1e-5
RMS_EPS = 1e-5


import heapq


class _Op:
    __slots__ = ("eng", "fn", "reads", "writes", "dma", "deps", "sig", "sigval", "waits", "idx", "cost")


class Sched:
    def __init__(self):
        self.ops = []
        self.lastw = {}
        self.readers = {}
        self.barrier_at = []

    def add(self, eng, fn, reads=(), writes=(), dma=None, cost=300.0):
        o = _Op()
        o.eng = eng; o.fn = fn; o.reads = tuple(reads); o.writes = tuple(writes); o.dma = dma
        o.idx = len(self.ops); o.sig = False; o.sigval = None; o.waits = []; o.cost = cost
        deps = set()
        for k in o.reads:
            w = self.lastw.get(k)
            if w is not None:
                deps.add(w)
        for k in o.writes:
            w = self.lastw.get(k)
            if w is not None:
                deps.add(w)
            for r in self.readers.get(k, ()):
                deps.add(r)
        deps.discard(o.idx)
        o.deps = deps
        for k in o.reads:
            self.readers.setdefault(k, []).append(o.idx)
        for k in o.writes:
            self.lastw[k] = o.idx
            self.readers[k] = []
        self.ops.append(o)
        return o

    def barrier(self):
        self.barrier_at.append(len(self.ops))

    def reorder(self):
        ops = self.ops
        n = len(ops)
        bounds = sorted(set([0] + [b for b in self.barrier_at if 0 < b < n] + [n]))
        lastdma = {}
        sdeps = [set(o.deps) for o in ops]
        for o in ops:
            if o.dma is not None:
                p = lastdma.get(o.dma)
                if p is not None:
                    sdeps[o.idx].add(p)
                lastdma[o.dma] = o.idx
        for (f0, f1) in getattr(self, "fix_act_ranges", []):
            lastact = None
            for o in ops[f0:f1]:
                if o.eng == "act":
                    if lastact is not None:
                        sdeps[o.idx].add(lastact)
                    lastact = o.idx
        succ = [[] for _ in range(n)]
        for o in ops:
            for d in sdeps[o.idx]:
                succ[d].append(o.idx)
        finish = [0.0] * n
        new_order = []
        LAT = 250.0
        rsel = None
        for r in range(len(bounds) - 1):
            lo, hi = bounds[r], bounds[r + 1]
            if rsel is not None and r not in rsel:
                new_order.extend(range(lo, hi))
                for i in range(lo, hi):
                    finish[i] = 0.0
                continue
            t_eng = {}
            indeg = {}
            heap = []

            def est(i):
                o = ops[i]
                q = o.eng if o.dma is None else ("q", o.eng)
                t = t_eng.get(q, 0.0)
                for d in sdeps[i]:
                    if d >= lo:
                        if ops[d].dma is not None and d not in o.deps:
                            f = finish[d] - ops[d].cost + 60.0
                        else:
                            f = finish[d] + (LAT if ops[d].eng != o.eng or ops[d].dma is not None else 60.0)
                        if f > t:
                            t = f
                return t
            for i in range(lo, hi):
                k = sum(1 for d in sdeps[i] if d >= lo)
                indeg[i] = k
                if k == 0:
                    heapq.heappush(heap, (est(i), i))
            while heap:
                key, i = heapq.heappop(heap)
                e = est(i)
                if e > key + 1e-6:
                    heapq.heappush(heap, (e, i))
                    continue
                o = ops[i]
                q = o.eng if o.dma is None else ("q", o.eng)
                if o.dma is not None:
                    t_eng[q] = e + 60.0
                    t_eng[o.eng] = max(t_eng.get(o.eng, 0.0), e) + 60.0 if o.eng != "sp" else t_eng.get(o.eng, 0.0)
                    finish[i] = e + o.cost
                else:
                    t_eng[q] = e + o.cost
                    finish[i] = e + o.cost
                new_order.append(i)
                for sidx in succ[i]:
                    if lo <= sidx < hi:
                        indeg[sidx] -= 1
                        if indeg[sidx] == 0:
                            heapq.heappush(heap, (est(sidx), sidx))
            assert len(new_order) == hi, (len(new_order), hi)
        remap = {old: new for new, old in enumerate(new_order)}
        new_ops = [ops[i] for i in new_order]
        for o in new_ops:
            o.idx = remap[o.idx]
            o.deps = set(remap[d] for d in o.deps)
        self.ops = new_ops

    def finalize(self):
        if getattr(self, "do_reorder", True):
            self.reorder()
        ops = self.ops
        for b in self.barrier_at:
            last = {}
            for o in ops[:b]:
                if o.fn is None:
                    continue
                if o.dma is not None:
                    last[("dma", o.dma)] = o.idx
                else:
                    last[("eng", o.eng)] = o.idx
            first = {}
            for o in ops[b:]:
                if o.eng not in first:
                    first[o.eng] = o.idx
            for e, fi in first.items():
                for le in last.values():
                    ops[fi].deps.add(le)
        for o in ops:
            nd = set()
            for d in o.deps:
                p = ops[d]
                if p.dma is None and o.dma is None and p.eng == "pe" and o.eng == "pe":
                    continue
                nd.add(d)
            o.deps = nd
            for d in nd:
                ops[d].sig = True
        cnt = {}
        for o in ops:
            if o.dma is not None:
                key = ("dma", o.dma)
                cnt[key] = cnt.get(key, 0) + 16
                o.sigval = (key, cnt[key])
            elif o.sig:
                key = ("eng", o.eng)
                cnt[key] = cnt.get(key, 0) + 1
                o.sigval = (key, cnt[key])
        self.semkeys = list(cnt.keys())
        seen = {e: {} for e in ENGS}
        for o in ops:
            need = {}
            for d in o.deps:
                k, v = ops[d].sigval
                if v > need.get(k, 0):
                    need[k] = v
            s = seen[o.eng]
            for k, v in need.items():
                if s.get(k, 0) >= v:
                    continue
                s[k] = v
                o.waits.append((k, v))
        return cnt

    def emit(self, nc, stack):
        cnt = self.finalize()
        sems = {}
        for i, k in enumerate(self.semkeys):
            sems[k] = stack.enter_context(nc.semaphore("sm%d" % i))
        block = stack.enter_context(nc.Block())
        ops = self.ops

        def run(engname):
            def body(eng):
                for o in ops:
                    if o.eng != engname:
                        continue
                    for (k, v) in o.waits:
                        eng.wait_ge(sems[k], v)
                    if o.fn is None:
                        continue
                    ins = o.fn(eng)
                    if o.dma is not None:
                        ins.then_inc(sems[o.sigval[0]], 16)
                    elif o.sig:
                        ins.then_inc(sems[o.sigval[0]], 1)
            return body

        block.tensor(run("pe"))
        block.scalar(run("act"))
        block.vector(run("dve"))
        block.gpsimd(run("pool"))
        block.sync(run("sp"))
        return cnt


class Recorder:
    def __init__(self):
        self.segs = [[]]

    def add(self, eng, fn, reads=(), writes=(), dma=None, cost=300.0):
        self.segs[-1].append((eng, fn, tuple(reads), tuple(writes), dma, cost))

    def cut(self):
        if self.segs[-1]:
            self.segs.append([])


def merge_streams(real, recs):
    segs = [[sg for sg in r.segs if sg] for r in recs]
    tot = [max(1, sum(len(sg) for sg in ss)) for ss in segs]
    done = [0] * len(recs)
    idx = [0] * len(recs)
    while True:
        cand = [i for i in range(len(recs)) if idx[i] < len(segs[i])]
        if not cand:
            break
        i = min(cand, key=lambda i: done[i] / tot[i])
        sg = segs[i][idx[i]]
        for a in sg:
            real.add(*a)
        done[i] += len(sg)
        idx[i] += 1


def _col8(v):
    return np.ascontiguousarray(np.asarray(v, np.float32).reshape(-1, 128).T)


class PCols:
    def __init__(self):
        self.parts = []
        self.off = {}
        self.n = 0

    def put(self, name, arr):
        arr = np.asarray(arr, np.float32)
        assert arr.shape[0] == 128
        self.off[name] = (self.n, arr.shape[1])
        self.parts.append(arr)
        self.n += arr.shape[1]

    def array(self):
        return np.ascontiguousarray(np.concatenate(self.parts, axis=1))


def pack_params(inp):
    pc = PCols()
    for l in range(2):
        pc.put("ln_mix_g%d" % l, _col8(inp["ln_mix_g"][l]))
        pc.put("ln_mix_b%d" % l, _col8(inp["ln_mix_b"][l]))
        pc.put("ln_ffn_g%d" % l, _col8(inp["ln_ffn_g"][l]))
        pc.put("ln_ffn_b%d" % l, _col8(inp["ln_ffn_b"][l]))
    pc.put("b_pw1", _col8(inp["conv_b_pw1"][0]))
    dw = np.asarray(inp["conv_dw_w"][0], np.float32)
    pc.put("dw_w", np.concatenate([_col8(dw[k]) for k in range(31)], axis=1))
    pc.put("dw_b", _col8(inp["conv_dw_b"][0]))
    pc.put("cln_g", _col8(inp["conv_ln_g"][0]))
    pc.put("cln_b", _col8(inp["conv_ln_b"][0]))
    pc.put("b_pw2", _col8(inp["conv_b_pw2"][0]))
    cw = np.asarray(inp["ssd_conv_w"][0], np.float32)
    pc.put("sconv_w", np.concatenate([_col8(cw[k]) for k in range(4)], axis=1))
    pc.put("sconv_b", _col8(inp["ssd_conv_b"][0]))
    pc.put("snorm_w", _col8(inp["ssd_norm_w"][0]))
    pc.put("ssd_d", _col8(np.repeat(np.asarray(inp["ssd_d"][0], np.float32), 64)))
    pc.put("dt_bias", np.tile(np.asarray(inp["ssd_dt_bias"][0], np.float32)[None, :], (128, 1)))
    pc.put("a_log", np.tile(np.asarray(inp["ssd_a_log"][0], np.float32)[None, :], (128, 1)))
    return pc


class K:
    def __init__(self, nc, pc_off, npc, stages, dbg=None):
        self.nc = nc
        self.S = Sched()
        self.pc_off = pc_off
        self.npc = npc
        self.stages = stages
        self.stages_opt = set(stages) | ({'mix0_out'} if 'nomix0out' not in stages else set())
        self.dbg = dbg or {}
        self._psrr = 0
        self._psrr2 = 0

    @staticmethod
    def _fd(ap):
        n = 1
        for d in ap.shape[1:]:
            n *= d
        return n

    def _cost(self, eng, out):
        fd = self._fd(out)
        if eng == "act":
            return 200.0 + 0.85 * fd
        if eng == "dve":
            return 90.0 + 1.0 * fd
        if eng == "pool":
            return 300.0 + 2.6 * fd
        return 300.0

    def MM(self, out, lhsT, rhs, start, stop, r, w):
        self.S.add("pe", lambda e: e.matmul(out, lhsT, rhs, start=start, stop=stop), r, w, cost=max(64.0, self._fd(rhs)) / 2.4 + 10.0)

    def TR(self, out, in_, ident, r, w):
        self.S.add("pe", lambda e: e.transpose(out, in_, ident), r, w, cost=100.0)

    def ACT(self, out, in_, func, r, w, bias=None, scale=None):
        kw = {}
        if bias is not None:
            kw["bias"] = bias
        if scale is not None:
            kw["scale"] = scale
        self.S.add("act", lambda e: e.activation(out, in_, func, **kw), r, w, cost=self._cost("act", out))

    def TT(self, eng, out, in0, in1, op, r, w):
        self.S.add(eng, lambda e: e.tensor_tensor(out, in0, in1, op), r, w, cost=self._cost(eng, out))

    def TS(self, eng, out, in0, s1, s2, op0, op1, r, w):
        if s2 is None:
            self.S.add(eng, lambda e: e.tensor_scalar(out, in0, s1, None, op0), r, w, cost=self._cost(eng, out))
        else:
            self.S.add(eng, lambda e: e.tensor_scalar(out, in0, s1, s2, op0, op1), r, w, cost=self._cost(eng, out))

    def STT(self, eng, out, in0, scalar, in1, op0, op1, r, w):
        self.S.add(eng, lambda e: e.scalar_tensor_tensor(out, in0, scalar, in1, op0, op1), r, w, cost=self._cost(eng, out))

    def CP(self, eng, out, in_, r, w):
        if eng == "act":
            self.S.add("act", lambda e: e.copy(out, in_), r, w, cost=self._cost("act", out))
        else:
            self.S.add(eng, lambda e: e.tensor_copy(out, in_), r, w, cost=self._cost(eng, out))

    def OP(self, eng, meth, args, r, w, **kw):
        self.S.add(eng, lambda e: getattr(e, meth)(*args, **kw), r, w, cost=self._cost(eng, args[0]))

    def cut(self):
        if hasattr(self.S, "cut"):
            self.S.cut()

    def MEMSET(self, eng, ap, val, w):
        self.S.add(eng, lambda e: e.memset(ap, val), (), w, cost=self._cost(eng, ap) * 0.5)

    def DMA(self, eng, out, in_, r, w, key):
        nbytes = self._fd(out) * 128 * (2 if out.dtype == BF16 else 4)
        self.S.add(eng, lambda e: e.dma_start(out=out, in_=in_), r, w, dma=key, cost=2000.0 + nbytes / 180.0)

    def pcol(self, name, j=0, n=1, rows=slice(0, 128)):
        o, cnt = self.pc_off[name]
        return self.pc[rows, o + j:o + j + n]

    def carve(self, off_bytes, nbytes, dtype):
        assert off_bytes % 4 == 0 and nbytes % 4 == 0
        a = self.arena[:, off_bytes // 4:(off_bytes + nbytes) // 4]
        if dtype == BF16:
            a = a.bitcast(BF16)
        return a

    def build(self):
        nc = self.nc
        st = ExitStack()
        self.st = st
        dr = {}

        def din(name, shape):
            dr[name] = nc.dram_tensor(name, list(shape), F32, kind="ExternalInput").ap()

        din("xT", (D, SEQ))
        din("pcols", (128, self.npc))
        din("ctab", (128, 1024))
        din("wtab", (128, 16))
        din("w_in", (D, IN_COLS))
        din("w_out", (D, D))
        din("w_pw1", (D, 2 * D))
        din("w_pw2", (D, D))
        for l in range(2):
            din("w_gate%d" % l, (D, FFN))
            din("w_up%d" % l, (D, FFN))
            din("w_down%d" % l, (FFN, D))
        dr["outT"] = nc.dram_tensor("outT", [D, SEQ], F32, kind="ExternalOutput").ap()
        for name, shape in self.dbg.items():
            dr[name] = nc.dram_tensor(name, list(shape), BF16 if name.endswith("T") else F32, kind="ExternalOutput").ap()
        self.dr = dr

        OFF_X = 0
        OFF_XT = 65536
        OFF_W = 98304
        WSZ = 49152
        OFF_S = OFF_W + WSZ
        SSZ = 61440
        self.OFF_W, self.WSZ, self.OFF_S, self.SSZ = OFF_W, WSZ, OFF_S, SSZ
        ARENA = OFF_S + SSZ
        self.arena = st.enter_context(nc.sbuf_tensor("arena", [128, ARENA // 4], F32))
        self.pc = st.enter_context(nc.sbuf_tensor("pc", [128, self.npc], F32))
        self.ones_bf = st.enter_context(nc.sbuf_tensor("ones_bf", [128, 128], BF16))
        self.eps_col = st.enter_context(nc.sbuf_tensor("eps_col", [128, 2], F32))
        self.ident_bf = st.enter_context(nc.sbuf_tensor("ident_bf", [128, 128], BF16))
        self.ident_f = st.enter_context(nc.sbuf_tensor("ident_f", [128, 128], F32))
        self.psum = st.enter_context(nc.psum_tensor("psum", [128, 8, 512], F32))
        self.X = self.carve(OFF_X, 65536, F32).rearrange("p (k t) -> p k t", k=8)
        self.XT = self.carve(OFF_XT, 32768, BF16).rearrange("p (k t) -> p k t", k=8)

        S = self.S
        self.MEMSET("pool", self.ones_bf[:], 1.0, [("ones",)])
        self.MEMSET("pool", self.eps_col[:], LN_EPS, [("ones",)])
        self.MEMSET("pool", self.ident_f[:], 1.0, [("identf",)])
        S.add("pool", lambda e: e.affine_select(self.ident_f[:], self.ident_f[:], [[-1, 128]], ALU.is_equal, 0.0,
                                                base=0, channel_multiplier=1),
              [("identf",)], [("identf",)])
        self.CP("dve", self.ident_bf[:], self.ident_f[:], [("identf",)], [("ident",)])
        self.DMA("sp", self.pc[:], dr["pcols"], [], [("pc",)], "pc")
        if self.stages and self.stages[0] == "mix0":
            xsrc = dr["xT"].rearrange("(k p) t -> p k t", p=128)
            for c in range(4):
                for hk in range(2):
                    self.DMA("pool", self.XT[:, hk * 4:(hk + 1) * 4, c * 512:(c + 1) * 512],
                             xsrc[:, hk * 4:(hk + 1) * 4, c * 512:(c + 1) * 512], [],
                             [("XT", kt, c) for kt in range(NKT)], "xtload%d" % c)
        else:
            self.load_X()
            for kt in range(NKT):
                for c in range(4):
                    eng = "dve" if (kt + c) % 2 == 0 else "act"
                    self.CP(eng, self.XT[:, kt, c * 512:(c + 1) * 512], self.X[:, kt, c * 512:(c + 1) * 512],
                            [("X", kt, c)], [("XT", kt, c)])

        for stg in self.stages:
            if stg == "ffn0":
                self.ffn(0)
            elif stg == "ffn1":
                self.ffn(1)
            elif stg == "mix0":
                self.mixer0()
            elif stg == "mix1":
                self.mixer1()

        for kt in range(NKT):
            self.DMA("sp", dr["outT"][kt * 128:(kt + 1) * 128, :], self.X[:, kt, :],
                     [("X", kt, c) for c in range(4)], [("out", kt)], "outst")
        S.add("sp", None, [("out", kt) for kt in range(NKT)] + [("dbg", n) for n in self.dbg], [])
        cnt = S.emit(nc, st)
        st.close()
        return cnt

    def load_X(self):
        xsrc = self.dr["xT"].rearrange("(k p) t -> p k t", p=128)
        for c in range(4):
            for hk in range(2):
                self.DMA("sp", self.X[:, hk * 4:(hk + 1) * 4, c * 512:(c + 1) * 512],
                         xsrc[:, hk * 4:(hk + 1) * 4, c * 512:(c + 1) * 512], [],
                         [("X", kt, c) for kt in range(NKT)], "xload%d" % c)

    def wslot(self, i, nsub, ncols):
        assert nsub * ncols * 2 <= 8192
        a = self.carve(self.OFF_W + i * 8192, nsub * ncols * 2, BF16)
        return a.rearrange("p (k c) -> p k c", k=nsub)

    def load_w_cols(self, dst, w_dram, c0, ncols, key, wkey, nk=8, k0=0):
        src = w_dram[k0 * 128:(k0 + nk) * 128, c0:c0 + ncols].rearrange("(k p) c -> p k c", p=128)
        self.DMA("pool", dst, src, [], [wkey], key)

    def ln_stats(self, srcs, keys):
        sc = self.ln_scr
        rb, sq, mean, rstd, mr = sc["rb"], sc["sq"], sc["mean"], sc["rstd"], sc["mr"]
        ps_s = self.psum[:, 6, :]
        ps_q = self.psum[:, 7, :]
        n = len(srcs)
        for kt in range(n):
            j = kt % 2
            self.CP("act", rb[:, j, :], srcs[kt], [keys[kt]], [("ln_rb", j)])
            self.ACT(sq[:, j, :], srcs[kt], AF.Square, [keys[kt]], [("ln_sq", j)])
            self.MM(ps_s, self.ones_bf[:], rb[:, j, :], kt == 0, kt == n - 1, [("ones",), ("ln_rb", j)], [("ps", 6)])
            self.MM(ps_q, self.ones_bf[:], sq[:, j, :], kt == 0, kt == n - 1, [("ones",), ("ln_sq", j)], [("ps", 7)])
        nf = 1.0 / (128 * n)
        self.ACT(mean[:], ps_s, AF.Identity, [("ps", 6)], [("ln_mean",)], scale=nf)
        self.TT("dve", mr[:], mean[:], mean[:], ALU.mult, [("ln_mean",)], [("ln_mr",)])
        self.STT("dve", rstd[:], ps_q, nf, mr[:], ALU.mult, ALU.subtract, [("ps", 7), ("ln_mr",)], [("ln_rstd",)])
        self.ACT(rstd[:], rstd[:], AF.Ln, [("ln_rstd",)], [("ln_rstd",)], bias=self.eps_col[:, 0:1])
        self.ACT(rstd[:], rstd[:], AF.Exp, [("ln_rstd",)], [("ln_rstd",)], scale=-0.5)
        self.TT("dve", mr[:], mean[:], rstd[:], ALU.mult, [("ln_mean",), ("ln_rstd",)], [("ln_mr",)])

    def ln_scratch(self, off):
        sc = {}
        o = off
        sc["rb"] = self.carve(o, 2048, BF16).rearrange("p (j t) -> p j t", j=2); o += 2048
        sc["sq"] = self.carve(o, 2048, BF16).rearrange("p (j t) -> p j t", j=2); o += 2048
        for nm in ("mean", "rstd", "mr"):
            sc[nm] = self.carve(o, 2048, F32); o += 2048
        self.ln_scr = sc
        return o

    def ln_chunk(self, c, gname, bname):
        tok = slice(c * 512, (c + 1) * 512)
        sc = self.ln_scr
        rstd, mr = sc["rstd"], sc["mr"]
        self.ln_stats([self.X[:, kt, tok] for kt in range(NKT)], [("X", kt, c) for kt in range(NKT)])
        for kt in range(NKT):
            xs = self.X[:, kt, tok]
            gcol = self.pcol(gname, kt)
            self.STT("dve", xs, xs, gcol, rstd[:], ALU.mult, ALU.mult, [("X", kt, c), ("ln_rstd",), ("pc",)], [("X", kt, c)])
            self.STT("dve", xs, mr[:], gcol, xs, ALU.mult, ALU.subtract, [("X", kt, c), ("ln_mr",), ("pc",)], [("X", kt, c)])
            self.ACT(self.XT[:, kt, tok], xs, AF.Identity, [("X", kt, c), ("pc",)], [("XT", kt, c)],
                     bias=self.pcol(bname, kt), scale=-1.0)
            self.ACT(xs, xs, AF.Identity, [("X", kt, c), ("pc",)], [("X", kt, c)],
                     bias=self.pcol(bname, kt), scale=-1.0)

    def ffn(self, l):
        S = self.S
        dr = self.dr
        S.barrier()
        OFF_S = self.OFF_S
        H = self.carve(OFF_S, 45056, BF16).rearrange("p (f t) -> p f t", f=NFT)
        o = OFF_S + 45056
        sg = self.carve(o, 2 * 2048, F32).rearrange("p (j t) -> p j t", j=2); o += 4096
        wd_all = self.carve(self.OFF_W, 45056, BF16).rearrange("p (f c) -> p f c", f=NFT)
        end_ln = self.ln_scratch(o)
        assert end_ln <= self.OFF_S + self.SSZ, end_ln
        groups = [(0, 4), (4, 4), (8, 4), (12, 4), (16, 4), (20, 2)]
        wg, wu, wdn = dr["w_gate%d" % l], dr["w_up%d" % l], dr["w_down%d" % l]
        for half in range(2):
            t0 = half * 1024
            for gi, (f0, nf) in enumerate(groups):
                sl = (gi % 2) * 2
                gsl = self.wslot(sl, 8, 512)
                usl = self.wslot(sl + 1, 8, 512)
                self.load_w_cols(gsl[:, :, 0:nf * 128], wg, f0 * 128, nf * 128, "wsl%d" % sl, ("wsl", sl))
                self.load_w_cols(usl[:, :, 0:nf * 128], wu, f0 * 128, nf * 128, "wsl%d" % (sl + 1), ("wsl", sl + 1))
                for fi in range(nf):
                    f = f0 + fi
                    for cc in range(2):
                        c = half * 2 + cc
                        tok = slice(c * 512, (c + 1) * 512)
                        bg = self._psrr % 2
                        bu = 2 + self._psrr % 2
                        self._psrr += 1
                        pg = self.psum[:, bg, :]
                        pu = self.psum[:, bu, :]
                        for kt in range(NKT):
                            self.MM(pg, gsl[:, kt, fi * 128:(fi + 1) * 128], self.XT[:, kt, tok], kt == 0, kt == NKT - 1,
                                    [("wsl", sl), ("XT", kt, c)], [("ps", bg)])
                        for kt in range(NKT):
                            self.MM(pu, usl[:, kt, fi * 128:(fi + 1) * 128], self.XT[:, kt, tok], kt == 0, kt == NKT - 1,
                                    [("wsl", sl + 1), ("XT", kt, c)], [("ps", bu)])
                        j = bg
                        self.ACT(sg[:, j, :], pg, AF.Silu, [("ps", bg)], [("sg", j)])
                        self.TT("dve", H[:, f, cc * 512:(cc + 1) * 512], sg[:, j, :], pu, ALU.mult,
                                [("sg", j), ("ps", bu)], [("H", f, cc)])
            for i, (f0, nf) in enumerate(groups):
                src = wdn[f0 * 128:(f0 + nf) * 128, :].rearrange("(f p) c -> p f c", p=128)
                self.DMA("pool", wd_all[:, f0:f0 + nf, :], src, [], [("wsl", i)], "wsl%d" % i)
            for cc in range(2):
                c = half * 2 + cc
                tok = slice(c * 512, (c + 1) * 512)
                for dt in range(NKT):
                    b = 4 + dt % 2
                    po = self.psum[:, b, :]
                    for f in range(NFT):
                        self.MM(po, wd_all[:, f, dt * 128:(dt + 1) * 128], H[:, f, cc * 512:(cc + 1) * 512],
                                f == 0, f == NFT - 1, [("wsl", f // 4), ("H", f, cc)], [("ps", b)])
                    self.STT("dve", self.X[:, dt, tok], self.X[:, dt, tok], ALPHA, po, ALU.mult, ALU.add,
                             [("X", dt, c), ("ps", b)], [("X", dt, c)])
                self.ln_chunk(c, "ln_ffn_g%d" % l, "ln_ffn_b%d" % l)
        S.barrier()

    def mixer0(self):
        S = self.S
        dr = self.dr
        S.barrier()
        o = self.OFF_W
        lim = self.OFF_S + self.SSZ
        self.catT = self.carve(o, 32768, BF16).rearrange("p (k t) -> p k t", k=8); o += 32768
        if "catT" in self.dbg:
            self.MEMSET("pool", self.catT[:], 0.0, [("catT", k, c) for k in range(8) for c in range(4)])
        real = self.S
        f0 = len(real.ops)
        ra = Recorder()
        self.S = ra
        self.attention(0, 65536)
        rs = Recorder()
        self.S = rs
        self.ssd(o, lim)
        self.S = real
        merge_streams(real, [ra, rs])
        if not hasattr(real, "fix_act_ranges"):
            real.fix_act_ranges = []
        real.fix_act_ranges.append((f0, len(real.ops)))
        S.barrier()
        if "catT" in self.dbg:
            self.DMA("sp", dr["catT"].rearrange("(k p) t -> p k t", p=128), self.catT[:], [("catT", k, c) for k in range(8) for c in range(4)],
                     [("dbg", "catT")], "dbgcat")
        self.load_X()
        if "mix0_out" in self.stages_opt:
            self.mix0_out(o, lim)
        S.barrier()

    def attention(self, o, lim):
        S = self.S
        dr = self.dr
        wqp = self.carve(o, 12288, BF16).rearrange("p (b k s c) -> p b k s c", b=2, k=8, s=3); o += 12288
        qT = self.carve(o, 8192, BF16).rearrange("p (b t) -> p b t", b=2); o += 8192
        kT = self.carve(o, 16384, BF16).rearrange("p (b h t) -> p b h t", b=2, h=2); o += 16384
        Vp = self.carve(o, 2 * 4608, BF16).rearrange("p (b t h e) -> p b t h e", b=2, t=16, h=2); o += 9216
        ctab = self.carve(o, 4096, F32).rearrange("p (q h n) -> p q h n", q=16, h=8); o += 4096
        wtab = self.carve(o, 64, F32).rearrange("p (a h) -> p a h", a=2); o += 64
        tri = self.carve(o, 256, BF16); o += 256
        E = self.carve(o, 3072, BF16).rearrange("p (b t) -> p b t", b=3); o += 3072
        Oacc = self.carve(o, 2 * 1040, F32).rearrange("p (b q e) -> p b q e", b=2, q=4); o += 2080
        ksum = self.carve(o, 64, F32).rearrange("p (b n) -> p b n", b=2); o += 64
        o = (o + 63) // 64 * 64
        kmean = self.carve(o, 128, BF16).rearrange("p (b h n) -> p b h n", b=2, h=2); o += 128
        gm = self.carve(o, 64, F32).rearrange("p (h n) -> p h n", h=2); o += 64
        top8 = self.carve(o, 64, F32).rearrange("p (h n) -> p h n", h=2); o += 64
        mc = self.carve(o, 2 * 1024, F32).rearrange("p (b q h n) -> p b q h n", b=2, q=16, h=2); o += 2048
        rcp = self.carve(o, 32, F32).rearrange("p (b q) -> p b q", b=2); o += 32
        atok = self.carve(o, 4096, BF16).rearrange("p (b q f) -> p b q f", b=4, q=4); o += 4096
        trif = self.carve(o, 512, F32); o += 512
        assert o <= lim, (o, lim)
        self.DMA("sp", ctab.rearrange("p q h n -> p (q h n)"), dr["ctab"], [], [("ctab",)], "ctab")
        self.DMA("sp", wtab.rearrange("p a h -> p (a h)"), dr["wtab"], [], [("wtab",)], "wtab")
        self.MEMSET("pool", trif[:], 1.0, [("trif",)])
        self.OP("pool", "affine_select", (trif[:], trif[:], [[1, 128]], ALU.is_ge, 0.0), [("trif",)], [("trif",)],
                base=0, channel_multiplier=-1)
        self.CP("pool", tri[:], trif[:], [("trif",)], [("tri",)])
        self.MEMSET("pool", kT[:], 0.0, [("kT", b, c) for b in range(2) for c in range(4)])
        self.MEMSET("pool", kmean[:], 0.0, [("kmean", 0), ("kmean", 1)])
        psb = self.psum.rearrange("p b t -> p (b t)").bitcast(BF16).rearrange("p (b t) -> p b t", b=8)

        w3 = dr["w_in"][:, 0:1536].rearrange("(k p) (s c) -> p k s c", p=128, s=3)
        for hp in range(4):
            pb = hp % 2
            for si in range(3):
                self.DMA("pool", wqp[:, pb, :, si, :], w3[:, :, si, hp * 128:(hp + 1) * 128], [], [("wqp", pb)], "wqp%d" % pb)
            self.cut()
            for c in range(4):
                tok = slice(c * 512, (c + 1) * 512)
                pq = self.psum[:, 0, :]
                pk = self.psum[:, 1, :]
                for kt in range(NKT):
                    self.MM(pq, wqp[:, pb, kt, 0, :], self.XT[:, kt, tok], kt == 0, kt == NKT - 1,
                            [("wqp", pb), ("XT", kt, c)], [("ps", 0)])
                for kt in range(NKT):
                    self.MM(pk, wqp[:, pb, kt, 1, :], self.XT[:, kt, tok], kt == 0, kt == NKT - 1,
                            [("wqp", pb), ("XT", kt, c)], [("ps", 1)])
                self.ACT(qT[:, pb, tok], pq, AF.Identity, [("ps", 0)], [("qT", pb, c)], scale=0.125)
                self.CP("dve", kT[0:64, pb, 0, tok], pk[0:64, :], [("ps", 1)], [("kT", pb, c)])
                self.CP("dve", kT[64:128, pb, 1, tok], pk[64:128, :], [("ps", 1)], [("kT", pb, c)])
                self.OP("dve", "tensor_reduce", (ksum[:, pb, 2 * c:2 * c + 2], pk.rearrange("p (n j) -> p n j", n=2), AX.X, ALU.add),
                        [("ps", 1)], [("ksum", pb, c)])
                self.cut()
            self.ACT(kmean[0:64, pb, 0, 0:8], ksum[0:64, pb, :], AF.Identity, [("ksum", pb, c) for c in range(4)], [("kmean", pb)], scale=1.0 / 256)
            self.ACT(kmean[64:128, pb, 1, 0:8], ksum[64:128, pb, :], AF.Identity, [("ksum", pb, c) for c in range(4)], [("kmean", pb)], scale=1.0 / 256)
            skip = ""
            for g4 in range(4):
                if "v" in skip:
                    break
                pv = self.psum[:, g4 % 2, :].rearrange("p (t f) -> p t f", t=4)
                for ti in range(4):
                    tt = g4 * 4 + ti
                    for kt in range(NKT):
                        self.MM(pv[:, ti, :], self.XT[:, kt, tt * 128:(tt + 1) * 128], wqp[:, pb, kt, 2, :],
                                kt == 0, kt == NKT - 1, [("wqp", pb), ("XT", kt, tt // 4)], [("ps", g4 % 2)])
                for ti in range(4):
                    tt = g4 * 4 + ti
                    for hh in range(2):
                        h = 2 * hp + hh
                        self.TS("dve" if hh == 0 else "pool" if False else "dve", Vp[:, pb, tt, hh, 0:64], pv[:, ti, hh * 64:(hh + 1) * 64],
                                wtab[:, tt % 2, h:h + 1], None, ALU.mult, None, [("ps", g4 % 2), ("wtab",)], [("Vp", pb)])
                self.cut()
            for hh in range(2):
                if "o" in skip:
                    break
                h = 2 * hp + hh
                for par in range(2):
                    self.CP("dve", Vp[:, pb, par:16:2, hh, 64], wtab[:, par, h:h + 1].to_broadcast([128, 8]), [("wtab",)], [("Vp", pb)])
            self.MEMSET("pool", gm[:], -1e30, [("gm",)])
            pgt_all = self.psum[:, 0, :].rearrange("p (q h n) -> p q h n", q=16, h=2)
            for qt in range(2, 16):
                if "m" in skip:
                    break
                self.MM(pgt_all[:, qt, :, :].rearrange("p h n -> p (h n)"), qT[:, pb, qt * 128:(qt + 1) * 128],
                        kmean[:, pb, :, :].rearrange("p h n -> p (h n)"),
                        True, True, [("qT", pb, qt // 4), ("kmean", pb)], [("ps", 0)])
            for qt in range(2, 16):
                if "c" in skip:
                    break
                own = qt // 2
                self.CP("dve", gm[:, :, 0:own], pgt_all[:, qt, :, 0:own], [("ps", 0)], [("gm",)])
                for hh in range(2):
                    h = 2 * hp + hh
                    self.OP("dve", "max", (top8[:, hh, :], gm[:, hh, :]), [("gm",)], [("top8", hh)])
                    self.STT("dve", mc[:, pb, qt, hh, 0:own], gm[:, hh, 0:own], top8[:, hh, 2:3], ctab[:, qt, h, 0:own],
                             ALU.is_ge, ALU.mult, [("gm",), ("top8", hh), ("ctab",)], [("mc", pb, qt)])
            self.cut()
            for hh in range(2):
                h = 2 * hp + hh
                slope = 2.0 ** (-(h + 1))
                hs_ = slice(hh * 64, (hh + 1) * 64)
                for qc in range(4):
                    ob = (hh * 4 + qc) % 2
                    self.MEMSET("pool", Oacc[:, ob], 0.0, [("Oacc", ob)])
                    units = [(t, 0) for t in range(4 * qc)] + [(4 * qc + a, a) for a in range(4)]
                    nu = len(units)
                    sb = {}

                    def emit_s(ui, units=units, sb=sb):
                        t, a = units[ui]
                        b = 1 + self._psrr % 2
                        eb = self._psrr % 3
                        self._psrr += 1
                        n = (4 - a) * 128
                        q0 = qc * 512 + a * 128
                        ps = self.psum[:, b, 0:n]
                        self.MM(ps, kT[:, pb, hh, t * 128:(t + 1) * 128], qT[:, pb, q0:q0 + n], True, True,
                                [("kT", pb, t // 4), ("qT", pb, qc)], [("ps", b)])
                        self.ACT(E[:, eb, 0:n], ps, AF.Exp, [("ps", b)], [("E", eb)])
                        if t >= 4 * qc:
                            self.TT("pool", E[:, eb, 0:128], E[:, eb, 0:128], tri[:], ALU.mult, [("E", eb), ("tri",)], [("E", eb)])
                        sb[ui] = eb

                    for ui in range(min(2, nu)):
                        emit_s(ui)
                    ui = 0
                    while ui < nu:
                        t, a = units[ui]
                        n_blk = t // 2
                        blk_units = [ui]
                        if ui + 1 < nu and units[ui + 1][0] // 2 == n_blk:
                            blk_units.append(ui + 1)
                        pob = 3 + (self._psrr2 % 2)
                        self._psrr2 += 1
                        po = self.psum[:, pob, 0:260].rearrange("p (q e) -> p q e", q=4)
                        for i in range(4):
                            qt = 4 * qc + i
                            srcu = [u for u in blk_units if units[u][1] <= i and units[u][0] <= qt]
                            if not srcu:
                                continue
                            for si, u in enumerate(srcu):
                                tu, au = units[u]
                                self.MM(po[:, i, :], E[:, sb[u], (i - au) * 128:(i - au + 1) * 128], Vp[:, pb, tu, hh, 0:65],
                                        si == 0, si == len(srcu) - 1, [("E", sb[u]), ("Vp", pb)], [("ps", pob)])
                        for i in range(4):
                            qt = 4 * qc + i
                            srcu = [u for u in blk_units if units[u][1] <= i and units[u][0] <= qt]
                            if not srcu:
                                continue
                            own = qt // 2
                            if n_blk == own:
                                cown = float(np.exp(slope * 128.0)) if qt % 2 == 0 else 1.0
                                self.STT("dve", Oacc[:, ob, i, :], po[:, i, :], cown, Oacc[:, ob, i, :], ALU.mult, ALU.add,
                                         [("ps", pob), ("Oacc", ob)], [("Oacc", ob)])
                            else:
                                self.STT("dve", Oacc[:, ob, i, :], po[:, i, :], mc[:, pb, qt, hh, n_blk:n_blk + 1], Oacc[:, ob, i, :],
                                         ALU.mult, ALU.add, [("ps", pob), ("Oacc", ob), ("mc", pb, qt)], [("Oacc", ob)])
                        for u in blk_units:
                            if u + 2 < nu:
                                emit_s(u + 2)
                        ui += len(blk_units)
                        self.cut()
                    self.OP("dve", "reciprocal", (rcp[:, ob, :], Oacc[:, ob, :, 64]), [("Oacc", ob)], [("rcp", ob)])
                    ab = qc
                    self.TT("dve", atok[:, ab, :, hs_], Oacc[:, ob, :, 0:64], rcp[:, ob, :].unsqueeze(2).to_broadcast([128, 4, 64]),
                            ALU.mult, [("Oacc", ob), ("rcp", ob)], [("atok", ab, hh)])
                    if hh == 1:
                        pt = psb[:, 0, 0:512].rearrange("p (q f) -> p q f", q=4)
                        for i in range(4):
                            self.TR(pt[:, i, :], atok[:, ab, i, :], self.ident_bf[:], [("atok", ab, 0), ("atok", ab, 1), ("ident",)], [("ps", 0)])
                        self.CP("act", self.catT[:, hp, qc * 512:(qc + 1) * 512], pt.rearrange("p q f -> p (q f)"), [("ps", 0)], [("catT", hp, qc)])
                    self.cut()

    def ssd(self, o, lim):
        S = self.S
        dr = self.dr
        L = 256

        def cv(nbytes, dt_):
            nonlocal o
            o = (o + 63) // 64 * 64
            a = self.carve(o, nbytes, dt_)
            o += nbytes
            return a
        wssd = cv(8 * 1544 * 2, BF16).rearrange("p (k c) -> p k c", k=8)
        xpad = cv(2 * 260 * 4, F32).rearrange("p (c t) -> p c t", c=2)
        halo = cv(8 * 4 * 4, F32).rearrange("p (c t) -> p c t", c=8)
        acc = cv(2 * 256 * 4, F32).rearrange("p (j t) -> p j t", j=2)
        zsil = cv(4 * 256 * 4, F32).rearrange("p (c t) -> p c t", c=4)
        xsT = cv(4 * 256 * 4, F32).rearrange("p (c t) -> p c t", c=4)
        xsb = cv(4 * 256 * 2, BF16).rearrange("p (c t) -> p c t", c=4)
        BT = cv(2 * 256 * 2, BF16).rearrange("p (g t) -> p g t", g=2)
        CT = cv(2 * 256 * 2, BF16).rearrange("p (g t) -> p g t", g=2)
        xtok = cv(2 * 512 * 2, BF16).rearrange("p (s f) -> p s f", s=2)
        xdtz = cv(2 * 8 * 128 * 2, BF16).rearrange("p (s h f) -> p s h f", s=2, h=8)
        xdtd = cv(2 * 512 * 2, BF16).rearrange("p (s f) -> p s f", s=2)
        Btok = cv(2 * 2 * 128 * 2, BF16).rearrange("p (s g n) -> p s g n", s=2, g=2)
        dtt = cv(2 * 8 * 4, F32).rearrange("p (s h) -> p s h", s=2)
        adt = cv(2 * 8 * 4, F32).rearrange("p (s h) -> p s h", s=2)
        adt_hi = cv(2 * 8 * 2, BF16).rearrange("p (s h) -> p s h", s=2)
        adt_lo = cv(2 * 8 * 2, BF16).rearrange("p (s h) -> p s h", s=2)
        adt_r = cv(2 * 8 * 4, F32).rearrange("p (s h) -> p s h", s=2)
        acs = cv(2 * 8 * 4, F32).rearrange("p (s h) -> p s h", s=2)
        dte = cv(2 * 8 * 4, F32).rearrange("p (s h) -> p s h", s=2)
        cdec = cv(8 * 4, F32)
        Aneg = cv(8 * 4, F32)
        tri_le = cv(128 * 2, BF16)
        maskT = cv(2 * 256 * 2, BF16).rearrange("p (s l) -> p s l", s=2)
        CBm = cv(2 * 2 * 256 * 2, BF16).rearrange("p (g s l) -> p g s l", g=2, s=2)
        seg2 = cv(2 * 2 * 256 * 4, F32).rearrange("p (j s l) -> p j s l", j=2, s=2)
        MT = cv(2 * 2 * 256 * 2, BF16).rearrange("p (j s l) -> p j s l", j=2, s=2)
        dfs2 = cv(2 * 256 * 4, F32).rearrange("p (j l) -> p j l", j=2)
        CD = cv(2 * 256 * 2, BF16).rearrange("p (j l) -> p j l", j=2)
        Abc = cv(2 * 2 * 2 * 128 * 2, BF16).rearrange("p (b j s m) -> p b j s m", b=2, j=2, s=2)
        maskf = seg2[:, 0]
        trif = dfs2[:, 0, 0:128]
        hin = cv(4 * 128 * 4, F32).rearrange("p (q f) -> p q f", q=4)
        hz = cv(8 * 128 * 2, BF16).rearrange("p (h f) -> p h f", h=8)
        yg = cv(4 * 256 * 4, F32).rearrange("p (c t) -> p c t", c=4)
        ysq = cv(1 * 256 * 2, BF16).rearrange("p (j t) -> p j t", j=1)
        rs = cv(256 * 4, F32)
        one_col = cv(4, F32)
        assert o <= lim, (o, lim)
        psb = self.psum.rearrange("p b t -> p (b t)").bitcast(BF16).rearrange("p (b t) -> p b t", b=8)

        for i, (c0, ncol) in enumerate([(0, 512), (512, 512), (1024, 512), (1536, 8)]):
            self.load_w_cols(wssd[:, :, c0:c0 + ncol], dr["w_in"], 1536 + c0, ncol, "wsl%d" % i, ("wssd", i))
        self.MEMSET("pool", one_col[:], 1.0, [("one_col",)])
        self.MEMSET("pool", trif, 1.0, [("dfs", 0)])
        self.OP("pool", "affine_select", (trif, trif, [[1, 128]], ALU.is_ge, 0.0), [("dfs", 0)], [("dfs", 0)],
                base=0, channel_multiplier=-1)
        self.CP("pool", tri_le[:], trif, [("dfs", 0)], [("tri_le",)])
        self.MEMSET("pool", maskf[:], 1.0, [("seg", 0)])
        for st in range(2):
            self.OP("pool", "affine_select", (maskf[:, st, :], maskf[:, st, :], [[1, 256]], ALU.is_ge, 0.0), [("seg", 0)], [("seg", 0)],
                    base=-128 * st, channel_multiplier=-1)
        self.CP("pool", maskT[:], maskf[:], [("seg", 0)], [("maskT",)])
        self.MEMSET("pool", halo[:], 0.0, [("halo", ct) for ct in range(8)])
        self.MEMSET("pool", xdtz[:], 0.0, [("xdtz",)])
        self.MEMSET("pool", hin[:], 0.0, [("hin",)])
        self.MEMSET("pool", hz[:], 0.0, [("hz",)])
        self.ACT(Aneg[:], self.pcol("a_log", 0, 8), AF.Exp, [("pc",)], [("Aneg",)])
        self.TS("dve", Aneg[:], Aneg[:], -1.0, None, ALU.mult, None, [("Aneg",)], [("Aneg",)])
        osw, _ = self.pc_off["sconv_w"]

        for c in range(8):
            t0 = c * L
            tok = slice(t0, t0 + L)
            cq = c // 2
            for zt in range(4):
                b = zt % 2
                pz = self.psum[:, 5, b * L:(b + 1) * L]
                for kt in range(NKT):
                    self.MM(pz, wssd[:, kt, zt * 128:(zt + 1) * 128], self.XT[:, kt, tok], kt == 0, kt == NKT - 1,
                            [("wssd", 0), ("XT", kt, cq)], [("ps", 5)])
                self.ACT(zsil[:, zt, :], pz, AF.Silu, [("ps", 5)], [("zsil", zt)])
                self.cut()
            for ct in range(8):
                b = ct % 2
                px = self.psum[:, 5, b * L:(b + 1) * L]
                for kt in range(NKT):
                    self.MM(px, wssd[:, kt, 512 + ct * 128:512 + (ct + 1) * 128], self.XT[:, kt, tok], kt == 0, kt == NKT - 1,
                            [("wssd", 1 + ct // 4), ("XT", kt, cq)], [("ps", 5)])
                j = ct % 2
                self.CP("pool", xpad[:, j, 0:3], halo[:, ct, 0:3], [("halo", ct)], [("xpad", j)])
                self.CP("act", xpad[:, j, 3:3 + L], px, [("ps", 5)], [("xpad", j)])
                self.CP("pool", halo[:, ct, 0:3], xpad[:, j, L:L + 3], [("xpad", j)], [("halo", ct)])
                self.TS("dve", acc[:, j, :], xpad[:, j, 0:L], self.pc[:, osw + ct:osw + ct + 1], None, ALU.mult, None,
                        [("xpad", j), ("pc",)], [("acc", j)])
                for k in range(1, 4):
                    self.STT("dve", acc[:, j, :], xpad[:, j, k:k + L], self.pc[:, osw + k * 8 + ct:osw + k * 8 + ct + 1], acc[:, j, :],
                             ALU.mult, ALU.add, [("xpad", j), ("acc", j), ("pc",)], [("acc", j)])
                if ct < 4:
                    self.ACT(xsT[:, ct, :], acc[:, j, :], AF.Silu, [("acc", j), ("pc",)], [("xsT", ct)], bias=self.pcol("sconv_b", ct))
                    self.ACT(xsb[:, ct, :], acc[:, j, :], AF.Silu, [("acc", j), ("pc",)], [("xsb", ct)], bias=self.pcol("sconv_b", ct))
                elif ct < 6:
                    self.ACT(BT[:, ct - 4, :], acc[:, j, :], AF.Silu, [("acc", j), ("pc",)], [("BT", ct - 4)], bias=self.pcol("sconv_b", ct))
                else:
                    self.ACT(CT[:, ct - 6, :], acc[:, j, :], AF.Silu, [("acc", j), ("pc",)], [("CT", ct - 6)], bias=self.pcol("sconv_b", ct))
            self.cut()
            P71 = [("ps", 5)]
            pdt = self.psum[:, 5, 256:272].rearrange("p (s h) -> p s h", s=2)
            for st in range(2):
                for kt in range(NKT):
                    self.MM(pdt[:, st, :], self.XT[:, kt, t0 + st * 128:t0 + (st + 1) * 128], wssd[:, kt, 1536:1544], kt == 0, kt == NKT - 1,
                            [("wssd", 3), ("XT", kt, cq)], P71)
            self.TT("dve", dtt[:], pdt, self.pcol("dt_bias", 0, 8).unsqueeze(1).to_broadcast([128, 2, 8]), ALU.add,
                    P71 + [("pc",)], [("dtt",)])
            self.ACT(dtt[:], dtt[:], AF.Exp, [("dtt",)], [("dtt",)])
            self.ACT(dtt[:], dtt[:], AF.Ln, [("dtt",), ("one_col",)], [("dtt",)], bias=one_col[:, 0:1])
            self.TT("dve", adt[:], dtt[:], Aneg[:].unsqueeze(1).to_broadcast([128, 2, 8]), ALU.mult, [("dtt",), ("Aneg",)], [("adt",)])
            self.CP("dve", adt_hi[:], adt[:], [("adt",)], [("adt_hi",)])
            self.TT("dve", adt_r[:], adt[:], adt_hi[:], ALU.subtract, [("adt",), ("adt_hi",)], [("adt_r",)])
            self.CP("dve", adt_lo[:], adt_r[:], [("adt_r",)], [("adt_lo",)])
            pacs = self.psum[:, 5, 272:288].rearrange("p (s h) -> p s h", s=2)
            ptot = self.psum[:, 5, 288:296]
            hl = [(adt_hi, ("adt_hi",)), (adt_lo, ("adt_lo",))]
            for i, (a_, k_) in enumerate(hl):
                self.MM(pacs[:, 0, :], tri_le[:], a_[:, 0, :], i == 0, i == 1, [("tri_le",), k_], P71)
            for i, (a_, k_) in enumerate(hl):
                self.MM(pacs[:, 1, :], self.ones_bf[:], a_[:, 0, :], i == 0, False, [("ones",), k_], P71)
            for i, (a_, k_) in enumerate(hl):
                self.MM(pacs[:, 1, :], tri_le[:], a_[:, 1, :], False, i == 1, [("tri_le",), k_], P71)
            n_ = 0
            for st in range(2):
                for i, (a_, k_) in enumerate(hl):
                    self.MM(ptot, self.ones_bf[:], a_[:, st, :], n_ == 0, n_ == 3, [("ones",), k_], P71)
                    n_ += 1
            self.CP("dve", acs[:], pacs, P71, [("acs",)])
            self.ACT(cdec[:], ptot, AF.Exp, P71, [("cdec",)])
            self.TT("dve", dte[:], ptot.unsqueeze(1).to_broadcast([128, 2, 8]), acs[:], ALU.subtract, P71 + [("acs",)], [("dte",)])
            self.ACT(dte[:], dte[:], AF.Exp, [("dte",)], [("dte",)])
            for st in range(2):
                self.cut()
                pt = psb[:, 5, 512:1024].rearrange("p (q f) -> p q f", q=4)
                for ct in range(4):
                    self.TR(pt[:, ct, :], xsb[:, ct, st * 128:(st + 1) * 128], self.ident_bf[:], [("xsb", ct), ("ident",)], P71)
                self.CP("act", xtok[:, st, :], pt.rearrange("p q f -> p (q f)"), P71, [("xtok", st)])
                pt2 = psb[:, 5, 512:768].rearrange("p (g n) -> p g n", g=2)
                for g in range(2):
                    self.TR(pt2[:, g, :], BT[:, g, st * 128:(st + 1) * 128], self.ident_bf[:], [("BT", g), ("ident",)], P71)
                self.CP("act", Btok[:, st, :, :].rearrange("p g n -> p (g n)"), pt2.rearrange("p g n -> p (g n)"), P71, [("Btok", st)])
                for h in range(8):
                    self.TS("dve", xdtz[:, st, h, (h % 2) * 64:(h % 2) * 64 + 64], xtok[:, st, h * 64:(h + 1) * 64], dtt[:, st, h:h + 1], None,
                            ALU.mult, None, [("xtok", st), ("dtt",)], [("xdtz",)])
                    self.ACT(xdtd[:, st, h * 64:(h + 1) * 64], xdtz[:, st, h, (h % 2) * 64:(h % 2) * 64 + 64], AF.Copy,
                             [("xdtz",), ("dte",)], [("xdtd", st)], scale=dte[:, st, h:h + 1])
            for g in range(2):
                for st in range(2):
                    pcb = self.psum[:, 5, 256:512]
                    self.MM(pcb, BT[:, g, st * 128:(st + 1) * 128], CT[:, g, :], True, True, [("BT", g), ("CT", g)], P71)
                    self.TT("dve", CBm[:, g, st, :], pcb, maskT[:, st, :], ALU.mult, P71 + [("maskT",)], [("CBm", g)])
            for pr in range(4):
                g = pr // 2
                self.cut()
                py = self.psum[:, 7, 0:L]
                for hh in range(2):
                    h = 2 * pr + hh
                    j = h % 2
                    seg = seg2[:, j]
                    dfs = dfs2[:, j]
                    for i, a_ in enumerate((adt_hi, adt_lo)):
                        for st in range(2):
                            self.CP("dve", Abc[:, j, i, st, :], a_[:, st, h:h + 1].to_broadcast([128, 128]),
                                    [("adt_hi",) if i == 0 else ("adt_lo",)], [("Abc", j)])
                    pbb = (6,)
                    pbc = self.psum[:, 6, 0:L]
                    self.MM(pbc[:, 0:128], Abc[:, j, 0, 0, :], tri_le[:], True, False, [("Abc", j), ("tri_le",)], [("ps",) + pbb])
                    self.MM(pbc[:, 0:128], Abc[:, j, 1, 0, :], tri_le[:], False, True, [("Abc", j), ("tri_le",)], [("ps",) + pbb])
                    self.MM(pbc[:, 128:256], Abc[:, j, 0, 0, :], self.ones_bf[:], True, False, [("Abc", j), ("ones",)], [("ps",) + pbb])
                    self.MM(pbc[:, 128:256], Abc[:, j, 1, 0, :], self.ones_bf[:], False, False, [("Abc", j), ("ones",)], [("ps",) + pbb])
                    self.MM(pbc[:, 128:256], Abc[:, j, 0, 1, :], tri_le[:], False, False, [("Abc", j), ("tri_le",)], [("ps",) + pbb])
                    self.MM(pbc[:, 128:256], Abc[:, j, 1, 1, :], tri_le[:], False, True, [("Abc", j), ("tri_le",)], [("ps",) + pbb])
                    for st in range(2):
                        self.TS("dve", seg[:, st, :], pbc, acs[:, st, h:h + 1], 0.0, ALU.subtract, ALU.min,
                                [("ps",) + pbb, ("acs",)], [("seg", j)])
                    self.ACT(seg, seg, AF.Exp, [("seg", j)], [("seg", j)])
                    self.TT("dve", MT[:, j], seg, CBm[:, g], ALU.mult, [("seg", j), ("CBm", g)], [("MT", j)])
                    self.ACT(dfs, pbc, AF.Exp, [("ps",) + pbb], [("dfs", j)])
                    self.TT("dve", CD[:, j, :], CT[:, g, :], dfs, ALU.mult, [("CT", g), ("dfs", j)], [("CD", j)])
                    for st in range(2):
                        self.MM(py, xdtz[:, st, h, :], MT[:, j, st, :], hh == 0 and st == 0, False, [("xdtz",), ("MT", j)], [("ps", 7)])
                    self.MM(py, hz[:, h, :], CD[:, j, :], False, hh == 1, [("hz",), ("CD", j)], [("ps", 7)])
                    self.cut()
                self.STT("dve", yg[:, pr, :], xsT[:, pr, :], self.pcol("ssd_d", pr), py, ALU.mult, ALU.add,
                         [("xsT", pr), ("ps", 7), ("pc",)], [("yg", pr)])
                self.TT("dve", yg[:, pr, :], yg[:, pr, :], zsil[:, pr, :], ALU.mult, [("yg", pr), ("zsil", pr)], [("yg", pr)])
            for pr in range(4):
                g = pr // 2
                self.cut()
                pst = self.psum[:, 5, 256:384]
                for st in range(2):
                    self.MM(pst, Btok[:, st, g, :], xdtd[:, st, pr * 128:(pr + 1) * 128], st == 0, st == 1,
                            [("Btok", st), ("xdtd", st)], P71)
                for hh in range(2):
                    h = 2 * pr + hh
                    cs = slice(hh * 64, hh * 64 + 64)
                    self.STT("dve", hin[:, pr, cs], hin[:, pr, cs], cdec[:, h:h + 1], pst[:, cs], ALU.mult, ALU.add,
                             [("hin",), ("cdec",)] + P71, [("hin",)])
                    self.CP("pool", hz[:, h, cs], hin[:, pr, cs], [("hin",)], [("hz",)])
            for g in range(2):
                self.cut()
                pss = self.psum[:, 5, 256:512]
                for i in range(2):
                    ct = 2 * g + i
                    self.ACT(ysq[:, 0, :], yg[:, ct, :], AF.Square, [("yg", ct)], [("ysq", 0)])
                    self.MM(pss, self.ones_bf[:], ysq[:, 0, :], i == 0, i == 1, [("ones",), ("ysq", 0)], P71)
                self.ACT(rs[:], pss, AF.Ln, P71, [("rs",)], bias=self.eps_col[:, 0:1], scale=1.0 / 256)
                self.ACT(rs[:], rs[:], AF.Exp, [("rs",)], [("rs",)], scale=-0.5)
                for i in range(2):
                    ct = 2 * g + i
                    self.TT("dve", yg[:, ct, :], yg[:, ct, :], rs[:], ALU.mult, [("yg", ct), ("rs",)], [("yg", ct)])
                    self.ACT(self.catT[:, 4 + ct, tok], yg[:, ct, :], AF.Identity, [("yg", ct), ("pc",)], [("catT", 4 + ct, cq)],
                             scale=self.pcol("snorm_w", ct))

    def mix0_out(self, o, lim):
        S = self.S
        dr = self.dr
        wo = self.carve(o, 16384, BF16).rearrange("p (k c) -> p k c", k=8); o += 16384
        o = self.ln_scratch(o)
        assert o <= lim
        for i in range(2):
            self.load_w_cols(wo[:, :, i * 512:(i + 1) * 512], dr["w_out"], i * 512, 512, "wsl%d" % i, ("wo", i))
        for c in range(4):
            tok = slice(c * 512, (c + 1) * 512)
            for dt in range(NKT):
                b = dt % 2
                po = self.psum[:, b, :]
                for kt in range(NKT):
                    self.MM(po, wo[:, kt, dt * 128:(dt + 1) * 128], self.catT[:, kt, tok], kt == 0, kt == NKT - 1,
                            [("wo", dt // 4), ("catT", kt, c)], [("ps", b)])
                self.STT("dve", self.X[:, dt, tok], self.X[:, dt, tok], ALPHA, po, ALU.mult, ALU.add,
                         [("X", dt, c), ("ps", b)], [("X", dt, c)])
            self.ln_chunk(c, "ln_mix_g0", "ln_mix_b0")

    def mixer1(self):
        S = self.S
        dr = self.dr
        S.barrier()
        pw1 = self.carve(self.OFF_W, 32768, BF16).rearrange("p (k c) -> p k c", k=8)
        pw2 = self.carve(self.OFF_W + 32768, 16384, BF16).rearrange("p (k c) -> p k c", k=8)
        for i in range(4):
            self.load_w_cols(pw1[:, :, i * 512:(i + 1) * 512], dr["w_pw1"], i * 512, 512, "wsl%d" % i, ("wsl", i))
        for i in range(2):
            self.load_w_cols(pw2[:, :, i * 512:(i + 1) * 512], dr["w_pw2"], i * 512, 512, "wsl%d" % (4 + i), ("wsl", 4 + i))
        o = self.OFF_S
        hp = self.carve(o, 8704, BF16)[:, 0:8 * 542].rearrange("p (c t) -> p c t", c=8); o += 8704
        diag2 = self.carve(o, 2 * 7936, BF16).rearrange("p (b k m) -> p b k m", b=2, k=31); o += 2 * 7936
        sig = self.carve(o, 2048, F32); o += 2048
        conv_o = self.carve(o, 16384, F32).rearrange("p (c t) -> p c t", c=8); o += 16384
        hs = self.carve(o, 8192, BF16).rearrange("p (c t) -> p c t", c=8); o += 8192
        o = self.ln_scratch(o)
        assert o <= self.OFF_S + self.SSZ, o - self.OFF_S
        sc = self.ln_scr
        for ct in range(8):
            self.MEMSET("pool", hp[:, ct, 0:30], 0.0, [("hp", ct)])
        ow, _ = self.pc_off["dw_w"]
        for c in range(4):
            tok = slice(c * 512, (c + 1) * 512)
            for dt in range(NKT):
                self.ACT(self.X[:, dt, tok], self.X[:, dt, tok], AF.Identity, [("X", dt, c), ("pc",)], [("X", dt, c)],
                         bias=self.pcol("b_pw2", dt), scale=ALPHA)
            for ct in range(8):
                pv = self.psum[:, 0 + ct % 2, :]
                pg = self.psum[:, 2 + ct % 2, :]
                pcv = self.psum[:, 4 + ct % 2, :]
                for kt in range(NKT):
                    self.MM(pv, pw1[:, kt, ct * 128:(ct + 1) * 128], self.XT[:, kt, tok], kt == 0, kt == NKT - 1,
                            [("wsl", ct // 4), ("XT", kt, c)], [("ps", ct % 2)])
                for kt in range(NKT):
                    self.MM(pg, pw1[:, kt, 1024 + ct * 128:1024 + (ct + 1) * 128], self.XT[:, kt, tok], kt == 0, kt == NKT - 1,
                            [("wsl", 2 + ct // 4), ("XT", kt, c)], [("ps", 2 + ct % 2)])
                self.ACT(sig[:], pg, AF.Sigmoid, [("ps", 2 + ct % 2), ("pc",)], [("sig",)], bias=self.pcol("b_pw1", 8 + ct))
                self.STT("dve", hp[:, ct, 30:542], pv, self.pcol("b_pw1", ct), sig[:], ALU.add, ALU.mult,
                         [("ps", ct % 2), ("sig",), ("pc",)], [("hp", ct)])
                wk = self.pc[:, ow + ct:ow + 248:8]
                diag = diag2[:, ct % 2]
                self.TT("pool", diag, self.ident_bf[:].unsqueeze(1).to_broadcast([128, 31, 128]),
                        wk.unsqueeze(2).to_broadcast([128, 31, 128]), ALU.mult, [("ident",), ("pc",)], [("diag", ct % 2)])
                for k in range(31):
                    self.MM(pcv, diag[:, k, :], hp[:, ct, k:k + 512], k == 0, k == 30,
                            [("diag", ct % 2), ("hp", ct)], [("ps", 4 + ct % 2)])
                self.ACT(conv_o[:, ct, :], pcv, AF.Identity, [("ps", 4 + ct % 2), ("pc",)], [("conv_o", ct)],
                         bias=self.pcol("dw_b", ct))
                self.CP("pool", hp[:, ct, 0:30], hp[:, ct, 512:542], [("hp", ct)], [("hp", ct)])
            self.ln_stats([conv_o[:, ct, :] for ct in range(8)], [("conv_o", ct) for ct in range(8)])
            for ct in range(8):
                xs = conv_o[:, ct, :]
                gcol = self.pcol("cln_g", ct)
                self.STT("dve", xs, xs, gcol, sc["rstd"][:], ALU.mult, ALU.mult, [("conv_o", ct), ("ln_rstd",), ("pc",)], [("conv_o", ct)])
                self.STT("dve", xs, sc["mr"][:], gcol, xs, ALU.mult, ALU.subtract, [("conv_o", ct), ("ln_mr",), ("pc",)], [("conv_o", ct)])
                self.ACT(hs[:, ct, :], xs, AF.Silu, [("conv_o", ct), ("pc",)], [("hs", ct)],
                         bias=self.pcol("cln_b", ct), scale=-1.0)
            for dt in range(NKT):
                b = dt % 2
                po = self.psum[:, b, :]
                for ct in range(8):
                    self.MM(po, pw2[:, ct, dt * 128:(dt + 1) * 128], hs[:, ct, :], ct == 0, ct == 7,
                            [("wsl", 4 + dt // 4), ("hs", ct)], [("ps", b)])
                self.TT("dve", self.X[:, dt, tok], self.X[:, dt, tok], po, ALU.add, [("X", dt, c), ("ps", b)], [("X", dt, c)])
            self.ln_chunk(c, "ln_mix_g1", "ln_mix_b1")
        S.barrier()

_STAGES = ["mix0", "ffn0", "mix1", "ffn1"]


def _prep_inputs(inp):
    pc = pack_params(inp)
    pcarr = pc.array()
    shared = {
        "pcols": pcarr,
        "w_in": np.ascontiguousarray(np.asarray(inp["mix_w_in"][0], np.float32)),
        "w_out": np.ascontiguousarray(np.asarray(inp["mix_w_out"][0], np.float32)),
        "w_pw1": np.ascontiguousarray(np.asarray(inp["conv_w_pw1"][0], np.float32)),
        "w_pw2": np.ascontiguousarray(np.asarray(inp["conv_w_pw2"][0], np.float32)),
    }
    for l in range(2):
        shared["w_gate%d" % l] = np.ascontiguousarray(np.asarray(inp["ffn_w_gate"][l], np.float32))
        shared["w_up%d" % l] = np.ascontiguousarray(np.asarray(inp["ffn_w_up"][l], np.float32))
        shared["w_down%d" % l] = np.ascontiguousarray(np.asarray(inp["ffn_w_down"][l], np.float32))
    slopes = 2.0 ** (-(np.arange(8, dtype=np.float64) + 1.0))
    qt = np.arange(16, dtype=np.float64)[:, None, None]
    nn = np.arange(8, dtype=np.float64)[None, None, :]
    ct = np.exp(np.minimum(-slopes[None, :, None] * 128.0 * (qt - 2 * nn - 1), 80.0))
    shared["ctab"] = np.ascontiguousarray(np.tile(ct.reshape(1, -1), (128, 1)).astype(np.float32))
    jrel = (np.arange(128, dtype=np.float64)[:, None, None] + 128.0 * np.arange(2)[None, :, None] - 128.0)
    shared["wtab"] = np.ascontiguousarray(np.exp(slopes[None, None, :] * jrel).reshape(128, 16).astype(np.float32))
    x = np.asarray(inp["x"], np.float32)
    in_maps = []
    for b in range(8):
        m = dict(shared)
        m["xT"] = np.ascontiguousarray(x[b].T)
        in_maps.append(m)
    return pc, in_maps


def run(inp, stages=_STAGES, dbg=None, trace=False):
    pc, in_maps = _prep_inputs(inp)
    nc = bass.Bass("TRN2", target_bir_lowering=False)
    k = K(nc, pc.off, pc.n, stages, dbg)
    k.build()
    res = run_bass_kernel_spmd(nc, in_maps, core_ids=list(range(8)), trace=trace)
    return res


def kernel(**inputs):
    res = run(inputs)
    out = np.stack([np.ascontiguousarray(r["outT"].T) for r in res.results], axis=0)
    return out.astype(np.float32)
```

```python
from contextlib import ExitStack
import numpy as np
import concourse.bass as bass
import concourse.mybir as mybir
from concourse.bass_utils import run_bass_kernel_spmd

F32 = mybir.dt.float32
BF16 = mybir.dt.bfloat16
AF = mybir.ActivationFunctionType
ALU = mybir.AluOpType
AX = mybir.AxisListType

ENGS = ("pe", "act", "dve", "pool", "sp")

D = 1024
SEQ = 2048
NKT = 8
FFN = 2816
NFT = 22
IN_COLS = 3080
ALPHA = float(4 ** 0.25)
LN_EPS = 1e-5
RMS_EPS = 1e-5


import heapq


class _Op:
    __slots__ = ("eng", "fn", "reads", "writes", "dma", "deps", "sig", "sigval", "waits", "idx", "cost")


class Sched:
    def __init__(self):
        self.ops = []
        self.lastw = {}
        self.readers = {}
        self.barrier_at = []

    def add(self, eng, fn, reads=(), writes=(), dma=None, cost=300.0):
        o = _Op()
        o.eng = eng; o.fn = fn; o.reads = tuple(reads); o.writes = tuple(writes); o.dma = dma
        o.idx = len(self.ops); o.sig = False; o.sigval = None; o.waits = []; o.cost = cost
        deps = set()
        for k in o.reads:
            w = self.lastw.get(k)
            if w is not None:
                deps.add(w)
        for k in o.writes:
            w = self.lastw.get(k)
            if w is not None:
                deps.add(w)
            for r in self.readers.get(k, ()):
                deps.add(r)
        deps.discard(o.idx)
        o.deps = deps
        for k in o.reads:
            self.readers.setdefault(k, []).append(o.idx)
        for k in o.writes:
            self.lastw[k] = o.idx
            self.readers[k] = []
        self.ops.append(o)
        return o

    def barrier(self):
        self.barrier_at.append(len(self.ops))

    def reorder(self):
        ops = self.ops
        n = len(ops)
        bounds = sorted(set([0] + [b for b in self.barrier_at if 0 < b < n] + [n]))
        lastdma = {}
        sdeps = [set(o.deps) for o in ops]
        for o in ops:
            if o.dma is not None:
                p = lastdma.get(o.dma)
                if p is not None:
                    sdeps[o.idx].add(p)
                lastdma[o.dma] = o.idx
        for (f0, f1) in getattr(self, "fix_act_ranges", []):
            lastact = None
            for o in ops[f0:f1]:
                if o.eng == "act":
                    if lastact is not None:
                        sdeps[o.idx].add(lastact)
                    lastact = o.idx
        succ = [[] for _ in range(n)]
        for o in ops:
            for d in sdeps[o.idx]:
                succ[d].append(o.idx)
        finish = [0.0] * n
        new_order = []
        LAT = 250.0
        rsel = None
        for r in range(len(bounds) - 1):
            lo, hi = bounds[r], bounds[r + 1]
            if rsel is not None and r not in rsel:
                new_order.extend(range(lo, hi))
                for i in range(lo, hi):
                    finish[i] = 0.0
                continue
            t_eng = {}
            indeg = {}
            heap = []

            def est(i):
                o = ops[i]
                q = o.eng if o.dma is None else ("q", o.eng)
                t = t_eng.get(q, 0.0)
                for d in sdeps[i]:
                    if d >= lo:
                        if ops[d].dma is not None and d not in o.deps:
                            f = finish[d] - ops[d].cost + 60.0
                        else:
                            f = finish[d] + (LAT if ops[d].eng != o.eng or ops[d].dma is not None else 60.0)
                        if f > t:
                            t = f
                return t
            ALPHA_B = getattr(self, "alpha_b", 0.002)
            bl = {}
            for i in range(hi - 1, lo - 1, -1):
                m = 0.0
                for sidx in succ[i]:
                    if lo <= sidx < hi and bl[sidx] > m:
                        m = bl[sidx]
                bl[i] = m + ops[i].cost + 100.0
            for i in range(lo, hi):
                k = sum(1 for d in sdeps[i] if d >= lo)
                indeg[i] = k
                if k == 0:
                    heapq.heappush(heap, (est(i) - ALPHA_B * bl[i], i))
            while heap:
                key, i = heapq.heappop(heap)
                e = est(i)
                if e - ALPHA_B * bl[i] > key + 1e-6:
                    heapq.heappush(heap, (e - ALPHA_B * bl[i], i))
                    continue
                o = ops[i]
                q = o.eng if o.dma is None else ("q", o.eng)
                if o.dma is not None:
                    t_eng[q] = e + 60.0
                    t_eng[o.eng] = max(t_eng.get(o.eng, 0.0), e) + 60.0 if o.eng != "sp" else t_eng.get(o.eng, 0.0)
                    finish[i] = e + o.cost
                else:
                    t_eng[q] = e + o.cost
                    finish[i] = e + o.cost
                new_order.append(i)
                for sidx in succ[i]:
                    if lo <= sidx < hi:
                        indeg[sidx] -= 1
                        if indeg[sidx] == 0:
                            heapq.heappush(heap, (est(sidx) - ALPHA_B * bl[sidx], sidx))
            assert len(new_order) == hi, (len(new_order), hi)
            if not hasattr(self, "makespans"):
                self.makespans = []
            self.makespans.append((lo, hi, round(max([finish[i] for i in range(lo, hi)] + [0.0]) / 1e3, 1)))
        remap = {old: new for new, old in enumerate(new_order)}
        new_ops = [ops[i] for i in new_order]
        for o in new_ops:
            o.idx = remap[o.idx]
            o.deps = set(remap[d] for d in o.deps)
        self.ops = new_ops

    def finalize(self):
        if getattr(self, "do_reorder", True):
            self.reorder()
        ops = self.ops
        for b in self.barrier_at:
            last = {}
            for o in ops[:b]:
                if o.fn is None:
                    continue
                if o.dma is not None:
                    last[("dma", o.dma)] = o.idx
                else:
                    last[("eng", o.eng)] = o.idx
            first = {}
            for o in ops[b:]:
                if o.eng not in first:
                    first[o.eng] = o.idx
            for e, fi in first.items():
                for le in last.values():
                    ops[fi].deps.add(le)
        for o in ops:
            nd = set()
            for d in o.deps:
                p = ops[d]
                if p.dma is None and o.dma is None and p.eng == "pe" and o.eng == "pe":
                    continue
                nd.add(d)
            o.deps = nd
            for d in nd:
                ops[d].sig = True
        cnt = {}
        for o in ops:
            if o.dma is not None:
                key = ("dma", o.dma)
                cnt[key] = cnt.get(key, 0) + 16
                o.sigval = (key, cnt[key])
            elif o.sig:
                key = ("eng", o.eng)
                cnt[key] = cnt.get(key, 0) + 1
                o.sigval = (key, cnt[key])
        self.semkeys = list(cnt.keys())
        seen = {e: {} for e in ENGS}
        for o in ops:
            need = {}
            for d in o.deps:
                k, v = ops[d].sigval
                if v > need.get(k, 0):
                    need[k] = v
            s = seen[o.eng]
            for k, v in need.items():
                if s.get(k, 0) >= v:
                    continue
                s[k] = v
                o.waits.append((k, v))
        return cnt

    def emit(self, nc, stack):
        cnt = self.finalize()
        sems = {}
        for i, k in enumerate(self.semkeys):
            sems[k] = stack.enter_context(nc.semaphore("sm%d" % i))
        block = stack.enter_context(nc.Block())
        ops = self.ops

        def run(engname):
            def body(eng):
                for o in ops:
                    if o.eng != engname:
                        continue
                    for (k, v) in o.waits:
                        eng.wait_ge(sems[k], v)
                    if o.fn is None:
                        continue
                    ins = o.fn(eng)
                    if o.dma is not None:
                        ins.then_inc(sems[o.sigval[0]], 16)
                    elif o.sig:
                        ins.then_inc(sems[o.sigval[0]], 1)
            return body

        block.tensor(run("pe"))
        block.scalar(run("act"))
        block.vector(run("dve"))
        block.gpsimd(run("pool"))
        block.sync(run("sp"))
        return cnt


class Recorder:
    def __init__(self):
        self.segs = [[]]

    def add(self, eng, fn, reads=(), writes=(), dma=None, cost=300.0):
        self.segs[-1].append((eng, fn, tuple(reads), tuple(writes), dma, cost))

    def cut(self):
        if self.segs[-1]:
            self.segs.append([])


def merge_streams(real, recs):
    segs = [[sg for sg in r.segs if sg] for r in recs]
    tot = [max(1, sum(len(sg) for sg in ss)) for ss in segs]
    done = [0] * len(recs)
    idx = [0] * len(recs)
    while True:
        cand = [i for i in range(len(recs)) if idx[i] < len(segs[i])]
        if not cand:
            break
        i = min(cand, key=lambda i: done[i] / tot[i])
        sg = segs[i][idx[i]]
        for a in sg:
            real.add(*a)
        done[i] += len(sg)
        idx[i] += 1


def _col8(v):
    return np.ascontiguousarray(np.asarray(v, np.float32).reshape(-1, 128).T)


class PCols:
    def __init__(self):
        self.parts = []
        self.off = {}
        self.n = 0

    def put(self, name, arr):
        arr = np.asarray(arr, np.float32)
        assert arr.shape[0] == 128
        self.off[name] = (self.n, arr.shape[1])
        self.parts.append(arr)
        self.n += arr.shape[1]

    def array(self):
        return np.ascontiguousarray(np.concatenate(self.parts, axis=1))


def pack_params(inp):
    pc = PCols()
    for l in range(2):
        pc.put("ln_mix_g%d" % l, _col8(inp["ln_mix_g"][l]))
        pc.put("ln_mix_b%d" % l, _col8(inp["ln_mix_b"][l]))
        pc.put("ln_ffn_g%d" % l, _col8(inp["ln_ffn_g"][l]))
        pc.put("ln_ffn_b%d" % l, _col8(inp["ln_ffn_b"][l]))
    pc.put("b_pw1", _col8(inp["conv_b_pw1"][0]))
    dw = np.asarray(inp["conv_dw_w"][0], np.float32)
    pc.put("dw_w", np.concatenate([_col8(dw[k]) for k in range(31)], axis=1))
    pc.put("dw_b", _col8(inp["conv_dw_b"][0]))
    pc.put("cln_g", _col8(inp["conv_ln_g"][0]))
    pc.put("cln_b", _col8(inp["conv_ln_b"][0]))
    pc.put("b_pw2", _col8(inp["conv_b_pw2"][0]))
    cw = np.asarray(inp["ssd_conv_w"][0], np.float32)
    pc.put("sconv_w", np.concatenate([_col8(cw[k]) for k in range(4)], axis=1))
    pc.put("sconv_b", _col8(inp["ssd_conv_b"][0]))
    pc.put("snorm_w", _col8(inp["ssd_norm_w"][0]))
    pc.put("ssd_d", _col8(np.repeat(np.asarray(inp["ssd_d"][0], np.float32), 64)))
    pc.put("dt_bias", np.tile(np.asarray(inp["ssd_dt_bias"][0], np.float32)[None, :], (128, 1)))
    pc.put("a_log", np.tile(np.asarray(inp["ssd_a_log"][0], np.float32)[None, :], (128, 1)))
    return pc


class K:
    def __init__(self, nc, pc_off, npc, stages, dbg=None):
        self.nc = nc
        self.S = Sched()
        self.pc_off = pc_off
        self.npc = npc
        self.stages = stages
        self.stages_opt = set(stages) | ({'mix0_out'} if 'nomix0out' not in stages else set())
        self.dbg = dbg or {}
        self._psrr = 0
        self._psrr2 = 0

    @staticmethod
    def _fd(ap):
        n = 1
        for d in ap.shape[1:]:
            n *= d
        return n

    def _cost(self, eng, out):
        fd = self._fd(out)
        if eng == "act":
            return 200.0 + 0.85 * fd
        if eng == "dve":
            return 90.0 + 1.0 * fd
        if eng == "pool":
            return 300.0 + 2.6 * fd
        return 300.0

    def MM(self, out, lhsT, rhs, start, stop, r, w):
        self.S.add("pe", lambda e: e.matmul(out, lhsT, rhs, start=start, stop=stop), r, w, cost=max(64.0, self._fd(rhs)) / 2.4 + 10.0)

    def TR(self, out, in_, ident, r, w):
        self.S.add("pe", lambda e: e.transpose(out, in_, ident), r, w, cost=100.0)

    def ACT(self, out, in_, func, r, w, bias=None, scale=None):
        kw = {}
        if bias is not None:
            kw["bias"] = bias
        if scale is not None:
            kw["scale"] = scale
        self.S.add("act", lambda e: e.activation(out, in_, func, **kw), r, w, cost=self._cost("act", out))

    def TT(self, eng, out, in0, in1, op, r, w):
        self.S.add(eng, lambda e: e.tensor_tensor(out, in0, in1, op), r, w, cost=self._cost(eng, out))

    def TS(self, eng, out, in0, s1, s2, op0, op1, r, w):
        if s2 is None:
            self.S.add(eng, lambda e: e.tensor_scalar(out, in0, s1, None, op0), r, w, cost=self._cost(eng, out))
        else:
            self.S.add(eng, lambda e: e.tensor_scalar(out, in0, s1, s2, op0, op1), r, w, cost=self._cost(eng, out))

    def STT(self, eng, out, in0, scalar, in1, op0, op1, r, w):
        self.S.add(eng, lambda e: e.scalar_tensor_tensor(out, in0, scalar, in1, op0, op1), r, w, cost=self._cost(eng, out))

    def CP(self, eng, out, in_, r, w):
        if eng == "act":
            self.S.add("act", lambda e: e.copy(out, in_), r, w, cost=self._cost("act", out))
        else:
            self.S.add(eng, lambda e: e.tensor_copy(out, in_), r, w, cost=self._cost(eng, out))

    def OP(self, eng, meth, args, r, w, **kw):
        self.S.add(eng, lambda e: getattr(e, meth)(*args, **kw), r, w, cost=self._cost(eng, args[0]))

    def cut(self):
        if hasattr(self.S, "cut"):
            self.S.cut()

    def MEMSET(self, eng, ap, val, w):
        self.S.add(eng, lambda e: e.memset(ap, val), (), w, cost=self._cost(eng, ap) * 0.5)

    def DMA(self, eng, out, in_, r, w, key):
        nbytes = self._fd(out) * 128 * (2 if out.dtype == BF16 else 4)
        self.S.add(eng, lambda e: e.dma_start(out=out, in_=in_), r, w, dma=key, cost=2000.0 + nbytes / 180.0)

    def pcol(self, name, j=0, n=1, rows=slice(0, 128)):
        o, cnt = self.pc_off[name]
        return self.pc[rows, o + j:o + j + n]

    def carve(self, off_bytes, nbytes, dtype):
        assert off_bytes % 4 == 0 and nbytes % 4 == 0
        a = self.arena[:, off_bytes // 4:(off_bytes + nbytes) // 4]
        if dtype == BF16:
            a = a.bitcast(BF16)
        return a

    def build(self):
        nc = self.nc
        st = ExitStack()
        self.st = st
        dr = {}

        def din(name, shape):
            dr[name] = nc.dram_tensor(name, list(shape), F32, kind="ExternalInput").ap()

        din("xT", (D, SEQ))
        din("pcols", (128, self.npc))
        din("ctab", (128, 1024))
        din("wtab", (128, 16))
        din("w_in", (D, IN_COLS))
        din("w_out", (D, D))
        din("w_pw1", (D, 2 * D))
        din("w_pw2", (D, D))
        for l in range(2):
            din("w_gate%d" % l, (D, FFN))
            din("w_up%d" % l, (D, FFN))
            din("w_down%d" % l, (FFN, D))
        dr["outT"] = nc.dram_tensor("outT", [D, SEQ], F32, kind="ExternalOutput").ap()
        for name, shape in self.dbg.items():
            dr[name] = nc.dram_tensor(name, list(shape), BF16 if name.endswith("T") else F32, kind="ExternalOutput").ap()
        self.dr = dr

        OFF_X = 0
        OFF_XT = 65536
        OFF_W = 98304
        WSZ = 49152
        OFF_S = OFF_W + WSZ
        SSZ = 61440
        self.OFF_W, self.WSZ, self.OFF_S, self.SSZ = OFF_W, WSZ, OFF_S, SSZ
        ARENA = OFF_S + SSZ
        self.arena = st.enter_context(nc.sbuf_tensor("arena", [128, ARENA // 4], F32))
        self.pc = st.enter_context(nc.sbuf_tensor("pc", [128, self.npc], F32))
        self.ones_bf = st.enter_context(nc.sbuf_tensor("ones_bf", [128, 128], BF16))
        self.eps_col = st.enter_context(nc.sbuf_tensor("eps_col", [128, 2], F32))
        self.ident_bf = st.enter_context(nc.sbuf_tensor("ident_bf", [128, 128], BF16))
        self.ident_f = st.enter_context(nc.sbuf_tensor("ident_f", [128, 128], F32))
        self.psum = st.enter_context(nc.psum_tensor("psum", [128, 8, 512], F32))
        self.X = self.carve(OFF_X, 65536, F32).rearrange("p (k t) -> p k t", k=8)
        self.XT = self.carve(OFF_XT, 32768, BF16).rearrange("p (k t) -> p k t", k=8)

        S = self.S
        self.MEMSET("pool", self.ones_bf[:], 1.0, [("ones",)])
        self.MEMSET("pool", self.eps_col[:], LN_EPS, [("ones",)])
        self.MEMSET("pool", self.ident_f[:], 1.0, [("identf",)])
        S.add("pool", lambda e: e.affine_select(self.ident_f[:], self.ident_f[:], [[-1, 128]], ALU.is_equal, 0.0,
                                                base=0, channel_multiplier=1),
              [("identf",)], [("identf",)])
        self.CP("dve", self.ident_bf[:], self.ident_f[:], [("identf",)], [("ident",)])
        self.DMA("sp", self.pc[:], dr["pcols"], [], [("pc",)], "pc")
        if self.stages and self.stages[0] == "mix0":
            xsrc = dr["xT"].rearrange("(k p) t -> p k t", p=128)
            for c in range(4):
                for hk in range(2):
                    self.DMA("pool", self.XT[:, hk * 4:(hk + 1) * 4, c * 512:(c + 1) * 512],
                             xsrc[:, hk * 4:(hk + 1) * 4, c * 512:(c + 1) * 512], [],
                             [("XT", kt, c) for kt in range(NKT)], "xtload%d" % c)
        else:
            self.load_X()
            for kt in range(NKT):
                for c in range(4):
                    eng = "dve" if (kt + c) % 2 == 0 else "act"
                    self.CP(eng, self.XT[:, kt, c * 512:(c + 1) * 512], self.X[:, kt, c * 512:(c + 1) * 512],
                            [("X", kt, c)], [("XT", kt, c)])

        for stg in self.stages:
            if stg == "ffn0":
                self.ffn(0)
            elif stg == "ffn1":
                self.ffn(1)
            elif stg == "mix0":
                self.mixer0()
            elif stg == "mix1":
                self.mixer1()

        for kt in range(NKT):
            self.DMA("sp", dr["outT"][kt * 128:(kt + 1) * 128, :], self.X[:, kt, :],
                     [("X", kt, c) for c in range(4)], [("out", kt)], "outst")
        S.add("sp", None, [("out", kt) for kt in range(NKT)] + [("dbg", n) for n in self.dbg], [])
        cnt = S.emit(nc, st)
        st.close()
        return cnt

    def load_X(self):
        xsrc = self.dr["xT"].rearrange("(k p) t -> p k t", p=128)
        for c in range(4):
            for hk in range(2):
                self.DMA("sp", self.X[:, hk * 4:(hk + 1) * 4, c * 512:(c + 1) * 512],
                         xsrc[:, hk * 4:(hk + 1) * 4, c * 512:(c + 1) * 512], [],
                         [("X", kt, c) for kt in range(NKT)], "xload%d" % c)

    def wslot(self, i, nsub, ncols):
        assert nsub * ncols * 2 <= 8192
        a = self.carve(self.OFF_W + i * 8192, nsub * ncols * 2, BF16)
        return a.rearrange("p (k c) -> p k c", k=nsub)

    def load_w_cols(self, dst, w_dram, c0, ncols, key, wkey, nk=8, k0=0):
        src = w_dram[k0 * 128:(k0 + nk) * 128, c0:c0 + ncols].rearrange("(k p) c -> p k c", p=128)
        self.DMA("pool", dst, src, [], [wkey], key)

    def ln_stats(self, srcs, keys):
        sc = self.ln_scr
        rb, sq, mean, rstd, mr = sc["rb"], sc["sq"], sc["mean"], sc["rstd"], sc["mr"]
        ps_s = self.psum[:, 6, :]
        ps_q = self.psum[:, 7, :]
        n = len(srcs)
        for kt in range(n):
            j = kt % 2
            self.CP("act", rb[:, j, :], srcs[kt], [keys[kt]], [("ln_rb", j)])
            self.ACT(sq[:, j, :], srcs[kt], AF.Square, [keys[kt]], [("ln_sq", j)])
            self.MM(ps_s, self.ones_bf[:], rb[:, j, :], kt == 0, kt == n - 1, [("ones",), ("ln_rb", j)], [("ps", 6)])
            self.MM(ps_q, self.ones_bf[:], sq[:, j, :], kt == 0, kt == n - 1, [("ones",), ("ln_sq", j)], [("ps", 7)])
        nf = 1.0 / (128 * n)
        self.ACT(mean[:], ps_s, AF.Identity, [("ps", 6)], [("ln_mean",)], scale=nf)
        self.TT("dve", mr[:], mean[:], mean[:], ALU.mult, [("ln_mean",)], [("ln_mr",)])
        self.STT("dve", rstd[:], ps_q, nf, mr[:], ALU.mult, ALU.subtract, [("ps", 7), ("ln_mr",)], [("ln_rstd",)])
        self.ACT(rstd[:], rstd[:], AF.Ln, [("ln_rstd",)], [("ln_rstd",)], bias=self.eps_col[:, 0:1])
        self.ACT(rstd[:], rstd[:], AF.Exp, [("ln_rstd",)], [("ln_rstd",)], scale=-0.5)
        self.TT("dve", mr[:], mean[:], rstd[:], ALU.mult, [("ln_mean",), ("ln_rstd",)], [("ln_mr",)])

    def ln_scratch(self, off):
        sc = {}
        o = off
        sc["rb"] = self.carve(o, 2048, BF16).rearrange("p (j t) -> p j t", j=2); o += 2048
        sc["sq"] = self.carve(o, 2048, BF16).rearrange("p (j t) -> p j t", j=2); o += 2048
        for nm in ("mean", "rstd", "mr"):
            sc[nm] = self.carve(o, 2048, F32); o += 2048
        self.ln_scr = sc
        return o

    def ln_chunk(self, c, gname, bname):
        tok = slice(c * 512, (c + 1) * 512)
        sc = self.ln_scr
        rstd, mr = sc["rstd"], sc["mr"]
        self.ln_stats([self.X[:, kt, tok] for kt in range(NKT)], [("X", kt, c) for kt in range(NKT)])
        for kt in range(NKT):
            xs = self.X[:, kt, tok]
            gcol = self.pcol(gname, kt)
            self.STT("dve", xs, xs, gcol, rstd[:], ALU.mult, ALU.mult, [("X", kt, c), ("ln_rstd",), ("pc",)], [("X", kt, c)])
            self.STT("dve", xs, mr[:], gcol, xs, ALU.mult, ALU.subtract, [("X", kt, c), ("ln_mr",), ("pc",)], [("X", kt, c)])
            self.ACT(self.XT[:, kt, tok], xs, AF.Identity, [("X", kt, c), ("pc",)], [("XT", kt, c)],
                     bias=self.pcol(bname, kt), scale=-1.0)
            self.ACT(xs, xs, AF.Identity, [("X", kt, c), ("pc",)], [("X", kt, c)],
                     bias=self.pcol(bname, kt), scale=-1.0)

    def ffn(self, l):
        S = self.S
        dr = self.dr
        S.barrier()
        OFF_S = self.OFF_S
        H = self.carve(OFF_S, 45056, BF16).rearrange("p (f t) -> p f t", f=NFT)
        o = OFF_S + 45056
        sg = self.carve(o, 2 * 2048, F32).rearrange("p (j t) -> p j t", j=2); o += 4096
        wd_all = self.carve(self.OFF_W, 45056, BF16).rearrange("p (f c) -> p f c", f=NFT)
        end_ln = self.ln_scratch(o)
        assert end_ln <= self.OFF_S + self.SSZ, end_ln
        groups = [(0, 4), (4, 4), (8, 4), (12, 4), (16, 4), (20, 2)]
        wg, wu, wdn = dr["w_gate%d" % l], dr["w_up%d" % l], dr["w_down%d" % l]
        for half in range(2):
            t0 = half * 1024
            for gi, (f0, nf) in enumerate(groups):
                sl = (gi % 2) * 2
                gsl = self.wslot(sl, 8, 512)
                usl = self.wslot(sl + 1, 8, 512)
                self.load_w_cols(gsl[:, :, 0:nf * 128], wg, f0 * 128, nf * 128, "wsl%d" % sl, ("wsl", sl))
                self.load_w_cols(usl[:, :, 0:nf * 128], wu, f0 * 128, nf * 128, "wsl%d" % (sl + 1), ("wsl", sl + 1))
                for fi in range(nf):
                    f = f0 + fi
                    for cc in range(2):
                        c = half * 2 + cc
                        tok = slice(c * 512, (c + 1) * 512)
                        bg = self._psrr % 2
                        bu = 2 + self._psrr % 2
                        self._psrr += 1
                        pg = self.psum[:, bg, :]
                        pu = self.psum[:, bu, :]
                        for kt in range(NKT):
                            self.MM(pg, gsl[:, kt, fi * 128:(fi + 1) * 128], self.XT[:, kt, tok], kt == 0, kt == NKT - 1,
                                    [("wsl", sl), ("XT", kt, c)], [("ps", bg)])
                        for kt in range(NKT):
                            self.MM(pu, usl[:, kt, fi * 128:(fi + 1) * 128], self.XT[:, kt, tok], kt == 0, kt == NKT - 1,
                                    [("wsl", sl + 1), ("XT", kt, c)], [("ps", bu)])
                        j = bg
                        self.ACT(sg[:, j, :], pg, AF.Silu, [("ps", bg)], [("sg", j)])
                        self.TT("dve", H[:, f, cc * 512:(cc + 1) * 512], sg[:, j, :], pu, ALU.mult,
                                [("sg", j), ("ps", bu)], [("H", f, cc)])
            for i, (f0, nf) in enumerate(groups):
                src = wdn[f0 * 128:(f0 + nf) * 128, :].rearrange("(f p) c -> p f c", p=128)
                self.DMA("pool", wd_all[:, f0:f0 + nf, :], src, [], [("wsl", i)], "wsl%d" % i)
            for cc in range(2):
                c = half * 2 + cc
                tok = slice(c * 512, (c + 1) * 512)
                for dt in range(NKT):
                    b = 4 + dt % 2
                    po = self.psum[:, b, :]
                    for f in range(NFT):
                        self.MM(po, wd_all[:, f, dt * 128:(dt + 1) * 128], H[:, f, cc * 512:(cc + 1) * 512],
                                f == 0, f == NFT - 1, [("wsl", f // 4), ("H", f, cc)], [("ps", b)])
                    self.STT("dve", self.X[:, dt, tok], self.X[:, dt, tok], ALPHA, po, ALU.mult, ALU.add,
                             [("X", dt, c), ("ps", b)], [("X", dt, c)])
                self.ln_chunk(c, "ln_ffn_g%d" % l, "ln_ffn_b%d" % l)
        S.barrier()

    def mixer0(self):
        S = self.S
        dr = self.dr
        S.barrier()
        o = self.OFF_W
        lim = self.OFF_S + self.SSZ
        self.catT = self.carve(o, 32768, BF16).rearrange("p (k t) -> p k t", k=8); o += 32768
        if "catT" in self.dbg:
            self.MEMSET("pool", self.catT[:], 0.0, [("catT", k, c) for k in range(8) for c in range(4)])
        real = self.S
        f0 = len(real.ops)
        ra = Recorder()
        self.S = ra
        self.attention(0, 65536)
        rs = Recorder()
        self.S = rs
        self.ssd(o, lim)
        self.S = real
        merge_streams(real, [ra, rs])
        if not hasattr(real, "fix_act_ranges"):
            real.fix_act_ranges = []
        real.fix_act_ranges.append((f0, len(real.ops)))
        S.barrier()
        if "catT" in self.dbg:
            self.DMA("sp", dr["catT"].rearrange("(k p) t -> p k t", p=128), self.catT[:], [("catT", k, c) for k in range(8) for c in range(4)],
                     [("dbg", "catT")], "dbgcat")
        self.load_X()
        if "mix0_out" in self.stages_opt:
            self.mix0_out(o, lim)
        S.barrier()

    def attention(self, o, lim):
        S = self.S
        dr = self.dr
        wqp = self.carve(o, 12288, BF16).rearrange("p (b k s c) -> p b k s c", b=2, k=8, s=3); o += 12288
        qT = self.carve(o, 8192, BF16).rearrange("p (b t) -> p b t", b=2); o += 8192
        kT = self.carve(o, 16384, BF16).rearrange("p (b h t) -> p b h t", b=2, h=2); o += 16384
        Vp = self.carve(o, 2 * 4608, BF16).rearrange("p (b t h e) -> p b t h e", b=2, t=16, h=2); o += 9216
        ctab = self.carve(o, 4096, F32).rearrange("p (q h n) -> p q h n", q=16, h=8); o += 4096
        wtab = self.carve(o, 64, F32).rearrange("p (a h) -> p a h", a=2); o += 64
        tri = self.carve(o, 256, BF16); o += 256
        E = self.carve(o, 3072, BF16).rearrange("p (b t) -> p b t", b=3); o += 3072
        Oacc = self.carve(o, 2 * 1040, F32).rearrange("p (b q e) -> p b q e", b=2, q=4); o += 2080
        ksum = self.carve(o, 64, F32).rearrange("p (b n) -> p b n", b=2); o += 64
        o = (o + 63) // 64 * 64
        kmean = self.carve(o, 128, BF16).rearrange("p (b h n) -> p b h n", b=2, h=2); o += 128
        gm = self.carve(o, 64, F32).rearrange("p (h n) -> p h n", h=2); o += 64
        top8 = self.carve(o, 64, F32).rearrange("p (h n) -> p h n", h=2); o += 64
        mc = self.carve(o, 2 * 1024, F32).rearrange("p (b q h n) -> p b q h n", b=2, q=16, h=2); o += 2048
        rcp = self.carve(o, 32, F32).rearrange("p (b q) -> p b q", b=2); o += 32
        atok = self.carve(o, 4096, BF16).rearrange("p (b q f) -> p b q f", b=4, q=4); o += 4096
        trif = self.carve(o, 512, F32); o += 512
        assert o <= lim, (o, lim)
        self.DMA("sp", ctab.rearrange("p q h n -> p (q h n)"), dr["ctab"], [], [("ctab",)], "ctab")
        self.DMA("sp", wtab.rearrange("p a h -> p (a h)"), dr["wtab"], [], [("wtab",)], "wtab")
        self.MEMSET("pool", trif[:], 1.0, [("trif",)])
        self.OP("pool", "affine_select", (trif[:], trif[:], [[1, 128]], ALU.is_ge, 0.0), [("trif",)], [("trif",)],
                base=0, channel_multiplier=-1)
        self.CP("pool", tri[:], trif[:], [("trif",)], [("tri",)])
        self.MEMSET("pool", kT[:], 0.0, [("kT", b, c) for b in range(2) for c in range(4)])
        self.MEMSET("pool", kmean[:], 0.0, [("kmean", 0), ("kmean", 1)])
        psb = self.psum.rearrange("p b t -> p (b t)").bitcast(BF16).rearrange("p (b t) -> p b t", b=8)

        w3 = dr["w_in"][:, 0:1536].rearrange("(k p) (s c) -> p k s c", p=128, s=3)
        for hp in range(4):
            pb = hp % 2
            for si in range(3):
                self.DMA("pool", wqp[:, pb, :, si, :], w3[:, :, si, hp * 128:(hp + 1) * 128], [], [("wqp", pb)], "wqp%d" % pb)
            self.cut()
            for c in range(4):
                tok = slice(c * 512, (c + 1) * 512)
                pq = self.psum[:, 0, :]
                pk = self.psum[:, 1, :]
                for kt in range(NKT):
                    self.MM(pq, wqp[:, pb, kt, 0, :], self.XT[:, kt, tok], kt == 0, kt == NKT - 1,
                            [("wqp", pb), ("XT", kt, c)], [("ps", 0)])
                for kt in range(NKT):
                    self.MM(pk, wqp[:, pb, kt, 1, :], self.XT[:, kt, tok], kt == 0, kt == NKT - 1,
                            [("wqp", pb), ("XT", kt, c)], [("ps", 1)])
                self.ACT(qT[:, pb, tok], pq, AF.Identity, [("ps", 0)], [("qT", pb, c)], scale=0.125)
                self.CP("dve", kT[0:64, pb, 0, tok], pk[0:64, :], [("ps", 1)], [("kT", pb, c)])
                self.CP("dve", kT[64:128, pb, 1, tok], pk[64:128, :], [("ps", 1)], [("kT", pb, c)])
                self.OP("dve", "tensor_reduce", (ksum[:, pb, 2 * c:2 * c + 2], pk.rearrange("p (n j) -> p n j", n=2), AX.X, ALU.add),
                        [("ps", 1)], [("ksum", pb, c)])
                self.cut()
            self.ACT(kmean[0:64, pb, 0, 0:8], ksum[0:64, pb, :], AF.Identity, [("ksum", pb, c) for c in range(4)], [("kmean", pb)], scale=1.0 / 256)
            self.ACT(kmean[64:128, pb, 1, 0:8], ksum[64:128, pb, :], AF.Identity, [("ksum", pb, c) for c in range(4)], [("kmean", pb)], scale=1.0 / 256)
            skip = ""
            for g4 in range(4):
                if "v" in skip:
                    break
                pv = self.psum[:, g4 % 2, :].rearrange("p (t f) -> p t f", t=4)
                for ti in range(4):
                    tt = g4 * 4 + ti
                    for kt in range(NKT):
                        self.MM(pv[:, ti, :], self.XT[:, kt, tt * 128:(tt + 1) * 128], wqp[:, pb, kt, 2, :],
                                kt == 0, kt == NKT - 1, [("wqp", pb), ("XT", kt, tt // 4)], [("ps", g4 % 2)])
                for ti in range(4):
                    tt = g4 * 4 + ti
                    for hh in range(2):
                        h = 2 * hp + hh
                        self.TS("dve" if hh == 0 else "pool" if False else "dve", Vp[:, pb, tt, hh, 0:64], pv[:, ti, hh * 64:(hh + 1) * 64],
                                wtab[:, tt % 2, h:h + 1], None, ALU.mult, None, [("ps", g4 % 2), ("wtab",)], [("Vp", pb)])
                self.cut()
            for hh in range(2):
                if "o" in skip:
                    break
                h = 2 * hp + hh
                for par in range(2):
                    self.CP("dve", Vp[:, pb, par:16:2, hh, 64], wtab[:, par, h:h + 1].to_broadcast([128, 8]), [("wtab",)], [("Vp", pb)])
            self.MEMSET("pool", gm[:], -1e30, [("gm",)])
            pgt_all = self.psum[:, 0, :].rearrange("p (q h n) -> p q h n", q=16, h=2)
            for qt in range(2, 16):
                if "m" in skip:
                    break
                self.MM(pgt_all[:, qt, :, :].rearrange("p h n -> p (h n)"), qT[:, pb, qt * 128:(qt + 1) * 128],
                        kmean[:, pb, :, :].rearrange("p h n -> p (h n)"),
                        True, True, [("qT", pb, qt // 4), ("kmean", pb)], [("ps", 0)])
            for qt in range(2, 16):
                if "c" in skip:
                    break
                own = qt // 2
                self.CP("dve", gm[:, :, 0:own], pgt_all[:, qt, :, 0:own], [("ps", 0)], [("gm",)])
                for hh in range(2):
                    h = 2 * hp + hh
                    self.OP("dve", "max", (top8[:, hh, :], gm[:, hh, :]), [("gm",)], [("top8", hh)])
                    self.STT("dve", mc[:, pb, qt, hh, 0:own], gm[:, hh, 0:own], top8[:, hh, 2:3], ctab[:, qt, h, 0:own],
                             ALU.is_ge, ALU.mult, [("gm",), ("top8", hh), ("ctab",)], [("mc", pb, qt)])
            self.cut()
            for hh in range(2):
                h = 2 * hp + hh
                slope = 2.0 ** (-(h + 1))
                hs_ = slice(hh * 64, (hh + 1) * 64)
                for qc in range(4):
                    ob = (hh * 4 + qc) % 2
                    self.MEMSET("pool", Oacc[:, ob], 0.0, [("Oacc", ob)])
                    units = [(t, 0) for t in range(4 * qc)] + [(4 * qc + a, a) for a in range(4)]
                    nu = len(units)
                    sb = {}

                    def emit_s(ui, units=units, sb=sb):
                        t, a = units[ui]
                        b = 1 + self._psrr % 2
                        eb = self._psrr % 3
                        self._psrr += 1
                        n = (4 - a) * 128
                        q0 = qc * 512 + a * 128
                        ps = self.psum[:, b, 0:n]
                        self.MM(ps, kT[:, pb, hh, t * 128:(t + 1) * 128], qT[:, pb, q0:q0 + n], True, True,
                                [("kT", pb, t // 4), ("qT", pb, qc)], [("ps", b)])
                        self.ACT(E[:, eb, 0:n], ps, AF.Exp, [("ps", b)], [("E", eb)])
                        if t >= 4 * qc:
                            self.TT("pool", E[:, eb, 0:128], E[:, eb, 0:128], tri[:], ALU.mult, [("E", eb), ("tri",)], [("E", eb)])
                        sb[ui] = eb

                    for ui in range(min(2, nu)):
                        emit_s(ui)
                    ui = 0
                    while ui < nu:
                        t, a = units[ui]
                        n_blk = t // 2
                        blk_units = [ui]
                        if ui + 1 < nu and units[ui + 1][0] // 2 == n_blk:
                            blk_units.append(ui + 1)
                        pob = 3 + (self._psrr2 % 2)
                        self._psrr2 += 1
                        po = self.psum[:, pob, 0:260].rearrange("p (q e) -> p q e", q=4)
                        for i in range(4):
                            qt = 4 * qc + i
                            srcu = [u for u in blk_units if units[u][1] <= i and units[u][0] <= qt]
                            if not srcu:
                                continue
                            for si, u in enumerate(srcu):
                                tu, au = units[u]
                                self.MM(po[:, i, :], E[:, sb[u], (i - au) * 128:(i - au + 1) * 128], Vp[:, pb, tu, hh, 0:65],
                                        si == 0, si == len(srcu) - 1, [("E", sb[u]), ("Vp", pb)], [("ps", pob)])
                        for i in range(4):
                            qt = 4 * qc + i
                            srcu = [u for u in blk_units if units[u][1] <= i and units[u][0] <= qt]
                            if not srcu:
                                continue
                            own = qt // 2
                            if n_blk == own:
                                cown = float(np.exp(slope * 128.0)) if qt % 2 == 0 else 1.0
                                self.STT("dve", Oacc[:, ob, i, :], po[:, i, :], cown, Oacc[:, ob, i, :], ALU.mult, ALU.add,
                                         [("ps", pob), ("Oacc", ob)], [("Oacc", ob)])
                            else:
                                self.STT("dve", Oacc[:, ob, i, :], po[:, i, :], mc[:, pb, qt, hh, n_blk:n_blk + 1], Oacc[:, ob, i, :],
                                         ALU.mult, ALU.add, [("ps", pob), ("Oacc", ob), ("mc", pb, qt)], [("Oacc", ob)])
                        for u in blk_units:
                            if u + 2 < nu:
                                emit_s(u + 2)
                        ui += len(blk_units)
                        self.cut()
                    self.OP("dve", "reciprocal", (rcp[:, ob, :], Oacc[:, ob, :, 64]), [("Oacc", ob)], [("rcp", ob)])
                    ab = qc
                    self.TT("dve", atok[:, ab, :, hs_], Oacc[:, ob, :, 0:64], rcp[:, ob, :].unsqueeze(2).to_broadcast([128, 4, 64]),
                            ALU.mult, [("Oacc", ob), ("rcp", ob)], [("atok", ab, hh)])
                    if hh == 1:
                        pt = psb[:, 0, 0:512].rearrange("p (q f) -> p q f", q=4)
                        for i in range(4):
                            self.TR(pt[:, i, :], atok[:, ab, i, :], self.ident_bf[:], [("atok", ab, 0), ("atok", ab, 1), ("ident",)], [("ps", 0)])
                        self.CP("act", self.catT[:, hp, qc * 512:(qc + 1) * 512], pt.rearrange("p q f -> p (q f)"), [("ps", 0)], [("catT", hp, qc)])
                    self.cut()

    def ssd(self, o, lim):
        S = self.S
        dr = self.dr
        L = 256

        def cv(nbytes, dt_):
            nonlocal o
            o = (o + 63) // 64 * 64
            a = self.carve(o, nbytes, dt_)
            o += nbytes
            return a
        wssd = cv(8 * 1544 * 2, BF16).rearrange("p (k c) -> p k c", k=8)
        xpad = cv(2 * 260 * 4, F32).rearrange("p (c t) -> p c t", c=2)
        halo = cv(8 * 4 * 4, F32).rearrange("p (c t) -> p c t", c=8)
        acc = cv(2 * 256 * 4, F32).rearrange("p (j t) -> p j t", j=2)
        zsil = cv(4 * 256 * 4, F32).rearrange("p (c t) -> p c t", c=4)
        xsT = cv(4 * 256 * 4, F32).rearrange("p (c t) -> p c t", c=4)
        xsb = cv(4 * 256 * 2, BF16).rearrange("p (c t) -> p c t", c=4)
        BT = cv(2 * 256 * 2, BF16).rearrange("p (g t) -> p g t", g=2)
        CT = cv(2 * 256 * 2, BF16).rearrange("p (g t) -> p g t", g=2)
        xtok = cv(2 * 512 * 2, BF16).rearrange("p (s f) -> p s f", s=2)
        xdtz = cv(2 * 8 * 128 * 2, BF16).rearrange("p (s h f) -> p s h f", s=2, h=8)
        xdtd = cv(2 * 512 * 2, BF16).rearrange("p (s f) -> p s f", s=2)
        Btok = cv(2 * 2 * 128 * 2, BF16).rearrange("p (s g n) -> p s g n", s=2, g=2)
        dtt = cv(2 * 8 * 4, F32).rearrange("p (s h) -> p s h", s=2)
        adt = cv(2 * 8 * 4, F32).rearrange("p (s h) -> p s h", s=2)
        adt_hi = cv(2 * 8 * 2, BF16).rearrange("p (s h) -> p s h", s=2)
        adt_lo = cv(2 * 8 * 2, BF16).rearrange("p (s h) -> p s h", s=2)
        adt_r = cv(2 * 8 * 4, F32).rearrange("p (s h) -> p s h", s=2)
        acs = cv(2 * 8 * 4, F32).rearrange("p (s h) -> p s h", s=2)
        dte = cv(2 * 8 * 4, F32).rearrange("p (s h) -> p s h", s=2)
        cdec = cv(8 * 4, F32)
        Aneg = cv(8 * 4, F32)
        tri_le = cv(128 * 2, BF16)
        maskT = cv(2 * 256 * 2, BF16).rearrange("p (s l) -> p s l", s=2)
        CBm = cv(2 * 2 * 256 * 2, BF16).rearrange("p (g s l) -> p g s l", g=2, s=2)
        seg2 = cv(2 * 2 * 256 * 4, F32).rearrange("p (j s l) -> p j s l", j=2, s=2)
        MT = cv(2 * 2 * 256 * 2, BF16).rearrange("p (j s l) -> p j s l", j=2, s=2)
        dfs2 = cv(2 * 256 * 4, F32).rearrange("p (j l) -> p j l", j=2)
        CD = cv(2 * 256 * 2, BF16).rearrange("p (j l) -> p j l", j=2)
        Abc = cv(2 * 2 * 2 * 128 * 2, BF16).rearrange("p (b j s m) -> p b j s m", b=2, j=2, s=2)
        maskf = seg2[:, 0]
        trif = dfs2[:, 0, 0:128]
        hin = cv(4 * 128 * 4, F32).rearrange("p (q f) -> p q f", q=4)
        hz = cv(8 * 128 * 2, BF16).rearrange("p (h f) -> p h f", h=8)
        yg = cv(4 * 256 * 4, F32).rearrange("p (c t) -> p c t", c=4)
        ysq = cv(1 * 256 * 2, BF16).rearrange("p (j t) -> p j t", j=1)
        rs = cv(256 * 4, F32)
        one_col = cv(4, F32)
        assert o <= lim, (o, lim)
        psb = self.psum.rearrange("p b t -> p (b t)").bitcast(BF16).rearrange("p (b t) -> p b t", b=8)

        for i, (c0, ncol) in enumerate([(0, 512), (512, 512), (1024, 512), (1536, 8)]):
            self.load_w_cols(wssd[:, :, c0:c0 + ncol], dr["w_in"], 1536 + c0, ncol, "wsl%d" % i, ("wssd", i))
        self.MEMSET("pool", one_col[:], 1.0, [("one_col",)])
        self.MEMSET("pool", trif, 1.0, [("dfs", 0)])
        self.OP("pool", "affine_select", (trif, trif, [[1, 128]], ALU.is_ge, 0.0), [("dfs", 0)], [("dfs", 0)],
                base=0, channel_multiplier=-1)
        self.CP("pool", tri_le[:], trif, [("dfs", 0)], [("tri_le",)])
        self.MEMSET("pool", maskf[:], 1.0, [("seg", 0)])
        for st in range(2):
            self.OP("pool", "affine_select", (maskf[:, st, :], maskf[:, st, :], [[1, 256]], ALU.is_ge, 0.0), [("seg", 0)], [("seg", 0)],
                    base=-128 * st, channel_multiplier=-1)
        self.CP("pool", maskT[:], maskf[:], [("seg", 0)], [("maskT",)])
        self.MEMSET("pool", halo[:], 0.0, [("halo", ct) for ct in range(8)])
        self.MEMSET("pool", xdtz[:], 0.0, [("xdtz",)])
        self.MEMSET("pool", hin[:], 0.0, [("hin",)])
        self.MEMSET("pool", hz[:], 0.0, [("hz",)])
        self.ACT(Aneg[:], self.pcol("a_log", 0, 8), AF.Exp, [("pc",)], [("Aneg",)])
        self.TS("dve", Aneg[:], Aneg[:], -1.0, None, ALU.mult, None, [("Aneg",)], [("Aneg",)])
        osw, _ = self.pc_off["sconv_w"]

        for c in range(8):
            t0 = c * L
            tok = slice(t0, t0 + L)
            cq = c // 2
            for zt in range(4):
                b = zt % 2
                pz = self.psum[:, 5, b * L:(b + 1) * L]
                for kt in range(NKT):
                    self.MM(pz, wssd[:, kt, zt * 128:(zt + 1) * 128], self.XT[:, kt, tok], kt == 0, kt == NKT - 1,
                            [("wssd", 0), ("XT", kt, cq)], [("ps", 5)])
                self.ACT(zsil[:, zt, :], pz, AF.Silu, [("ps", 5)], [("zsil", zt)])
                self.cut()
            for ct in range(8):
                b = ct % 2
                px = self.psum[:, 5, b * L:(b + 1) * L]
                for kt in range(NKT):
                    self.MM(px, wssd[:, kt, 512 + ct * 128:512 + (ct + 1) * 128], self.XT[:, kt, tok], kt == 0, kt == NKT - 1,
                            [("wssd", 1 + ct // 4), ("XT", kt, cq)], [("ps", 5)])
                j = ct % 2
                self.CP("pool", xpad[:, j, 0:3], halo[:, ct, 0:3], [("halo", ct)], [("xpad", j)])
                self.CP("act", xpad[:, j, 3:3 + L], px, [("ps", 5)], [("xpad", j)])
                self.CP("pool", halo[:, ct, 0:3], xpad[:, j, L:L + 3], [("xpad", j)], [("halo", ct)])
                self.TS("dve", acc[:, j, :], xpad[:, j, 0:L], self.pc[:, osw + ct:osw + ct + 1], None, ALU.mult, None,
                        [("xpad", j), ("pc",)], [("acc", j)])
                for k in range(1, 4):
                    self.STT("dve", acc[:, j, :], xpad[:, j, k:k + L], self.pc[:, osw + k * 8 + ct:osw + k * 8 + ct + 1], acc[:, j, :],
                             ALU.mult, ALU.add, [("xpad", j), ("acc", j), ("pc",)], [("acc", j)])
                if ct < 4:
                    self.ACT(xsT[:, ct, :], acc[:, j, :], AF.Silu, [("acc", j), ("pc",)], [("xsT", ct)], bias=self.pcol("sconv_b", ct))
                    self.ACT(xsb[:, ct, :], acc[:, j, :], AF.Silu, [("acc", j), ("pc",)], [("xsb", ct)], bias=self.pcol("sconv_b", ct))
                elif ct < 6:
                    self.ACT(BT[:, ct - 4, :], acc[:, j, :], AF.Silu, [("acc", j), ("pc",)], [("BT", ct - 4)], bias=self.pcol("sconv_b", ct))
                else:
                    self.ACT(CT[:, ct - 6, :], acc[:, j, :], AF.Silu, [("acc", j), ("pc",)], [("CT", ct - 6)], bias=self.pcol("sconv_b", ct))
            self.cut()
            P71 = [("ps", 5)]
            pdt = self.psum[:, 5, 256:272].rearrange("p (s h) -> p s h", s=2)
            for st in range(2):
                for kt in range(NKT):
                    self.MM(pdt[:, st, :], self.XT[:, kt, t0 + st * 128:t0 + (st + 1) * 128], wssd[:, kt, 1536:1544], kt == 0, kt == NKT - 1,
                            [("wssd", 3), ("XT", kt, cq)], P71)
            self.TT("dve", dtt[:], pdt, self.pcol("dt_bias", 0, 8).unsqueeze(1).to_broadcast([128, 2, 8]), ALU.add,
                    P71 + [("pc",)], [("dtt",)])
            self.ACT(dtt[:], dtt[:], AF.Exp, [("dtt",)], [("dtt",)])
            self.ACT(dtt[:], dtt[:], AF.Ln, [("dtt",), ("one_col",)], [("dtt",)], bias=one_col[:, 0:1])
            self.TT("dve", adt[:], dtt[:], Aneg[:].unsqueeze(1).to_broadcast([128, 2, 8]), ALU.mult, [("dtt",), ("Aneg",)], [("adt",)])
            self.CP("dve", adt_hi[:], adt[:], [("adt",)], [("adt_hi",)])
            self.TT("dve", adt_r[:], adt[:], adt_hi[:], ALU.subtract, [("adt",), ("adt_hi",)], [("adt_r",)])
            self.CP("dve", adt_lo[:], adt_r[:], [("adt_r",)], [("adt_lo",)])
            pacs = self.psum[:, 5, 272:288].rearrange("p (s h) -> p s h", s=2)
            ptot = self.psum[:, 5, 288:296]
            hl = [(adt_hi, ("adt_hi",)), (adt_lo, ("adt_lo",))]
            for i, (a_, k_) in enumerate(hl):
                self.MM(pacs[:, 0, :], tri_le[:], a_[:, 0, :], i == 0, i == 1, [("tri_le",), k_], P71)
            for i, (a_, k_) in enumerate(hl):
                self.MM(pacs[:, 1, :], self.ones_bf[:], a_[:, 0, :], i == 0, False, [("ones",), k_], P71)
            for i, (a_, k_) in enumerate(hl):
                self.MM(pacs[:, 1, :], tri_le[:], a_[:, 1, :], False, i == 1, [("tri_le",), k_], P71)
            n_ = 0
            for st in range(2):
                for i, (a_, k_) in enumerate(hl):
                    self.MM(ptot, self.ones_bf[:], a_[:, st, :], n_ == 0, n_ == 3, [("ones",), k_], P71)
                    n_ += 1
            self.CP("dve", acs[:], pacs, P71, [("acs",)])
            self.ACT(cdec[:], ptot, AF.Exp, P71, [("cdec",)])
            self.TT("dve", dte[:], ptot.unsqueeze(1).to_broadcast([128, 2, 8]), acs[:], ALU.subtract, P71 + [("acs",)], [("dte",)])
            self.ACT(dte[:], dte[:], AF.Exp, [("dte",)], [("dte",)])
            for st in range(2):
                self.cut()
                pt = psb[:, 5, 512:1024].rearrange("p (q f) -> p q f", q=4)
                for ct in range(4):
                    self.TR(pt[:, ct, :], xsb[:, ct, st * 128:(st + 1) * 128], self.ident_bf[:], [("xsb", ct), ("ident",)], P71)
                self.CP("act", xtok[:, st, :], pt.rearrange("p q f -> p (q f)"), P71, [("xtok", st)])
                pt2 = psb[:, 5, 512:768].rearrange("p (g n) -> p g n", g=2)
                for g in range(2):
                    self.TR(pt2[:, g, :], BT[:, g, st * 128:(st + 1) * 128], self.ident_bf[:], [("BT", g), ("ident",)], P71)
                self.CP("act", Btok[:, st, :, :].rearrange("p g n -> p (g n)"), pt2.rearrange("p g n -> p (g n)"), P71, [("Btok", st)])
                for h in range(8):
                    self.TS("dve", xdtz[:, st, h, (h % 2) * 64:(h % 2) * 64 + 64], xtok[:, st, h * 64:(h + 1) * 64], dtt[:, st, h:h + 1], None,
                            ALU.mult, None, [("xtok", st), ("dtt",)], [("xdtz",)])
                    self.ACT(xdtd[:, st, h * 64:(h + 1) * 64], xdtz[:, st, h, (h % 2) * 64:(h % 2) * 64 + 64], AF.Copy,
                             [("xdtz",), ("dte",)], [("xdtd", st)], scale=dte[:, st, h:h + 1])
            for g in range(2):
                for st in range(2):
                    pcb = self.psum[:, 5, 256:512]
                    self.MM(pcb, BT[:, g, st * 128:(st + 1) * 128], CT[:, g, :], True, True, [("BT", g), ("CT", g)], P71)
                    self.TT("dve", CBm[:, g, st, :], pcb, maskT[:, st, :], ALU.mult, P71 + [("maskT",)], [("CBm", g)])
            for pr in range(4):
                g = pr // 2
                self.cut()
                py = self.psum[:, 7, 0:L]
                for hh in range(2):
                    h = 2 * pr + hh
                    j = h % 2
                    seg = seg2[:, j]
                    dfs = dfs2[:, j]
                    for i, a_ in enumerate((adt_hi, adt_lo)):
                        for st in range(2):
                            self.CP("dve", Abc[:, j, i, st, :], a_[:, st, h:h + 1].to_broadcast([128, 128]),
                                    [("adt_hi",) if i == 0 else ("adt_lo",)], [("Abc", j)])
                    pbb = (6,)
                    pbc = self.psum[:, 6, 0:L]
                    self.MM(pbc[:, 0:128], Abc[:, j, 0, 0, :], tri_le[:], True, False, [("Abc", j), ("tri_le",)], [("ps",) + pbb])
                    self.MM(pbc[:, 0:128], Abc[:, j, 1, 0, :], tri_le[:], False, True, [("Abc", j), ("tri_le",)], [("ps",) + pbb])
                    self.MM(pbc[:, 128:256], Abc[:, j, 0, 0, :], self.ones_bf[:], True, False, [("Abc", j), ("ones",)], [("ps",) + pbb])
                    self.MM(pbc[:, 128:256], Abc[:, j, 1, 0, :], self.ones_bf[:], False, False, [("Abc", j), ("ones",)], [("ps",) + pbb])
                    self.MM(pbc[:, 128:256], Abc[:, j, 0, 1, :], tri_le[:], False, False, [("Abc", j), ("tri_le",)], [("ps",) + pbb])
                    self.MM(pbc[:, 128:256], Abc[:, j, 1, 1, :], tri_le[:], False, True, [("Abc", j), ("tri_le",)], [("ps",) + pbb])
                    for st in range(2):
                        self.TS("dve", seg[:, st, :], pbc, acs[:, st, h:h + 1], 0.0, ALU.subtract, ALU.min,
                                [("ps",) + pbb, ("acs",)], [("seg", j)])
                    self.ACT(seg, seg, AF.Exp, [("seg", j)], [("seg", j)])
                    self.TT("dve", MT[:, j], seg, CBm[:, g], ALU.mult, [("seg", j), ("CBm", g)], [("MT", j)])
                    self.ACT(dfs, pbc, AF.Exp, [("ps",) + pbb], [("dfs", j)])
                    self.TT("dve", CD[:, j, :], CT[:, g, :], dfs, ALU.mult, [("CT", g), ("dfs", j)], [("CD", j)])
                    for st in range(2):
                        self.MM(py, xdtz[:, st, h, :], MT[:, j, st, :], hh == 0 and st == 0, False, [("xdtz",), ("MT", j)], [("ps", 7)])
                    self.MM(py, hz[:, h, :], CD[:, j, :], False, hh == 1, [("hz",), ("CD", j)], [("ps", 7)])
                    self.cut()
                self.STT("dve", yg[:, pr, :], xsT[:, pr, :], self.pcol("ssd_d", pr), py, ALU.mult, ALU.add,
                         [("xsT", pr), ("ps", 7), ("pc",)], [("yg", pr)])
                self.TT("dve", yg[:, pr, :], yg[:, pr, :], zsil[:, pr, :], ALU.mult, [("yg", pr), ("zsil", pr)], [("yg", pr)])
            for pr in range(4):
                g = pr // 2
                self.cut()
                pst = self.psum[:, 5, 256:384]
                for st in range(2):
                    self.MM(pst, Btok[:, st, g, :], xdtd[:, st, pr * 128:(pr + 1) * 128], st == 0, st == 1,
                            [("Btok", st), ("xdtd", st)], P71)
                for hh in range(2):
                    h = 2 * pr + hh
                    cs = slice(hh * 64, hh * 64 + 64)
                    self.STT("dve", hin[:, pr, cs], hin[:, pr, cs], cdec[:, h:h + 1], pst[:, cs], ALU.mult, ALU.add,
                             [("hin",), ("cdec",)] + P71, [("hin",)])
                    self.CP("pool", hz[:, h, cs], hin[:, pr, cs], [("hin",)], [("hz",)])
            for g in range(2):
                self.cut()
                pss = self.psum[:, 5, 256:512]
                for i in range(2):
                    ct = 2 * g + i
                    self.ACT(ysq[:, 0, :], yg[:, ct, :], AF.Square, [("yg", ct)], [("ysq", 0)])
                    self.MM(pss, self.ones_bf[:], ysq[:, 0, :], i == 0, i == 1, [("ones",), ("ysq", 0)], P71)
                self.ACT(rs[:], pss, AF.Ln, P71, [("rs",)], bias=self.eps_col[:, 0:1], scale=1.0 / 256)
                self.ACT(rs[:], rs[:], AF.Exp, [("rs",)], [("rs",)], scale=-0.5)
                for i in range(2):
                    ct = 2 * g + i
                    self.TT("dve", yg[:, ct, :], yg[:, ct, :], rs[:], ALU.mult, [("yg", ct), ("rs",)], [("yg", ct)])
                    self.ACT(self.catT[:, 4 + ct, tok], yg[:, ct, :], AF.Identity, [("yg", ct), ("pc",)], [("catT", 4 + ct, cq)],
                             scale=self.pcol("snorm_w", ct))

    def mix0_out(self, o, lim):
        S = self.S
        dr = self.dr
        wo = self.carve(o, 16384, BF16).rearrange("p (k c) -> p k c", k=8); o += 16384
        o = self.ln_scratch(o)
        assert o <= lim
        for i in range(2):
            self.load_w_cols(wo[:, :, i * 512:(i + 1) * 512], dr["w_out"], i * 512, 512, "wsl%d" % i, ("wo", i))
        for c in range(4):
            tok = slice(c * 512, (c + 1) * 512)
            for dt in range(NKT):
                b = dt % 2
                po = self.psum[:, b, :]
                for kt in range(NKT):
                    self.MM(po, wo[:, kt, dt * 128:(dt + 1) * 128], self.catT[:, kt, tok], kt == 0, kt == NKT - 1,
                            [("wo", dt // 4), ("catT", kt, c)], [("ps", b)])
                self.STT("dve", self.X[:, dt, tok], self.X[:, dt, tok], ALPHA, po, ALU.mult, ALU.add,
                         [("X", dt, c), ("ps", b)], [("X", dt, c)])
            self.ln_chunk(c, "ln_mix_g0", "ln_mix_b0")

    def mixer1(self):
        S = self.S
        dr = self.dr
        S.barrier()
        pw1 = self.carve(self.OFF_W, 32768, BF16).rearrange("p (k c) -> p k c", k=8)
        pw2 = self.carve(self.OFF_W + 32768, 16384, BF16).rearrange("p (k c) -> p k c", k=8)
        for i in range(4):
            self.load_w_cols(pw1[:, :, i * 512:(i + 1) * 512], dr["w_pw1"], i * 512, 512, "wsl%d" % i, ("wsl", i))
        for i in range(2):
            self.load_w_cols(pw2[:, :, i * 512:(i + 1) * 512], dr["w_pw2"], i * 512, 512, "wsl%d" % (4 + i), ("wsl", 4 + i))
        o = self.OFF_S
        hp = self.carve(o, 8704, BF16)[:, 0:8 * 542].rearrange("p (c t) -> p c t", c=8); o += 8704
        diag2 = self.carve(o, 2 * 7936, BF16).rearrange("p (b k m) -> p b k m", b=2, k=31); o += 2 * 7936
        sig = self.carve(o, 2048, F32); o += 2048
        conv_o = self.carve(o, 16384, F32).rearrange("p (c t) -> p c t", c=8); o += 16384
        hs = self.carve(o, 8192, BF16).rearrange("p (c t) -> p c t", c=8); o += 8192
        o = self.ln_scratch(o)
        assert o <= self.OFF_S + self.SSZ, o - self.OFF_S
        sc = self.ln_scr
        for ct in range(8):
            self.MEMSET("pool", hp[:, ct, 0:30], 0.0, [("hp", ct)])
        ow, _ = self.pc_off["dw_w"]
        for c in range(4):
            tok = slice(c * 512, (c + 1) * 512)
            for dt in range(NKT):
                self.ACT(self.X[:, dt, tok], self.X[:, dt, tok], AF.Identity, [("X", dt, c), ("pc",)], [("X", dt, c)],
                         bias=self.pcol("b_pw2", dt), scale=ALPHA)
            for ct in range(8):
                pv = self.psum[:, 0 + ct % 2, :]
                pg = self.psum[:, 2 + ct % 2, :]
                pcv = self.psum[:, 4 + ct % 2, :]
                for kt in range(NKT):
                    self.MM(pv, pw1[:, kt, ct * 128:(ct + 1) * 128], self.XT[:, kt, tok], kt == 0, kt == NKT - 1,
                            [("wsl", ct // 4), ("XT", kt, c)], [("ps", ct % 2)])
                for kt in range(NKT):
                    self.MM(pg, pw1[:, kt, 1024 + ct * 128:1024 + (ct + 1) * 128], self.XT[:, kt, tok], kt == 0, kt == NKT - 1,
                            [("wsl", 2 + ct // 4), ("XT", kt, c)], [("ps", 2 + ct % 2)])
                self.ACT(sig[:], pg, AF.Sigmoid, [("ps", 2 + ct % 2), ("pc",)], [("sig",)], bias=self.pcol("b_pw1", 8 + ct))
                self.STT("dve", hp[:, ct, 30:542], pv, self.pcol("b_pw1", ct), sig[:], ALU.add, ALU.mult,
                         [("ps", ct % 2), ("sig",), ("pc",)], [("hp", ct)])
                wk = self.pc[:, ow + ct:ow + 248:8]
                diag = diag2[:, ct % 2]
                self.TT("pool", diag, self.ident_bf[:].unsqueeze(1).to_broadcast([128, 31, 128]),
                        wk.unsqueeze(2).to_broadcast([128, 31, 128]), ALU.mult, [("ident",), ("pc",)], [("diag", ct % 2)])
                for k in range(31):
                    self.MM(pcv, diag[:, k, :], hp[:, ct, k:k + 512], k == 0, k == 30,
                            [("diag", ct % 2), ("hp", ct)], [("ps", 4 + ct % 2)])
                self.ACT(conv_o[:, ct, :], pcv, AF.Identity, [("ps", 4 + ct % 2), ("pc",)], [("conv_o", ct)],
                         bias=self.pcol("dw_b", ct))
                self.CP("pool", hp[:, ct, 0:30], hp[:, ct, 512:542], [("hp", ct)], [("hp", ct)])
            self.ln_stats([conv_o[:, ct, :] for ct in range(8)], [("conv_o", ct) for ct in range(8)])
            for ct in range(8):
                xs = conv_o[:, ct, :]
                gcol = self.pcol("cln_g", ct)
                self.STT("dve", xs, xs, gcol, sc["rstd"][:], ALU.mult, ALU.mult, [("conv_o", ct), ("ln_rstd",), ("pc",)], [("conv_o", ct)])
                self.STT("dve", xs, sc["mr"][:], gcol, xs, ALU.mult, ALU.subtract, [("conv_o", ct), ("ln_mr",), ("pc",)], [("conv_o", ct)])
                self.ACT(hs[:, ct, :], xs, AF.Silu, [("conv_o", ct), ("pc",)], [("hs", ct)],
                         bias=self.pcol("cln_b", ct), scale=-1.0)
            for dt in range(NKT):
                b = dt % 2
                po = self.psum[:, b, :]
                for ct in range(8):
                    self.MM(po, pw2[:, ct, dt * 128:(dt + 1) * 128], hs[:, ct, :], ct == 0, ct == 7,
                            [("wsl", 4 + dt // 4), ("hs", ct)], [("ps", b)])
                self.TT("dve", self.X[:, dt, tok], self.X[:, dt, tok], po, ALU.add, [("X", dt, c), ("ps", b)], [("X", dt, c)])
            self.ln_chunk(c, "ln_mix_g1", "ln_mix_b1")
        S.barrier()

_STAGES = ["mix0", "ffn0", "mix1", "ffn1"]


def _prep_inputs(inp):
    pc = pack_params(inp)
    pcarr = pc.array()
    shared = {
        "pcols": pcarr,
        "w_in": np.ascontiguousarray(np.asarray(inp["mix_w_in"][0], np.float32)),
        "w_out": np.ascontiguousarray(np.asarray(inp["mix_w_out"][0], np.float32)),
        "w_pw1": np.ascontiguousarray(np.asarray(inp["conv_w_pw1"][0], np.float32)),
        "w_pw2": np.ascontiguousarray(np.asarray(inp["conv_w_pw2"][0], np.float32)),
    }
    for l in range(2):
        shared["w_gate%d" % l] = np.ascontiguousarray(np.asarray(inp["ffn_w_gate"][l], np.float32))
        shared["w_up%d" % l] = np.ascontiguousarray(np.asarray(inp["ffn_w_up"][l], np.float32))
        shared["w_down%d" % l] = np.ascontiguousarray(np.asarray(inp["ffn_w_down"][l], np.float32))
    slopes = 2.0 ** (-(np.arange(8, dtype=np.float64) + 1.0))
    qt = np.arange(16, dtype=np.float64)[:, None, None]
    nn = np.arange(8, dtype=np.float64)[None, None, :]
    ct = np.exp(np.minimum(-slopes[None, :, None] * 128.0 * (qt - 2 * nn - 1), 80.0))
    shared["ctab"] = np.ascontiguousarray(np.tile(ct.reshape(1, -1), (128, 1)).astype(np.float32))
    jrel = (np.arange(128, dtype=np.float64)[:, None, None] + 128.0 * np.arange(2)[None, :, None] - 128.0)
    shared["wtab"] = np.ascontiguousarray(np.exp(slopes[None, None, :] * jrel).reshape(128, 16).astype(np.float32))
    x = np.asarray(inp["x"], np.float32)
    in_maps = []
    for b in range(8):
        m = dict(shared)
        m["xT"] = np.ascontiguousarray(x[b].T)
        in_maps.append(m)
    return pc, in_maps


def run(inp, stages=_STAGES, dbg=None, trace=False):
    pc, in_maps = _prep_inputs(inp)
    nc = bass.Bass("TRN2", target_bir_lowering=False)
    k = K(nc, pc.off, pc.n, stages, dbg)
    k.build()
    res = run_bass_kernel_spmd(nc, in_maps, core_ids=list(range(8)), trace=trace)
    return res


def kernel(**inputs):
    res = run(inputs)
    out = np.stack([np.ascontiguousarray(r["outT"].T) for r in res.results], axis=0)
    return out.astype(np.float32)
```
